# Optimizing a Trainium2 kernel written in Bass

```python
import jax
import jax.numpy as jnp
from jax import lax
import numpy as np


D_MODEL = 2048
BATCH = 1
SEQ = 8192
DEPTH = 1

GRID_W = 64
CTX_LEN = 256
HEAD_DIM = 128
N_Q_HEADS = 8
N_KV_HEADS = 2
Q_PER_KV = N_Q_HEADS // N_KV_HEADS
ATTN_WIDTH = N_Q_HEADS * HEAD_DIM
KV_WIDTH = N_KV_HEADS * HEAD_DIM
CONV_WIDTH = D_MODEL - ATTN_WIDTH
CONV_KSIZE = 31
WINDOW = 128
BLOCK = 128
ROPE_THETA = 10000.0
ROPE_AXIS_DIM = HEAD_DIM // 2
N_EXPERTS = 16
EC_CAPACITY = 2
D_FF = 5632
N_MOD = 6
LN_EPS = 1e-5
NEG_INF = -1e30
DEEPNORM_ALPHA = (2.0 * DEPTH) ** 0.25
DEEPNORM_BETA = (8.0 * DEPTH) ** -0.25
CONV_G_START = CONV_WIDTH
Q_START = 2 * CONV_WIDTH
K_START = Q_START + ATTN_WIDTH
V_START = K_START + KV_WIDTH
IN_COLS = V_START + KV_WIDTH

kernel_name = 'hybrid_conv_swa_ecmoe_diffusion_block'


def layer_norm(x, g=None, b=None):
    xf = x.astype(jnp.float32)
    mu = jnp.mean(xf, axis=-1, keepdims=True)
    var = jnp.mean(jnp.square(xf - mu), axis=-1, keepdims=True)
    y = (xf - mu) * lax.rsqrt(var + LN_EPS)
    if g is not None:
        y = y * g.astype(jnp.float32) + b.astype(jnp.float32)
    return y.astype(x.dtype)


def modulate(x, shift, scale):
    return layer_norm(x) * (1.0 + scale) + shift


def axial_rope_tables(row, col):
    inv = ROPE_THETA ** (-jnp.arange(0, ROPE_AXIS_DIM, 2, dtype=jnp.float32) / ROPE_AXIS_DIM)
    ang_r = row[:, None] * inv[None, :]
    ang_c = col[:, None] * inv[None, :]
    expand = lambda a: a[None, :, None, :]
    return (expand(jnp.cos(ang_r)), expand(jnp.sin(ang_r)), expand(jnp.cos(ang_c)), expand(jnp.sin(ang_c)))


def rotate_half_rope(x, cos, sin):
    x1, x2 = jnp.split(x, 2, axis=-1)
    return jnp.concatenate([x1 * cos - x2 * sin, x2 * cos + x1 * sin], axis=-1)


def apply_axial_rope(x, tables):
    cos_r, sin_r, cos_c, sin_c = tables
    xf = x.astype(jnp.float32)
    xr = rotate_half_rope(xf[..., :ROPE_AXIS_DIM], cos_r, sin_r)
    xc = rotate_half_rope(xf[..., ROPE_AXIS_DIM:], cos_c, sin_c)
    return jnp.concatenate([xr, xc], axis=-1).astype(x.dtype)


def split_proj(p, n_batch, n):
    v_conv = p[..., :CONV_G_START]
    g_conv = p[..., CONV_G_START:Q_START]
    q = p[..., Q_START:K_START].reshape(n_batch, n, N_Q_HEADS, HEAD_DIM)
    k = p[..., K_START:V_START].reshape(n_batch, n, N_KV_HEADS, HEAD_DIM)
    v = p[..., V_START:].reshape(n_batch, n, N_KV_HEADS, HEAD_DIM)
    return v_conv, g_conv, q, k, v


def conv_group(v_conv, g_conv, w_dw, b_dw, ln_g, ln_b):
    u = v_conv * jax.nn.sigmoid(g_conv)
    u = lax.conv_general_dilated(
        u, w_dw[:, None, :].astype(u.dtype), window_strides=(1,),
        padding=[(CONV_KSIZE // 2, CONV_KSIZE // 2)],
        dimension_numbers=('NWC', 'WIO', 'NWC'),
        feature_group_count=u.shape[-1]) + b_dw
    u = layer_norm(u, ln_g, ln_b)
    return jax.nn.silu(u)


def window_attention(q, k, v, kc, vc, sink):
    b, n = q.shape[0], q.shape[1]
    nb = n // BLOCK
    scale = HEAD_DIM ** -0.5
    qb = q.reshape(b, nb, BLOCK, N_KV_HEADS, Q_PER_KV, HEAD_DIM)
    pad = ((0, 0), (BLOCK, BLOCK), (0, 0), (0, 0))
    kp = jnp.pad(k, pad).reshape(b, nb + 2, BLOCK, N_KV_HEADS, HEAD_DIM)
    vp = jnp.pad(v, pad).reshape(b, nb + 2, BLOCK, N_KV_HEADS, HEAD_DIM)
    kb = jnp.concatenate([kp[:, :-2], kp[:, 1:-1], kp[:, 2:]], axis=2)
    vb = jnp.concatenate([vp[:, :-2], vp[:, 1:-1], vp[:, 2:]], axis=2)
    s_win = jnp.einsum('bnqkgd,bnmkd->bnkgqm', qb, kb).astype(jnp.float32) * scale
    s_ctx = jnp.einsum('bnqkgd,bckd->bnkgqc', qb, kc).astype(jnp.float32) * scale
    qi = jnp.arange(BLOCK)[:, None]
    m = jnp.arange(3 * BLOCK)[None, :]
    kpos = jnp.arange(nb)[:, None, None] * BLOCK - BLOCK + m[None]
    valid = (jnp.abs(m - qi - BLOCK) <= WINDOW)[None] & (kpos >= 0) & (kpos < n)
    s_win = jnp.where(valid[None, :, None, None], s_win, NEG_INF)
    s_sink = jnp.broadcast_to(
        sink.astype(jnp.float32).reshape(N_KV_HEADS, Q_PER_KV)[None, None, :, :, None, None],
        s_win.shape[:-1] + (1,))
    p = jax.nn.softmax(jnp.concatenate([s_ctx, s_win, s_sink], axis=-1), axis=-1)
    n_ctx = kc.shape[1]
    p_ctx = p[..., :n_ctx].astype(v.dtype)
    p_win = p[..., n_ctx:n_ctx + 3 * BLOCK].astype(v.dtype)
    o = (jnp.einsum('bnkgqc,bckd->bnqkgd', p_ctx, vc)
         + jnp.einsum('bnkgqm,bnmkd->bnqkgd', p_win, vb))
    return o.reshape(b, n, ATTN_WIDTH)


def context_attention(q, k, v, sink):
    b, n_ctx = q.shape[0], q.shape[1]
    qg = q.reshape(b, n_ctx, N_KV_HEADS, Q_PER_KV, HEAD_DIM)
    s = jnp.einsum('bqkgd,bckd->bkgqc', qg, k).astype(jnp.float32) * HEAD_DIM ** -0.5
    s_sink = jnp.broadcast_to(
        sink.astype(jnp.float32).reshape(N_KV_HEADS, Q_PER_KV)[None, :, :, None, None],
        s.shape[:-1] + (1,))
    p = jax.nn.softmax(jnp.concatenate([s, s_sink], axis=-1), axis=-1)
    o = jnp.einsum('bkgqc,bckd->bqkgd', p[..., :n_ctx].astype(v.dtype), v)
    return o.reshape(b, n_ctx, ATTN_WIDTH)


def expert_choice_ffn(h, w_router, w_gate, w_up, w_down):
    b, n, _ = h.shape
    cap = EC_CAPACITY * n // N_EXPERTS
    aff = jax.nn.softmax(jnp.einsum('bnd,de->bne', h, w_router).astype(jnp.float32), axis=-1)
    g, idx = lax.top_k(jnp.transpose(aff, (0, 2, 1)), cap)
    bidx = jnp.arange(b)[:, None, None]
    xs = h[bidx, idx]
    a = jnp.einsum('becd,edf->becf', xs, w_gate)
    u = jnp.einsum('becd,edf->becf', xs, w_up)
    y = jnp.einsum('becf,efd->becd', jax.nn.silu(a) * u, w_down) * g[..., None].astype(h.dtype)
    return jnp.zeros_like(h).at[bidx, idx].add(y)


def setup_inputs(seed: int = 0) -> dict:
    key = jax.random.key(seed)
    ks = jax.random.split(key, 24)
    nrm = lambda k, shape, s: jax.random.normal(k, shape, jnp.float32) * s
    d = D_MODEL
    return {
        'x': nrm(ks[0], (BATCH, SEQ, d), 1.0),
        'c': nrm(ks[1], (BATCH, d), 1.0),
        'ctx': nrm(ks[2], (BATCH, CTX_LEN, d), 1.0),
        'c_ctx': nrm(ks[3], (d,), 1.0),
        'w_mod': nrm(ks[4], (DEPTH, d, N_MOD * d), 0.5 * d ** -0.5),
        'b_mod': nrm(ks[5], (DEPTH, N_MOD * d), 0.02),
        'w_in': nrm(ks[6], (DEPTH, d, IN_COLS), d ** -0.5),
        'b_in': nrm(ks[7], (DEPTH, IN_COLS), 0.02),
        'w_dw': nrm(ks[8], (DEPTH, CONV_KSIZE, CONV_WIDTH), CONV_KSIZE ** -0.5),
        'b_dw': nrm(ks[9], (DEPTH, CONV_WIDTH), 0.02),
        'conv_ln_g': 1.0 + nrm(ks[10], (DEPTH, CONV_WIDTH), 0.02),
        'conv_ln_b': nrm(ks[11], (DEPTH, CONV_WIDTH), 0.02),
        'sink': nrm(ks[12], (DEPTH, N_Q_HEADS), 0.5),
        'w_out': nrm(ks[13], (DEPTH, d, d), DEEPNORM_BETA * d ** -0.5),
        'b_out': nrm(ks[14], (DEPTH, d), 0.02),
        'ln1_g': 1.0 + nrm(ks[15], (DEPTH, d), 0.02),
        'ln1_b': nrm(ks[16], (DEPTH, d), 0.02),
        'w_router': nrm(ks[17], (DEPTH, d, N_EXPERTS), d ** -0.5),
        'w_gate': nrm(ks[18], (DEPTH, N_EXPERTS, d, D_FF), d ** -0.5),
        'w_up': nrm(ks[19], (DEPTH, N_EXPERTS, d, D_FF), d ** -0.5),
        'w_down': nrm(ks[20], (DEPTH, N_EXPERTS, D_FF, d), DEEPNORM_BETA * D_FF ** -0.5),
        'ln2_g': 1.0 + nrm(ks[21], (DEPTH, d), 0.02),
        'ln2_b': nrm(ks[22], (DEPTH, d), 0.02),
    }


def reference(x, c, ctx, c_ctx, w_mod, b_mod, w_in, b_in, w_dw, b_dw, conv_ln_g, conv_ln_b,
              sink, w_out, b_out, ln1_g, ln1_b, w_router, w_gate, w_up, w_down, ln2_g, ln2_b):
    b, n, _ = x.shape
    n_ctx = ctx.shape[1]
    rows = n // GRID_W
    row = jnp.repeat(jnp.arange(rows, dtype=jnp.float32), GRID_W)
    col = jnp.tile(jnp.arange(GRID_W, dtype=jnp.float32), rows)
    rope = axial_rope_tables(row, col)
    for l in range(DEPTH):
        last = l == DEPTH - 1
        mod = (jax.nn.silu(c) @ w_mod[l] + b_mod[l]).reshape(b, N_MOD, 1, D_MODEL)
        mod_c = (jax.nn.silu(c_ctx) @ w_mod[l] + b_mod[l]).reshape(1, N_MOD, 1, D_MODEL)
        h = modulate(x, mod[:, 0], mod[:, 1])
        hc = modulate(ctx, mod_c[:, 0], mod_c[:, 1])
        v_conv, g_conv, q, k, v = split_proj(h @ w_in[l] + b_in[l], b, n)
        q = apply_axial_rope(q, rope)
        k = apply_axial_rope(k, rope)
        if last:
            pc = hc @ w_in[l][:, K_START:] + b_in[l][K_START:]
            kc = pc[..., :KV_WIDTH].reshape(b, n_ctx, N_KV_HEADS, HEAD_DIM)
            vc = pc[..., KV_WIDTH:].reshape(b, n_ctx, N_KV_HEADS, HEAD_DIM)
        else:
            v_conv_c, g_conv_c, qc, kc, vc = split_proj(hc @ w_in[l] + b_in[l], b, n_ctx)
        a_conv = conv_group(v_conv, g_conv, w_dw[l], b_dw[l], conv_ln_g[l], conv_ln_b[l])
        a_attn = window_attention(q, k, v, kc, vc, sink[l])
        mix = jnp.concatenate([a_conv, a_attn], axis=-1) @ w_out[l] + b_out[l]
        x_mid = layer_norm(DEEPNORM_ALPHA * x + mod[:, 2] * mix, ln1_g[l], ln1_b[l])
        h2 = modulate(x_mid, mod[:, 3], mod[:, 4])
        ffn = expert_choice_ffn(h2, w_router[l], w_gate[l], w_up[l], w_down[l])
        x_next = layer_norm(DEEPNORM_ALPHA * x_mid + mod[:, 5] * ffn, ln2_g[l], ln2_b[l])
        if not last:
            a_conv_c = conv_group(v_conv_c, g_conv_c, w_dw[l], b_dw[l], conv_ln_g[l], conv_ln_b[l])
            a_attn_c = context_attention(qc, kc, vc, sink[l])
            mix_c = jnp.concatenate([a_conv_c, a_attn_c], axis=-1) @ w_out[l] + b_out[l]
            ctx_mid = layer_norm(DEEPNORM_ALPHA * ctx + mod_c[:, 2] * mix_c, ln1_g[l], ln1_b[l])
            h2c = modulate(ctx_mid, mod_c[:, 3], mod_c[:, 4])
            ffn_c = expert_choice_ffn(h2c, w_router[l], w_gate[l], w_up[l], w_down[l])
            ctx = layer_norm(DEEPNORM_ALPHA * ctx_mid + mod_c[:, 5] * ffn_c, ln2_g[l], ln2_b[l])
        x = x_next
    return x
```

```python
import numpy as np
import concourse.bass as bass
import concourse.mybir as mybir
from concourse.bass_utils import run_bass_kernel_spmd

F32 = mybir.dt.float32
BF16 = mybir.dt.bfloat16
I32 = mybir.dt.int32
AF = mybir.ActivationFunctionType
ALU = mybir.AluOpType
AX = mybir.AxisListType

NCORES = 8
D = 2048
KC = 16
NT = 10
TOK = 1280
DFF = 5632
FC = 44
CAP = 1024
ALPHA = 2.0 ** 0.25
EPS = 1e-5


class Eng:
    def __init__(self, nc, e, name):
        self.e = e
        self.name = name
        self.sem = nc.alloc_semaphore("s_" + name)
        self.n = 0
        self.seen = {}
        self.serial = False


class Ser:
    def __init__(self, eng):
        self._eng = eng
        eng.serial = True

    def __getattr__(self, name):
        eng = self._eng
        fn = getattr(eng.e, name)

        def call(*a, **k):
            if eng.n > 0:
                eng.e.wait_ge(eng.sem, eng.n)
            ins = fn(*a, **k)
            eng.n += 1
            ins.then_inc(eng.sem, 1)
            return ins
        return call


class Ctx:
    def __init__(self, nc):
        self.nc = nc
        self.pe = Eng(nc, nc.tensor, "pe")
        self.act = Eng(nc, nc.scalar, "act")
        self.dve = Eng(nc, nc.vector, "dve")
        self.pool = Eng(nc, nc.gpsimd, "pool")
        self.sp = Eng(nc, nc.sync, "sp")
        self.nsem = 0

    def done(self, eng, ins):
        if eng.serial:
            return ("e", eng, eng.n)
        eng.n += 1
        ins.then_inc(eng.sem, 1)
        return ("e", eng, eng.n)

    def newsem(self):
        self.nsem += 1
        return [self.nc.alloc_semaphore("d%d" % self.nsem), 0]

    def dma(self, eng, out, in_, ds, **kw):
        ins = eng.e.dma_start(out=out, in_=in_, **kw)
        ds[1] += 16
        ins.then_inc(ds[0], 16)
        return ("d", ds[0], ds[1])

    def need(self, eng, tok):
        if tok is None:
            return
        if isinstance(tok, list):
            for t in tok:
                self.need(eng, t)
            return
        if tok[0] == "e":
            src, n = tok[1], tok[2]
            if src is eng:
                return
            key = src.name
        else:
            key, n = id(tok[1]), tok[2]
        if eng.seen.get(key, 0) >= n:
            return
        eng.seen[key] = n
        eng.e.wait_ge(tok[1].sem if tok[0] == "e" else tok[1], n)


def build(debug=None):
    nc = bass.Bass("TRN2", target_bir_lowering=False)

    def din(name, shape, dt=F32):
        return nc.dram_tensor(name, list(shape), dt, kind="ExternalInput").ap()

    def dout(name, shape, dt=F32):
        return nc.dram_tensor(name, list(shape), dt, kind="ExternalOutput").ap()

    xh = din("xh", [TOK, D])
    ctxd = din("ctx", [256, D])
    win_d = din("win", [7, 128, KC * 512])
    pcols_d = din("pcols", [128, 16 + 8 * 31 + 24 + 2])
    bqkv_d = din("bqkv", [128, 1536])
    rope_d = din("rope", [128, NT, 128])
    masks_d = din("masks", [128, 2, 128])
    sink_d = din("sink", [128, 8])
    wout_d = din("wout", [4, 128, KC * 512])
    rows_d = din("rows", [5, 128, D])
    wr_d = din("wr", [128, KC * 16])
    modT_d = din("modT", [128, 192])
    mrows_d = din("mrows", [3, 128, D])

    dbg = {}

    def dbg_out(name, shape, dt=F32):
        dbg[name] = dout("dbg_" + name, shape, dt)
        return dbg[name]

    mixd = nc.dram_tensor("mixd", [1024, D], F32)

    def sb(name, cols, dt=F32):
        return nc.alloc_sbuf_tensor("sb_" + name, [128, cols], dt)[:]

    ident_b = sb("ident_b", 128, BF16)
    ident_f = sb("ident_f", 128, F32)
    pcols = sb("pcols", 16 + 8 * 31 + 24 + 2)
    bconv = pcols[:, 0:16]
    wdw = pcols[:, 16:16 + 248].rearrange("p (c k) -> p c k", k=31)
    bdw = pcols[:, 264:272]
    clg = pcols[:, 272:280]
    clb = pcols[:, 280:288]
    hval = pcols[:, 288:290]
    modT = sb("modT", 2 * 96)
    sc1p = sb("sc1p", 32)
    sc2p = sb("sc2p", 16)
    ones_f = sb("ones_f", 128, F32)
    ones_b = sb("ones_b", 128, BF16)
    small = sb("small", 64)
    stats = sb("stats", 4 * 6 * 2)
    bqkv = sb("bqkv", 1536)
    rope_t = sb("rope_t", NT * 128)
    masks_f = sb("masks_f", 256)
    masks = sb("masks", 4 * 128, BF16)
    sinke = sb("sinke", 8)
    wr = sb("wr", KC * 16)

    RA = sb("RA", 16384)
    RB = sb("RB", 8448)
    RC = sb("RC", 8192)
    RE = sb("RE", 7680)
    RF = sb("RF", 6144)

    hT = RA[:, 0:10240].bitcast(BF16).rearrange("p (k t) -> p k t", k=KC)
    hcT = RA[:, 10240:12288].bitcast(BF16).rearrange("p (k t) -> p k t", k=KC)
    xstage = [RA[:, 12288:14336], RA[:, 14336:16384]]
    xn_b = [RF[:, 0:1024].bitcast(BF16), RF[:, 1024:2048].bitcast(BF16)]
    wslot = [RC[:, 0:4096].bitcast(BF16).rearrange("p (k c) -> p k c", k=KC),
             RC[:, 4096:8192].bitcast(BF16).rearrange("p (k c) -> p k c", k=KC)]
    wmod_sb = RC.rearrange("p (k c) -> p k c", k=KC)
    uT = RB.rearrange("p (c t) -> p c t", c=8)
    qT = RE[:, 0:4096].bitcast(BF16).rearrange("p (h t) -> p h t", h=8)
    kT = RE[:, 4096:5376].bitcast(BF16).rearrange("p (h t) -> p h t", h=2)
    vtok = RE[:, 5376:6656].bitcast(BF16).rearrange("p (t c) -> p t c", t=NT)
    kcT = RE[:, 6656:6912].bitcast(BF16).rearrange("p (h t) -> p h t", h=2)
    vctok = RE[:, 6912:7168].bitcast(BF16).rearrange("p (t c) -> p t c", t=2)
    qtok = RF[:, 2048:2560]
    ropeA = RF[:, 2560:3072]
    ropeB = RF[:, 3072:3584]
    qrb = RF[:, 3584:3840].bitcast(BF16)
    gsig = RF[:, 3840:4224]

    ps = [nc.alloc_psum_tensor("ps%d" % i, [128, 512], F32)[:] for i in range(6)]
    psb = [nc.alloc_psum_tensor("psb%d" % i, [128, 1024], BF16)[:] for i in range(2)]

    C = Ctx(nc)
    pe, act, dve, pool, sp = C.pe, C.act, C.dve, C.pool, C.sp
    T, S = nc.tensor, nc.sync
    V, A, G = Ser(dve), Ser(act), Ser(pool)

    with nc.Block() as block:
        @block.sync
        def _(sync_engine):
            ld0 = C.newsem()
            C.dma(sp, pcols, pcols_d, ld0)
            C.dma(sp, bqkv, bqkv_d, ld0)
            C.dma(sp, rope_t, rope_d.rearrange("p t c -> p (t c)"), ld0)
            C.dma(sp, masks_f, masks_d.rearrange("p a c -> p (a c)"), ld0)
            C.dma(sp, sinke, sink_d, ld0)
            t_ld0 = C.dma(sp, wr, wr_d, ld0)

            G.memset(ident_b, 0.0)
            G.affine_select(out=ident_b, in_=ident_b, pattern=[[-1, 128]], compare_op=ALU.not_equal,
                            fill=1.0, base=0, channel_multiplier=1)
            G.memset(ones_f, 1.0)
            G.memset(ones_b, 1.0)
            G.memset(ident_f, 0.0)
            t_id = C.done(pool, G.affine_select(out=ident_f, in_=ident_f, pattern=[[-1, 128]],
                                                compare_op=ALU.not_equal, fill=1.0, base=0,
                                                channel_multiplier=1))

            t_mt = C.dma(sp, modT, modT_d, ld0)
            C.need(dve, t_mt)
            V.tensor_scalar(out=sc1p[:, 0:16], in0=modT[:, 16:32], scalar1=1.0, scalar2=None, op0=ALU.add)
            V.tensor_scalar(out=sc1p[:, 16:32], in0=modT[:, 96 + 16:96 + 32], scalar1=1.0, scalar2=None, op0=ALU.add)
            t_mod = C.done(dve, V.tensor_scalar(out=sc2p, in0=modT[:, 64:80], scalar1=1.0, scalar2=None, op0=ALU.add))
            sh1 = [modT[:, 0:16], modT[:, 96:96 + 16]]
            sh2 = modT[:, 48:64]

            if debug == "ln0":
                o = dbg_out("sc", [128, 32])
                C.need(sp, t_mod)
                C.need(sp, t_id)
                t = C.dma(sp, o, sc1p, C.newsem())
                C.need(sp, t)
                return
            wsem = [C.newsem(), C.newsem()]
            wtok = {}
            wrel = {}

            def issue_w(g, src):
                slot = g % 2
                if g - 2 in wrel:
                    C.need(pool, wrel[g - 2])
                for q in range(4):
                    tk = C.dma(pool, wslot[slot].rearrange("p k c -> p (k c)")[:, q * 2048:(q + 1) * 2048],
                               src[:, q * 2048:(q + 1) * 2048], wsem[slot])
                wtok[g] = tk

            if debug != "ln1":
                issue_w(0, win_d[0])
                issue_w(1, win_d[1])

            xsem = [C.newsem(), C.newsem()]
            x_rel = [None, None]
            xn_rel = [None, None]
            tr_rel = [None, None]
            t_h = None
            for it in range(NT + 2):
                s = it % 2
                isctx = it >= NT
                src = ctxd[(it - NT) * 128:(it - NT + 1) * 128, :] if isctx else xh[it * 128:(it + 1) * 128, :]
                C.need(sp, x_rel[s])
                t_x = C.dma(sp, xstage[s], src, xsem[s])
                C.need(dve, t_x)
                st = stats[:, 0:24].rearrange("p (c s) -> p c s", s=6)
                for c4 in range(4):
                    V.bn_stats(out=st[:, c4, :], in_=xstage[s][:, c4 * 512:(c4 + 1) * 512])
                mv = small[:, 0:2]
                V.bn_aggr(out=mv, in_=stats[:, 0:24])
                rstd = small[:, 2 + 2 * s:3 + 2 * s]
                nmr = small[:, 3 + 2 * s:4 + 2 * s]
                C.need(dve, xn_rel[s])
                t_v = C.done(dve, V.tensor_scalar(out=small[:, 8:9], in0=mv[:, 1:2], scalar1=EPS, scalar2=None, op0=ALU.add))
                C.need(act, t_v)
                t_sq = C.done(act, A.sqrt(out=small[:, 9:10], in_=small[:, 8:9]))
                C.need(dve, t_sq)
                V.reciprocal(out=rstd, in_=small[:, 9:10])
                t_st = C.done(dve, V.scalar_tensor_tensor(out=nmr, in0=mv[:, 0:1], scalar=-1.0, in1=rstd,
                                                          op0=ALU.mult, op1=ALU.mult))
                C.need(act, t_st)
                C.need(act, xn_rel[s])
                t_xn = C.done(act, A.activation(out=xn_b[s], in_=xstage[s], func=AF.Identity, bias=nmr, scale=rstd))
                x_rel[s] = t_xn
                C.need(pe, t_xn)
                C.need(pe, t_id)
                for half in range(2):
                    pst = psb[half]
                    C.need(pe, tr_rel[half])
                    for k8 in range(8):
                        kc = half * 8 + k8
                        mm = T.transpose(pst[:, k8 * 128:(k8 + 1) * 128], xn_b[s][:, kc * 128:(kc + 1) * 128], ident_b)
                    t_tr = C.done(pe, mm)
                    if half == 1:
                        xn_rel[s] = t_tr
                    dst = hcT if isctx else hT
                    t0 = (it - NT) * 128 if isctx else it * 128
                    j = 1 if isctx else 0
                    C.need(dve, t_tr)
                    C.need(act, t_tr)
                    C.need(dve, t_mod)
                    C.need(act, t_mod)
                    for k8 in range(8):
                        kc = half * 8 + k8
                        o_ = dst[:, kc, t0:t0 + 128]
                        i_ = pst[:, k8 * 128:(k8 + 1) * 128]
                        insd = V.tensor_scalar(out=o_, in0=i_, scalar1=sc1p[:, j * 16 + kc:j * 16 + kc + 1],
                                               scalar2=sh1[j][:, kc:kc + 1], op0=ALU.mult, op1=ALU.add)
                    td = C.done(dve, insd)
                    ta = td
                    tr_rel[half] = [td, ta]
                    t_h = [td, ta]

            if debug in ("h", "ln1"):
                o = dbg_out("hT", [128, 6 * TOK], F32)
                C.need(dve, t_h)
                t_c = C.done(dve, V.tensor_copy(out=RB[:, 0:6 * TOK], in_=hT.rearrange("p k t -> p (k t)")[:, 0:6 * TOK]))
                C.need(sp, t_c)
                t = C.dma(sp, o, RB[:, 0:6 * TOK], C.newsem())
                C.need(sp, t)
                return

            C.need(pe, t_h)
            C.need(act, t_ld0)
            C.need(dve, t_ld0)
            bank_rel = {}

            def getbank(b, eng):
                C.need(eng, bank_rel.get(b))

            TP = 352
            t_u = None
            for g in range(4):
                if g >= 2:
                    pass
                C.need(pe, wtok[g])
                W = wslot[g % 2]
                for cj in range(2):
                    cc = 2 * g + cj
                    for tp in range(3):
                        tk0 = 112 + tp * TP
                        bg, bv = (tp % 2) * 2, 1 + (tp % 2) * 2
                        getbank(bg, pe)
                        for kc in range(KC):
                            mm = T.matmul(ps[bg][:, 0:TP], lhsT=W[:, kc, 256 + cj * 128:256 + (cj + 1) * 128],
                                          rhs=hT[:, kc, tk0:tk0 + TP], start=(kc == 0), stop=(kc == KC - 1))
                        t_g = C.done(pe, mm)
                        getbank(bv, pe)
                        for kc in range(KC):
                            mm = T.matmul(ps[bv][:, 0:TP], lhsT=W[:, kc, cj * 128:(cj + 1) * 128],
                                          rhs=hT[:, kc, tk0:tk0 + TP], start=(kc == 0), stop=(kc == KC - 1))
                        t_v = C.done(pe, mm)
                        C.need(act, t_g)
                        C.need(act, bank_rel.get(("gsig", tp % 2)))
                        gs_ = gsig if tp % 2 == 0 else ropeA[:, 0:384]
                        t_s = C.done(act, A.activation(out=gs_[:, 0:TP], in_=ps[bg][:, 0:TP], func=AF.Sigmoid,
                                                       bias=bconv[:, 8 + cc:9 + cc]))
                        bank_rel[bg] = t_s
                        C.need(dve, t_s)
                        C.need(dve, t_v)
                        t_u = C.done(dve, V.scalar_tensor_tensor(out=uT[:, cc, tp * TP:(tp + 1) * TP], in0=ps[bv][:, 0:TP],
                                                                 scalar=bconv[:, cc:cc + 1], in1=gs_[:, 0:TP],
                                                                 op0=ALU.add, op1=ALU.mult))
                        bank_rel[bv] = t_u
                        bank_rel[("gsig", tp % 2)] = t_u
                wrel[g] = C.done(pe, T.matmul(ps[bv][:, 0:1], lhsT=W[:, 0, 0:128], rhs=hT[:, 0, 0:1], start=True, stop=True)) if False else t_v
                if g + 2 < 7:
                    issue_w(g + 2, win_d[g + 2])

            if debug == "u":
                o = dbg_out("uT", [128, 8 * 1056])
                C.need(sp, t_u)
                t = C.dma(sp, o, RB, C.newsem())
                C.need(sp, t)
                return

            def rope(dst_b, src_f, nh, tile):
                for a in range(2):
                    Xa = src_f.rearrange("p (h a f) -> p h a f", a=2, f=64)[:, :, a, :]
                    Oa = dst_b.rearrange("p (h a f) -> p h a f", a=2, f=64)[:, :, a, :]
                    Aa = ropeA[:, 0:nh * 64].rearrange("p (h f) -> p h f", f=64)
                    Ba = ropeB[:, 0:nh * 64].rearrange("p (h f) -> p h f", f=64)
                    cs = rope_t[:, tile * 128 + a * 32:tile * 128 + a * 32 + 32]
                    sn = rope_t[:, tile * 128 + 64 + a * 32:tile * 128 + 64 + a * 32 + 32]
                    for hf in range(2):
                        V.tensor_tensor(out=Aa[:, :, hf * 32:(hf + 1) * 32], in0=Xa[:, :, hf * 32:(hf + 1) * 32],
                                        in1=cs.unsqueeze(1).to_broadcast([128, nh, 32]), op=ALU.mult)
                        V.tensor_tensor(out=Ba[:, :, hf * 32:(hf + 1) * 32], in0=Xa[:, :, (1 - hf) * 32:(2 - hf) * 32],
                                        in1=sn.unsqueeze(1).to_broadcast([128, nh, 32]), op=ALU.mult)
                    V.tensor_tensor(out=Oa[:, :, 0:32], in0=Aa[:, :, 0:32], in1=Ba[:, :, 0:32], op=ALU.subtract)
                    last = V.tensor_tensor(out=Oa[:, :, 32:64], in0=Aa[:, :, 32:64], in1=Ba[:, :, 32:64], op=ALU.add)
                return last

            qrb_rel = None
            trb_rel = None
            t_last = None
            for g in (4, 5, 6):
                C.need(pe, wtok[g])
                W = wslot[g % 2]
                tiles = list(range(1, 9)) if g < 6 else list(range(NT + 2))
                mmtok = {}

                def emit_mm(ti):
                    tl = tiles[ti]
                    isctx = tl >= NT
                    lh = hcT[:, :, (tl - NT) * 128:(tl - NT + 1) * 128] if isctx else hT[:, :, tl * 128:(tl + 1) * 128]
                    b = 4 + (ti % 2)
                    getbank(b, pe)
                    for kc in range(KC):
                        mm = T.matmul(ps[b], lhsT=lh[:, kc, :], rhs=W[:, kc, :], start=(kc == 0), stop=(kc == KC - 1))
                    mmtok[ti] = C.done(pe, mm)

                emit_mm(0)
                for ti, tl in enumerate(tiles):
                    if ti + 1 < len(tiles):
                        emit_mm(ti + 1)
                    isctx = tl >= NT
                    b = 4 + (ti % 2)
                    t_mm = mmtok[ti]
                    C.need(dve, t_mm)
                    boff = (g - 4) * 512
                    C.need(dve, qrb_rel)
                    if g < 6:
                        V.tensor_tensor(out=qtok, in0=ps[b], in1=bqkv[:, boff:boff + 512], op=ALU.add)
                        t_r = C.done(dve, rope(qrb, qtok, 4, tl))
                        bank_rel[b] = t_r
                        ntr = 4
                    else:
                        V.tensor_tensor(out=qtok, in0=ps[b], in1=bqkv[:, boff:boff + 512], op=ALU.add)
                        vdst = vctok[:, tl - NT, :] if isctx else vtok[:, tl, :]
                        if isctx:
                            V.tensor_copy(out=qrb[:, 0:256], in_=qtok[:, 0:256])
                            t_r = C.done(dve, V.tensor_copy(out=vdst, in_=qtok[:, 256:512]))
                        else:
                            V.tensor_copy(out=vdst, in_=qtok[:, 256:512])
                            t_r = C.done(dve, rope(qrb[:, 0:256], qtok[:, 0:256], 2, tl))
                        bank_rel[b] = t_r
                        ntr = 2
                    C.need(pe, t_r)
                    C.need(pe, trb_rel)
                    C.need(pe, tr_rel[0])
                    pst = psb[0]
                    for hh in range(ntr):
                        mm = T.transpose(pst[:, hh * 128:(hh + 1) * 128], qrb[:, hh * 128:(hh + 1) * 128], ident_b)
                    t_tr = C.done(pe, mm)
                    qrb_rel = t_tr
                    C.need(act, t_tr)
                    if g < 6:
                        o_ = qT[:, (g - 4) * 4:(g - 4) * 4 + 4, (tl - 1) * 128:tl * 128]
                    elif isctx:
                        o_ = kcT[:, :, (tl - NT) * 128:(tl - NT + 1) * 128]
                    else:
                        o_ = kT[:, :, tl * 128:(tl + 1) * 128]
                    t_last = C.done(act, A.activation(out=o_, in_=pst[:, 0:ntr * 128].rearrange("p (h t) -> p h t", t=128),
                                                      func=AF.Copy))
                    trb_rel = t_last
                t_mm = mmtok[len(tiles) - 1]
                wrel[g] = t_tr
                if g + 2 < 7:
                    issue_w(g + 2, win_d[g + 2])

            if debug == "qkv":
                o1 = dbg_out("qT", [128, 8 * 1024], BF16)
                o2 = dbg_out("kT", [128, 2 * 1280], BF16)
                o3 = dbg_out("vtok", [128, NT * 256], BF16)
                o4 = dbg_out("kcT", [128, 512], BF16)
                C.need(sp, t_last)
                C.need(sp, t_r)
                ds_ = C.newsem()
                C.dma(sp, o1, qT.rearrange("p h t -> p (h t)"), ds_)
                C.dma(sp, o2, kT.rearrange("p h t -> p (h t)"), ds_)
                C.dma(sp, o3, vtok.rearrange("p t c -> p (t c)"), ds_)
                t = C.dma(sp, o4, kcT.rearrange("p h t -> p (h t)"), ds_)
                C.need(sp, t)
                return
            cv = RA[:, 0:8192].rearrange("p (c t) -> p c t", c=8)
            aT = RA[:, 8192:16384].bitcast(BF16).rearrange("p (k t) -> p k t", k=KC)
            C.need(dve, [t_last, t_mm, t_r])
            C.need(pool, [t_last, t_mm, t_r])
            V.tensor_scalar(out=uT[:, :, 0:16], in0=uT[:, :, 0:16], scalar1=hval[:, 0:1], scalar2=None, op0=ALU.mult)
            t_hm = C.done(dve, V.tensor_scalar(out=uT[:, :, 1040:1056], in0=uT[:, :, 1040:1056],
                                               scalar1=hval[:, 1:2], scalar2=None, op0=ALU.mult))
            C.need(pool, t_hm)
            t_cv = []
            ptmp = RF[:, 3584:4608]

            def taps_dve(cc):
                V.tensor_scalar(out=cv[:, cc, :], in0=uT[:, cc, 1:1025], scalar1=wdw[:, cc, 0:1],
                                scalar2=bdw[:, cc:cc + 1], op0=ALU.mult, op1=ALU.add)
                for k in range(1, 31):
                    ins = V.scalar_tensor_tensor(out=cv[:, cc, :], in0=uT[:, cc, k + 1:k + 1025],
                                                 scalar=wdw[:, cc, k:k + 1], in1=cv[:, cc, :],
                                                 op0=ALU.mult, op1=ALU.add)
                t_cv.append(C.done(dve, ins))

            def taps_pool(cc):
                G.tensor_scalar(out=cv[:, cc, :], in0=uT[:, cc, 1:1025], scalar1=wdw[:, cc, 0:1],
                                scalar2=bdw[:, cc:cc + 1], op0=ALU.mult, op1=ALU.add)
                for k in range(1, 31):
                    G.tensor_scalar(out=ptmp, in0=uT[:, cc, k + 1:k + 1025], scalar1=wdw[:, cc, k:k + 1],
                                    scalar2=None, op0=ALU.mult)
                    ins = G.tensor_tensor(out=cv[:, cc, :], in0=cv[:, cc, :], in1=ptmp, op=ALU.add)
                t_cv.append(C.done(pool, ins))


            V.tensor_copy(out=masks[:, 0:256], in_=masks_f)
            V.tensor_scalar(out=masks[:, 256:384], in0=masks_f[:, 0:128], scalar1=hval[:, 0:1], scalar2=None, op0=ALU.mult)
            t_mk = C.done(dve, V.tensor_scalar(out=masks[:, 384:512], in0=masks_f[:, 128:256], scalar1=hval[:, 1:2],
                                               scalar2=None, op0=ALU.mult))
            t_sk = C.done(act, A.activation(out=sinke, in_=sinke, func=AF.Exp))
            pbuf = [RC[:, i * 256:(i + 1) * 256].bitcast(BF16) for i in range(10)]
            dtmp = RF[:, 3072:3584]
            SCALE = 128.0 ** -0.5
            C.need(pe, [t_mk, t_sk])
            s_rel = [None, None]
            p_rel = [None] * 10
            od_rel = [None, None]
            itn = 0
            scnt = 0
            t_f = None
            t_pv = None
            next_tap = 0

            def hq(ap):
                return ap.rearrange("p (h q) -> p h q", h=4)

            for t in range(1, 9):
                for kv in range(2):
                    par = itn % 2
                    tiles = [("c", 0), ("c", 1), ("w", t - 1), ("w", t), ("w", t + 1)]
                    ptoks = []
                    for i, (kind, idx) in enumerate(tiles):
                        keyT = kcT[:, kv, idx * 128:(idx + 1) * 128] if kind == "c" else kT[:, kv, idx * 128:(idx + 1) * 128]
                        sbk = scnt % 2
                        scnt += 1
                        C.need(pe, s_rel[sbk])
                        t_s = C.done(pe, T.matmul(ps[sbk], lhsT=keyT, rhs=qT[:, 4 * kv:4 * kv + 4, (t - 1) * 128:t * 128],
                                                  start=True, stop=True))
                        pb = pbuf[par * 5 + i]
                        C.need(act, t_s)
                        C.need(act, p_rel[par * 5 + i])
                        t_e = C.done(act, A.activation(out=pb, in_=ps[sbk], func=AF.Exp, scale=SCALE))
                        s_rel[sbk] = t_e
                        if kind == "w" and idx != t:
                            if idx == t - 1:
                                mk = masks[:, 256:384] if t == 1 else masks[:, 0:128]
                            else:
                                mk = masks[:, 384:512] if t == 8 else masks[:, 128:256]
                            C.need(dve, t_e)
                            t_e = C.done(dve, V.tensor_tensor(out=hq(pb), in0=hq(pb),
                                                              in1=mk.unsqueeze(1).to_broadcast([128, 4, 128]), op=ALU.mult))
                        ptoks.append(t_e)
                    C.need(pe, od_rel[par])
                    for i, (kind, idx) in enumerate(tiles):
                        vt = vctok[:, idx, kv * 128:(kv + 1) * 128] if kind == "c" else vtok[:, idx, kv * 128:(kv + 1) * 128]
                        C.need(pe, ptoks[i])
                        T.matmul(ps[2 + par], lhsT=vt, rhs=pbuf[par * 5 + i], start=(i == 0), stop=(i == 4))
                    for i in range(5):
                        mm = T.matmul(ps[4 + par], lhsT=ones_b, rhs=pbuf[par * 5 + i], start=(i == 0), stop=(i == 4))
                    t_pv = C.done(pe, mm)
                    for i in range(5):
                        p_rel[par * 5 + i] = t_pv
                    C.need(dve, t_pv)
                    C.need(dve, t_sk)
                    V.tensor_tensor(out=hq(dtmp), in0=hq(ps[4 + par]),
                                    in1=sinke[:, 4 * kv:4 * kv + 4].unsqueeze(2).to_broadcast([128, 4, 128]), op=ALU.add)
                    V.reciprocal(out=dtmp, in_=dtmp)
                    t_f = C.done(dve, V.tensor_tensor(out=aT[:, 8 + 4 * kv:8 + 4 * kv + 4, (t - 1) * 128:t * 128],
                                                      in0=hq(ps[2 + par]), in1=hq(dtmp), op=ALU.mult))
                    od_rel[par] = t_f
                    itn += 1
                    if itn % 2 == 0 and next_tap < 8:
                        taps_dve(next_tap)
                        next_tap += 1
            while next_tap < 8:
                taps_dve(next_tap)
                next_tap += 1

            if debug == "attn":
                o = dbg_out("attn", [128, 8 * 1024], BF16)
                C.need(sp, t_f)
                t = C.dma(sp, o, aT[:, 8:16, :].rearrange("p k t -> p (k t)"), C.newsem())
                C.need(sp, t)
                return

            sq = RB[:, 0:8192].rearrange("p (c t) -> p c t", c=8)
            C.need(act, t_cv)
            for cc in range(8):
                ins = A.activation(out=sq[:, cc, :], in_=cv[:, cc, :], func=AF.Square)
            t_sq = C.done(act, ins)
            C.need(pe, t_sq)
            C.need(pe, t_cv)
            C.need(pe, t_id)
            C.need(pe, [t_f] + s_rel)
            for th in range(2):
                for cc in range(8):
                    T.matmul(ps[th], lhsT=ones_f, rhs=cv[:, cc, th * 512:(th + 1) * 512], start=(cc == 0), stop=(cc == 7))
                for cc in range(8):
                    mm = T.matmul(ps[2 + th], lhsT=ones_f, rhs=sq[:, cc, th * 512:(th + 1) * 512], start=(cc == 0), stop=(cc == 7))
            t_stat = C.done(pe, mm)
            cmean = RF[:, 0:1024]
            crstd = RF[:, 1024:2048]
            ctmp = RF[:, 2048:3072]
            C.need(dve, t_stat)
            C.need(dve, t_cv)
            for th in range(2):
                sl = slice(th * 512, (th + 1) * 512)
                V.tensor_scalar(out=cmean[:, sl], in0=ps[th], scalar1=1.0 / 1024, scalar2=None, op0=ALU.mult)
                V.tensor_tensor(out=ctmp[:, sl], in0=cmean[:, sl], in1=cmean[:, sl], op=ALU.mult)
                V.scalar_tensor_tensor(out=ctmp[:, sl], in0=ps[2 + th], scalar=1.0 / 1024, in1=ctmp[:, sl],
                                       op0=ALU.mult, op1=ALU.subtract)
                ins = V.tensor_scalar(out=ctmp[:, sl], in0=ctmp[:, sl], scalar1=EPS, scalar2=None, op0=ALU.add)
            t_var = C.done(dve, ins)
            C.need(act, t_var)
            t_sd = C.done(act, A.sqrt(out=ctmp, in_=ctmp))
            C.need(dve, t_sd)
            V.reciprocal(out=crstd, in_=ctmp)
            t_ac = None
            for cc in range(8):
                V.tensor_tensor(out=cv[:, cc, :], in0=cv[:, cc, :], in1=cmean, op=ALU.subtract)
                t_z = C.done(dve, V.tensor_tensor(out=cv[:, cc, :], in0=cv[:, cc, :], in1=crstd, op=ALU.mult))
                C.need(act, t_z)
                t_ac = C.done(act, A.activation(out=aT[:, cc, :], in_=cv[:, cc, :], func=AF.Silu,
                                                bias=clb[:, cc:cc + 1], scale=clg[:, cc:cc + 1]))

            if debug == "aconv":
                o = dbg_out("aconv", [128, 8 * 1024], BF16)
                C.need(sp, t_ac)
                t = C.dma(sp, o, aT[:, 0:8, :].rearrange("p k t -> p (k t)"), C.newsem())
                C.need(sp, t)
                return

            rowsA = RB[:, 0:8192].rearrange("p (r d) -> p r d", r=4)
            rowsB = RE[:, 0:4096].rearrange("p (r d) -> p r d", r=2)
            C.need(sp, [t_stat, t_pv, t_f])
            rsem = C.newsem()
            C.dma(sp, rowsA[:, 0:3, :], rows_d[0:3].rearrange("r p d -> p r d"), rsem)
            C.dma(sp, rowsA[:, 3, :], mrows_d[0], rsem)
            t_rows = C.dma(sp, rowsB, mrows_d[1:3].rearrange("r p d -> p r d"), rsem)
            C.need(dve, t_rows)
            t_rows2 = C.done(dve, V.tensor_scalar(out=rowsB[:, 0, :], in0=rowsB[:, 0, :], scalar1=1.0, scalar2=None, op0=ALU.add))
            wosem = [C.newsem(), C.newsem()]
            wotok = {}
            worel = {}

            def issue_wo(g):
                slot = g % 2
                if g - 2 in worel:
                    C.need(pool, worel[g - 2])
                for q in range(4):
                    tk = C.dma(pool, wslot[slot].rearrange("p k c -> p (k c)")[:, q * 2048:(q + 1) * 2048],
                               wout_d[g][:, q * 2048:(q + 1) * 2048], wosem[slot])
                wotok[g] = tk

            C.need(pool, t_pv)
            issue_wo(0)
            issue_wo(1)
            C.need(pe, [t_f, t_ac, t_var])
            C.need(pe, s_rel)
            ev = [RF[:, 3584:4096], RF[:, 4096:4608]]
            ev_rel = [None, None]
            mx_sem = [C.newsem(), C.newsem()]
            bk_rel = [None, None]
            cnt = 0
            for g in range(4):
                C.need(pe, wotok[g])
                W = wslot[g % 2]
                for t in range(8):
                    b = cnt % 2
                    cnt += 1
                    C.need(pe, bk_rel[b])
                    for fc in range(KC):
                        mm = T.matmul(ps[b], lhsT=aT[:, fc, t * 128:(t + 1) * 128], rhs=W[:, fc, :],
                                      start=(fc == 0), stop=(fc == KC - 1))
                    t_mm = C.done(pe, mm)
                    C.need(dve, t_mm)
                    C.need(dve, ev_rel[b])
                    t_c = C.done(dve, V.tensor_tensor(out=ev[b], in0=ps[b], in1=rowsA[:, 0, g * 512:(g + 1) * 512], op=ALU.add))
                    bk_rel[b] = t_c
                    C.need(sp, t_c)
                    ev_rel[b] = C.dma(sp, mixd.ap()[t * 128:(t + 1) * 128, g * 512:(g + 1) * 512], ev[b], mx_sem[b])
                worel[g] = t_mm
                if g + 2 < 4:
                    issue_wo(g + 2)

            xmid_o = dout("x_mid", [1024, D])
            h2_o = dout("h2b", [1024, D], BF16)
            aff_o = dout("aff", [1024, 16])
            C.need(sp, ev_rel)
            C.need(sp, [t_mm, t_ac])
            xt = RA[:, 0:2048]
            mt = RA[:, 2048:4096]
            xmb = [RA[:, 4096:6144], RA[:, 6144:8192]]
            h2buf = [RA[:, 8192:10240], RA[:, 14336:16384]]
            h2b = RA[:, 10240:11264].bitcast(BF16)
            h2T = RA[:, 12288:14336].rearrange("p (k t) -> p k t", k=KC)
            lg = small[:, 16:32]
            lsem = C.newsem()
            osem0 = [C.newsem(), C.newsem()]
            osem1 = C.newsem()
            osem2 = C.newsem()
            st8 = {"rel_xt": None, "o0": [None, None], "o1": None, "o2": None, "tp": [None, None], "h2": [None, None],
                   "sub": None}

            def ln_stats(src, k):
                st_ = stats[:, 0:24]
                for c4 in range(4):
                    V.bn_stats(out=st_[:, c4 * 6:(c4 + 1) * 6], in_=src[:, c4 * 512:(c4 + 1) * 512])
                mv_ = small[:, 0:2]
                V.bn_aggr(out=mv_, in_=st_)
                tv = C.done(dve, V.tensor_scalar(out=small[:, 8:9], in0=mv_[:, 1:2], scalar1=EPS, scalar2=None, op0=ALU.add))
                C.need(act, tv)
                ts = C.done(act, A.sqrt(out=small[:, 9:10], in_=small[:, 8:9]))
                C.need(dve, ts)
                rs_ = small[:, 10 + 2 * k:11 + 2 * k]
                nm_ = small[:, 11 + 2 * k:12 + 2 * k]
                V.reciprocal(out=rs_, in_=small[:, 9:10])
                tk = C.done(dve, V.scalar_tensor_tensor(out=nm_, in0=mv_[:, 0:1], scalar=-1.0, in1=rs_,
                                                        op0=ALU.mult, op1=ALU.mult))
                return rs_, nm_, tk

            def H1(t):
                xm = xmb[t % 2]
                C.need(sp, st8["rel_xt"])
                C.dma(sp, xt, xh[(t + 1) * 128:(t + 2) * 128, :], lsem)
                t_l = C.dma(sp, mt, mixd.ap()[t * 128:(t + 1) * 128, :], lsem)
                C.need(dve, t_l)
                C.need(dve, t_rows2)
                V.tensor_tensor(out=mt, in0=mt, in1=rowsA[:, 3, :], op=ALU.mult)
                V.scalar_tensor_tensor(out=mt, in0=xt, scalar=ALPHA, in1=mt, op0=ALU.mult, op1=ALU.add)
                rs_, nm_, tk = ln_stats(mt, 0)
                C.need(act, tk)
                C.need(act, st8["o0"][t % 2])
                t_a = C.done(act, A.activation(out=xm, in_=mt, func=AF.Identity, bias=nm_, scale=rs_))
                st8["rel_xt"] = t_a
                C.need(dve, t_a)
                V.tensor_tensor(out=xm, in0=xm, in1=rowsA[:, 1, :], op=ALU.mult)
                t_xm = C.done(dve, V.tensor_tensor(out=xm, in0=xm, in1=rowsA[:, 2, :], op=ALU.add))
                C.need(sp, t_xm)
                st8["o0"][t % 2] = C.dma(sp, xmid_o[t * 128:(t + 1) * 128, :], xm, osem0[t % 2])

            def H2a(t):
                xm = xmb[t % 2]
                h2 = h2buf[t % 2]
                rs_, nm_, tk = ln_stats(xm, 1)
                C.need(act, tk)
                C.need(act, st8["tp"][t % 2])
                t_a = C.done(act, A.activation(out=h2, in_=xm, func=AF.Identity, bias=nm_, scale=rs_))
                C.need(dve, t_a)
                V.tensor_tensor(out=h2, in0=h2, in1=rowsB[:, 0, :], op=ALU.mult)
                t_h2 = C.done(dve, V.tensor_tensor(out=h2, in0=h2, in1=rowsB[:, 1, :], op=ALU.add))
                st8["h2"][t % 2] = t_h2
                C.need(act, t_h2)
                C.need(act, st8["o1"])
                t_hb = C.done(act, A.activation(out=h2b, in_=h2, func=AF.Copy))
                C.need(sp, t_hb)
                st8["o1"] = C.dma(sp, h2_o[t * 128:(t + 1) * 128, :], h2b, osem1)

            def H3(t):
                h2 = h2buf[t % 2]
                C.need(pe, st8["h2"][t % 2])
                C.need(pe, st8["sub"])
                for kc in range(KC):
                    mm = T.transpose(ps[kc // 4][:, (kc % 4) * 128:(kc % 4 + 1) * 128], h2[:, kc * 128:(kc + 1) * 128], ident_f)
                t_tp = C.done(pe, mm)
                st8["tp"][t % 2] = t_tp
                C.need(act, t_tp)
                for q in range(4):
                    ins = A.activation(out=h2T[:, 4 * q:4 * q + 4, :], in_=ps[q].rearrange("p (k t) -> p k t", k=4), func=AF.Copy)
                t_cp = C.done(act, ins)
                C.need(pe, t_cp)
                for kc in range(KC):
                    mm = T.matmul(ps[4][:, 0:16], lhsT=h2T[:, kc, :], rhs=wr[:, kc * 16:(kc + 1) * 16],
                                  start=(kc == 0), stop=(kc == KC - 1))
                t_lg = C.done(pe, mm)
                C.need(dve, t_lg)
                C.need(dve, st8["o2"])
                V.tensor_reduce(out=small[:, 32:33], in_=ps[4][:, 0:16], axis=AX.X, op=ALU.max)
                t_sub = C.done(dve, V.tensor_scalar(out=lg, in0=ps[4][:, 0:16], scalar1=small[:, 32:33], scalar2=None,
                                                    op0=ALU.subtract))
                st8["sub"] = t_sub
                C.need(act, t_sub)
                t_ex = C.done(act, A.activation(out=lg, in_=lg, func=AF.Exp))
                C.need(dve, t_ex)
                V.tensor_reduce(out=small[:, 33:34], in_=lg, axis=AX.X, op=ALU.add)
                V.reciprocal(out=small[:, 34:35], in_=small[:, 33:34])
                t_af = C.done(dve, V.tensor_scalar(out=lg, in0=lg, scalar1=small[:, 34:35], scalar2=None, op0=ALU.mult))
                C.need(sp, t_af)
                st8["o2"] = C.dma(sp, aff_o[t * 128:(t + 1) * 128, :], lg, osem2)

            H1(0)
            for t in range(8):
                if t + 1 < 8:
                    H1(t + 1)
                H2a(t)
                if t >= 1:
                    H3(t - 1)
            H3(7)
            C.need(sp, st8["o0"])
            C.need(sp, [st8["o1"], st8["o2"]])
    return nc, dbg


def build_mod():
    nc = bass.Bass("TRN2", target_bir_lowering=False)
    cT_d = nc.dram_tensor("cT", [128, KC, 2], F32, kind="ExternalInput").ap()
    wmod_d = nc.dram_tensor("wmod", [3, 128, KC * 512], F32, kind="ExternalInput").ap()
    bmod_d = nc.dram_tensor("bmod", [2, 1536], F32, kind="ExternalInput").ap()
    o = nc.dram_tensor("modsl", [2, 1536], F32, kind="ExternalOutput").ap()
    cT = nc.alloc_sbuf_tensor("cT_sb", [128, KC * 2], F32)[:]
    scT = nc.alloc_sbuf_tensor("scT_sb", [128, KC * 2], F32)[:]
    bmod_sb = nc.alloc_sbuf_tensor("bm_sb", [2, 1536], F32)[:]
    modsl = nc.alloc_sbuf_tensor("modsl_sb", [2, 1536], F32)[:]
    RC = nc.alloc_sbuf_tensor("wm_sb", [128, KC * 512], F32)[:]
    wmod_sb = RC.rearrange("p (k c) -> p k c", k=KC)
    ps0 = nc.alloc_psum_tensor("psm", [128, 512], F32)[:]
    C = Ctx(nc)
    pe, act, dve, pool, sp = C.pe, C.act, C.dve, C.pool, C.sp
    T = nc.tensor
    V, A = Ser(dve), Ser(act)
    with nc.Block() as block:
        @block.sync
        def _(sync_engine):
            ld0 = C.newsem()
            C.dma(sp, cT, cT_d.rearrange("p k j -> p (k j)"), ld0)
            t_ld0 = C.dma(sp, bmod_sb, bmod_d, ld0)
            C.need(act, t_ld0)
            t_sc = C.done(act, A.activation(out=scT, in_=cT, func=AF.Silu))
            wm_sem = C.newsem()
            t_ev = None
            for cc in range(3):
                C.need(sp, t_ev)
                t_w = C.dma(sp, RC, wmod_d[cc], wm_sem)
                C.need(pe, t_w)
                C.need(pe, t_sc)
                for kc in range(KC):
                    mm = T.matmul(ps0[0:2, :], lhsT=scT.rearrange("p (k j) -> p k j", j=2)[:, kc, :],
                                  rhs=wmod_sb[:, kc, :], start=(kc == 0), stop=(kc == KC - 1))
                t_mm = C.done(pe, mm)
                C.need(dve, t_mm)
                C.need(dve, t_ld0)
                t_ev = C.done(dve, V.tensor_tensor(out=modsl[:, cc * 512:(cc + 1) * 512], in0=ps0[0:2, :],
                                                   in1=bmod_sb[:, cc * 512:(cc + 1) * 512], op=ALU.add))
            C.need(sp, t_ev)
            t = C.dma(sp, o, modsl, C.newsem())
            C.need(sp, t)
    return nc


def host_mod_inputs(inp):
    f = np.float32
    w_mod = np.asarray(inp["w_mod"], f)[0]
    b_mod = np.asarray(inp["b_mod"], f)[0]
    cvec = np.stack([np.asarray(inp["c"], f)[0], np.asarray(inp["c_ctx"], f)], 0)
    cT = np.ascontiguousarray(cvec.reshape(2, KC, 128).transpose(2, 1, 0))
    maps = []
    for i in range(NCORES):
        m = {"cT": cT}
        m["wmod"] = np.ascontiguousarray(
            w_mod[:, i * 1536:(i + 1) * 1536].reshape(KC, 128, 3, 512).transpose(2, 1, 0, 3)).reshape(3, 128, KC * 512)
        m["bmod"] = np.ascontiguousarray(np.broadcast_to(b_mod[i * 1536:(i + 1) * 1536][None], (2, 1536)))
        maps.append(m)
    return maps


def run_mod(inp):
    res = run_bass_kernel_spmd(build_mod(), host_mod_inputs(inp), core_ids=list(range(NCORES)))
    sl = np.stack([np.asarray(r["modsl"], np.float32) for r in res.results], 0)
    mod = np.ascontiguousarray(sl[:, 0, :]).reshape(6, D)
    mod_c = np.ascontiguousarray(sl[:, 1, :]).reshape(6, D)
    return mod, mod_c


def host_inputs(inp, mod, mod_c):
    f = np.float32
    x = np.asarray(inp["x"], f)[0]
    w_in = np.asarray(inp["w_in"], f)[0]
    b_in = np.asarray(inp["b_in"], f)[0]
    xpad = np.zeros((8192 + 256, D), f)
    xpad[128:128 + 8192] = x
    cols = []
    for g in range(4):
        cols.append(np.concatenate([np.arange(256 * g, 256 * g + 256), np.arange(1024 + 256 * g, 1024 + 256 * g + 256)]))
    cols.append(np.arange(2048, 2560))
    cols.append(np.arange(2560, 3072))
    cols.append(np.arange(3072, 3584))
    win = np.stack([np.ascontiguousarray(w_in[:, c].reshape(KC, 128, 512).transpose(1, 0, 2)).reshape(128, KC * 512)
                    for c in cols], 0)
    pc = np.zeros((128, 16 + 248 + 24 + 2), f)
    pc[:, 0:16] = b_in[:2048].reshape(16, 128).T
    pc[:, 16:264] = np.asarray(inp["w_dw"], f)[0].T.reshape(8, 128, 31).transpose(1, 0, 2).reshape(128, 248)
    pc[:, 264:272] = np.asarray(inp["b_dw"], f)[0].reshape(8, 128).T
    pc[:, 272:280] = np.asarray(inp["conv_ln_g"], f)[0].reshape(8, 128).T
    pc[:, 280:288] = np.asarray(inp["conv_ln_b"], f)[0].reshape(8, 128).T
    bqkv = np.ascontiguousarray(np.broadcast_to(b_in[2048:3584][None], (128, 1536)))
    inv = (10000.0 ** (-np.arange(0, 64, 2, dtype=np.float32) / 64)).astype(f)
    mk = np.zeros((128, 2, 128), f)
    jj = np.arange(128)[:, None]
    ii = np.arange(128)[None, :]
    mk[:, 0, :] = (jj >= ii)
    mk[:, 1, :] = (jj <= ii)
    sink = np.ascontiguousarray(np.broadcast_to(np.asarray(inp["sink"], f)[0][None], (128, 8)))
    w_out = np.asarray(inp["w_out"], f)[0]
    wout = np.stack([np.ascontiguousarray(w_out[:, g * 512:(g + 1) * 512].reshape(KC, 128, 512).transpose(1, 0, 2)).reshape(128, KC * 512)
                     for g in range(4)], 0)
    rows = np.stack([np.broadcast_to(np.asarray(inp[k], f)[0][None], (128, D))
                     for k in ("b_out", "ln1_g", "ln1_b", "ln2_g", "ln2_b")], 0)
    rows = np.ascontiguousarray(rows)
    wr = np.ascontiguousarray(np.asarray(inp["w_router"], f)[0].reshape(KC, 128, 16).transpose(1, 0, 2)).reshape(128, KC * 16)
    ctx = np.asarray(inp["ctx"], f)[0]
    modT = np.ascontiguousarray(np.concatenate([mod.reshape(96, 128).T, mod_c.reshape(96, 128).T], 1))
    mrows = np.ascontiguousarray(np.stack([np.broadcast_to(mod[k][None], (128, D)) for k in (2, 4, 3)], 0))
    maps = []
    for i in range(NCORES):
        m = {}
        m["xh"] = np.ascontiguousarray(xpad[1024 * i:1024 * i + TOK])
        m["ctx"] = ctx
        m["win"] = win
        p = pc.copy()
        p[:, 288] = 0.0 if i == 0 else 1.0
        p[:, 289] = 0.0 if i == NCORES - 1 else 1.0
        m["pcols"] = p
        m["bqkv"] = bqkv
        t = np.arange(1024 * i - 128, 1024 * i - 128 + TOK)
        ar = (t // 64).astype(f)[:, None] * inv[None]
        ac = (t % 64).astype(f)[:, None] * inv[None]
        tab = np.concatenate([np.cos(ar), np.cos(ac), np.sin(ar), np.sin(ac)], 1).astype(f)
        m["rope"] = np.ascontiguousarray(tab.reshape(NT, 128, 128).transpose(1, 0, 2))
        m["masks"] = mk
        m["sink"] = sink
        m["wout"] = wout
        m["rows"] = rows
        m["wr"] = wr
        m["modT"] = modT
        m["mrows"] = mrows
        maps.append(m)
    return maps


def _mk(nc):
    C = Ctx(nc)
    return C, nc.tensor, Ser(C.dve), Ser(C.act), Ser(C.pool)


def build_route():
    nc = bass.Bass("TRN2", target_bir_lowering=False)
    aff_d = nc.dram_tensor("aff", [8192, 16], F32, kind="ExternalInput").ap()
    mask_o = nc.dram_tensor("mask", [8192, 16], F32, kind="ExternalOutput").ap()

    def sb(name, cols, dt=F32):
        return nc.alloc_sbuf_tensor("r_" + name, [128, cols], dt)[:]
    aff = sb("aff", 1024)
    cmp_ = sb("cmp", 1024)
    ones = sb("ones", 128)
    lo, hi, mid, cntp, ge, dd = [sb(n, 16) for n in ("lo", "hi", "mid", "cntp", "ge", "dd")]
    ps = nc.alloc_psum_tensor("r_ps", [128, 512], F32)[:]
    C, T, V, A, G = _mk(nc)
    pe, dve, pool, sp = C.pe, C.dve, C.pool, C.sp
    aff3 = aff.rearrange("p (c e) -> p c e", e=16)
    cmp3 = cmp_.rearrange("p (c e) -> p c e", e=16)
    with nc.Block() as block:
        @block.sync
        def _(se):
            t_l = C.dma(sp, aff, aff_d.rearrange("(p c) e -> p (c e)", p=128), C.newsem())
            t_o = C.done(pool, G.memset(ones, 1.0))
            V.memset(lo, 0.0)
            V.memset(hi, 1.0)
            C.need(dve, t_l)
            C.need(pe, t_o)
            for it in range(34):
                V.tensor_tensor(out=mid, in0=lo, in1=hi, op=ALU.add)
                V.tensor_scalar(out=mid, in0=mid, scalar1=0.5, scalar2=None, op0=ALU.mult)
                V.tensor_tensor(out=cmp3, in0=aff3, in1=mid.unsqueeze(1).to_broadcast([128, 64, 16]), op=ALU.is_ge)
                t_c = C.done(dve, V.tensor_reduce(out=cntp, in_=cmp_.rearrange("p (c e) -> p e c", e=16),
                                                  axis=AX.X, op=ALU.add))
                C.need(pe, t_c)
                t_m = C.done(pe, T.matmul(ps[:, 0:16], lhsT=ones, rhs=cntp, start=True, stop=True))
                C.need(dve, t_m)
                V.tensor_scalar(out=ge, in0=ps[:, 0:16], scalar1=float(CAP) - 0.5, scalar2=None, op0=ALU.is_ge)
                V.tensor_tensor(out=dd, in0=mid, in1=lo, op=ALU.subtract)
                V.tensor_tensor(out=dd, in0=dd, in1=ge, op=ALU.mult)
                V.tensor_tensor(out=lo, in0=lo, in1=dd, op=ALU.add)
                V.tensor_tensor(out=dd, in0=hi, in1=mid, op=ALU.subtract)
                V.tensor_tensor(out=dd, in0=dd, in1=ge, op=ALU.mult)
                V.tensor_tensor(out=hi, in0=mid, in1=dd, op=ALU.add)
            t_f = C.done(dve, V.tensor_tensor(out=cmp3, in0=aff3, in1=lo.unsqueeze(1).to_broadcast([128, 64, 16]),
                                              op=ALU.is_ge))
            C.need(sp, t_f)
            t = C.dma(sp, mask_o.rearrange("(p c) e -> p (c e)", p=128), cmp_, C.newsem())
            C.need(sp, t)
    return nc


NFG = 22
NDB = 8


def build_experts():
    nc = bass.Bass("TRN2", target_bir_lowering=False)
    xs_d = nc.dram_tensor("xsT", [2, 128, KC * CAP], BF16, kind="ExternalInput").ap()
    gs_d = nc.dram_tensor("gsl", [128, 16], F32, kind="ExternalInput").ap()
    wg_d = nc.dram_tensor("wg", [2, NFG, 128, KC * 256], F32, kind="ExternalInput").ap()
    wu_d = nc.dram_tensor("wu", [2, NFG, 128, KC * 256], F32, kind="ExternalInput").ap()
    wd_d = nc.dram_tensor("wd", [2, NDB, 128, FC * 256], F32, kind="ExternalInput").ap()
    y_o = nc.dram_tensor("y", [2, CAP, D], F32, kind="ExternalOutput").ap()

    def sb(name, cols, dt=F32):
        return nc.alloc_sbuf_tensor("x_" + name, [128, cols], dt)[:]
    XS = sb("XS", KC * CAP, BF16).rearrange("p (k t) -> p k t", k=KC)
    HM = sb("HM", FC * CAP, BF16).rearrange("p (f t) -> p f t", f=FC)
    WGU = [sb("wgu%d" % i, 2 * KC * 256, BF16) for i in range(2)]
    WD = [sb("wd%d" % i, FC * 256, BF16) for i in range(2)]
    gs = sb("gs", 16)
    sg = [sb("sg%d" % i, 512) for i in range(2)]
    yst = [sb("yst%d" % i, 256) for i in range(2)]
    ps = [nc.alloc_psum_tensor("x_ps%d" % i, [128, 512], F32)[:] for i in range(8)]
    C, T, V, A, G = _mk(nc)
    pe, act, dve, pool, sp = C.pe, C.act, C.dve, C.pool, C.sp
    with nc.Block() as block:
        @block.sync
        def _(se):
            t_gs = C.dma(sp, gs, gs_d, C.newsem())
            xsem = C.newsem()
            gsem = [C.newsem(), C.newsem()]
            dsem = [C.newsem(), C.newsem()]
            ysem = [C.newsem(), C.newsem()]
            g_rel = [None, None]
            d_rel = [None, None]
            bank_rel = [None] * 8
            sg_rel = [None, None]
            y_rel = [None, None]
            gcnt = 0
            dcnt = 0
            ycnt = 0
            bcnt = 0
            t_gu_last = None
            t_hm = None
            for e in range(2):
                C.need(sp, t_gu_last)
                t_xs = C.dma(sp, XS.rearrange("p k t -> p (k t)"), xs_d[e], xsem)
                C.need(pe, t_xs)
                for fg in range(NFG):
                    slot = gcnt % 2
                    gcnt += 1
                    C.need(pool, g_rel[slot])
                    for half in range(2):
                        C.dma(pool, WGU[slot][:, half * 2048:(half + 1) * 2048],
                              wg_d[e, fg][:, half * 2048:(half + 1) * 2048], gsem[slot])
                    for half in range(2):
                        t_w = C.dma(pool, WGU[slot][:, 4096 + half * 2048:4096 + (half + 1) * 2048],
                                    wu_d[e, fg][:, half * 2048:(half + 1) * 2048], gsem[slot])
                    C.need(pe, t_w)
                    Wg = WGU[slot][:, 0:4096].rearrange("p (k c) -> p k c", k=KC)
                    Wu = WGU[slot][:, 4096:8192].rearrange("p (k c) -> p k c", k=KC)
                    for fj in range(2):
                        fc = fg * 2 + fj
                        base = (fc % 2) * 4
                        for th in range(2):
                            bg, bu = base + th, base + 2 + th
                            C.need(pe, bank_rel[bg])
                            for kc in range(KC):
                                mm = T.matmul(ps[bg], lhsT=Wg[:, kc, fj * 128:(fj + 1) * 128],
                                              rhs=XS[:, kc, th * 512:(th + 1) * 512], start=(kc == 0), stop=(kc == KC - 1))
                            t_g = C.done(pe, mm)
                            C.need(pe, bank_rel[bu])
                            for kc in range(KC):
                                mm = T.matmul(ps[bu], lhsT=Wu[:, kc, fj * 128:(fj + 1) * 128],
                                              rhs=XS[:, kc, th * 512:(th + 1) * 512], start=(kc == 0), stop=(kc == KC - 1))
                            t_u = C.done(pe, mm)
                            C.need(act, t_g)
                            C.need(act, sg_rel[th])
                            t_s = C.done(act, A.activation(out=sg[th], in_=ps[bg], func=AF.Silu))
                            bank_rel[bg] = t_s
                            C.need(dve, [t_s, t_u])
                            C.need(dve, d_rel)
                            t_hm = C.done(dve, V.tensor_tensor(out=HM[:, fc, th * 512:(th + 1) * 512], in0=ps[bu],
                                                               in1=sg[th], op=ALU.mult))
                            bank_rel[bu] = t_hm
                            sg_rel[th] = t_hm
                    g_rel[slot] = t_u
                    t_gu_last = t_u
                C.need(pe, t_hm)
                C.need(dve, t_gs)
                for db in range(NDB):
                    slot = dcnt % 2
                    dcnt += 1
                    C.need(pool, d_rel[slot])
                    for q in range(6):
                        c0 = q * 2048
                        c1 = min(c0 + 2048, FC * 256)
                        t_w = C.dma(pool, WD[slot][:, c0:c1], wd_d[e, db][:, c0:c1], dsem[slot])
                    C.need(pe, t_w)
                    Wd = WD[slot].rearrange("p (f c) -> p f c", f=FC)
                    for tt in range(8):
                        b = bcnt % 4
                        bcnt += 1
                        C.need(pe, bank_rel[b])
                        for fc in range(FC):
                            mm = T.matmul(ps[b][:, 0:256], lhsT=HM[:, fc, tt * 128:(tt + 1) * 128], rhs=Wd[:, fc, :],
                                          start=(fc == 0), stop=(fc == FC - 1))
                        t_d = C.done(pe, mm)
                        yb = ycnt % 2
                        ycnt += 1
                        C.need(dve, t_d)
                        C.need(dve, y_rel[yb])
                        t_y = C.done(dve, V.tensor_scalar(out=yst[yb], in0=ps[b][:, 0:256],
                                                          scalar1=gs[:, e * 8 + tt:e * 8 + tt + 1], scalar2=None, op0=ALU.mult))
                        bank_rel[b] = t_y
                        C.need(sp, t_y)
                        y_rel[yb] = C.dma(sp, y_o[e, tt * 128:(tt + 1) * 128, db * 256:(db + 1) * 256], yst[yb], ysem[yb])
                    d_rel[slot] = t_d
            C.need(sp, y_rel)
    return nc


def build_combine(K):
    nc = bass.Bass("TRN2", target_bir_lowering=False)
    xm_d = nc.dram_tensor("xm", [1024, D], F32, kind="ExternalInput").ap()
    yk_d = nc.dram_tensor("yk", [K, 1024, D], F32, kind="ExternalInput").ap()
    rows_d = nc.dram_tensor("rows", [3, 128, D], F32, kind="ExternalInput").ap()
    out_o = nc.dram_tensor("out", [1024, D], F32, kind="ExternalOutput").ap()

    def sb(name, cols, dt=F32):
        return nc.alloc_sbuf_tensor("c_" + name, [128, cols], dt)[:]
    rows = sb("rows", 3 * D).rearrange("p (r d) -> p r d", r=3)
    xm = sb("xm", D)
    acc = sb("acc", D)
    yb = [sb("yb%d" % i, D) for i in range(4)]
    ot = sb("ot", D)
    stats = sb("stats", 24)
    small = sb("small", 16)
    C, T, V, A, G = _mk(nc)
    act, dve, sp, pool = C.act, C.dve, C.sp, C.pool
    with nc.Block() as block:
        @block.sync
        def _(se):
            t_rows = C.dma(sp, rows, rows_d.rearrange("r p d -> p r d"), C.newsem())
            xsem = C.newsem()
            ysem = [C.newsem() for _ in range(4)]
            osem = C.newsem()
            y_rel = [None] * 4
            x_rel = None
            o_rel = None
            ycnt = 0
            C.need(dve, t_rows)
            for t in range(8):
                C.need(sp, x_rel)
                t_x = C.dma(sp, xm, xm_d[t * 128:(t + 1) * 128, :], xsem)
                for k in range(K):
                    b = ycnt % 4
                    ycnt += 1
                    q_ = sp if b % 2 == 0 else pool
                    C.need(q_, y_rel[b])
                    t_y = C.dma(q_, yb[b], yk_d[k, t * 128:(t + 1) * 128, :], ysem[b])
                    C.need(dve, t_y)
                    if k == 0:
                        ins = V.tensor_copy(out=acc, in_=yb[b])
                    else:
                        ins = V.tensor_tensor(out=acc, in0=acc, in1=yb[b], op=ALU.add)
                    y_rel[b] = C.done(dve, ins)
                C.need(dve, t_x)
                V.tensor_tensor(out=acc, in0=acc, in1=rows[:, 0, :], op=ALU.mult)
                V.scalar_tensor_tensor(out=acc, in0=xm, scalar=ALPHA, in1=acc, op0=ALU.mult, op1=ALU.add)
                for c4 in range(4):
                    V.bn_stats(out=stats[:, c4 * 6:(c4 + 1) * 6], in_=acc[:, c4 * 512:(c4 + 1) * 512])
                V.bn_aggr(out=small[:, 0:2], in_=stats)
                tv = C.done(dve, V.tensor_scalar(out=small[:, 8:9], in0=small[:, 1:2], scalar1=EPS, scalar2=None, op0=ALU.add))
                x_rel = tv
                C.need(act, tv)
                ts = C.done(act, A.sqrt(out=small[:, 9:10], in_=small[:, 8:9]))
                C.need(dve, ts)
                V.reciprocal(out=small[:, 2:3], in_=small[:, 9:10])
                tk = C.done(dve, V.scalar_tensor_tensor(out=small[:, 3:4], in0=small[:, 0:1], scalar=-1.0, in1=small[:, 2:3],
                                                        op0=ALU.mult, op1=ALU.mult))
                C.need(act, tk)
                C.need(act, o_rel)
                ta = C.done(act, A.activation(out=ot, in_=acc, func=AF.Identity, bias=small[:, 3:4], scale=small[:, 2:3]))
                C.need(dve, ta)
                V.tensor_tensor(out=ot, in0=ot, in1=rows[:, 1, :], op=ALU.mult)
                to = C.done(dve, V.tensor_tensor(out=ot, in0=ot, in1=rows[:, 2, :], op=ALU.add))
                C.need(sp, to)
                o_rel = C.dma(sp, out_o[t * 128:(t + 1) * 128, :], ot, osem)
            C.need(sp, o_rel)
    return nc


def _run(nc, maps):
    return run_bass_kernel_spmd(nc, maps, core_ids=list(range(NCORES))).results


def kernel(**inputs):
    f = np.float32
    cores = list(range(NCORES))
    mod, mod_c = run_mod(inputs)
    nc, _ = build()
    res = _run(nc, host_inputs(inputs, mod, mod_c))
    x_mid = np.concatenate([np.asarray(r["x_mid"]) for r in res], 0)
    h2b = np.concatenate([np.asarray(r["h2b"]) for r in res], 0)
    aff = np.ascontiguousarray(np.concatenate([np.asarray(r["aff"], f) for r in res], 0))
    res = _run(build_route(), [{"aff": aff} for _ in cores])
    mask = np.asarray(res[0]["mask"]) > 0.5
    idx = np.zeros((16, CAP), np.int64)
    valid = np.zeros((16, CAP), bool)
    for e in range(16):
        ii = np.nonzero(mask[:, e])[0][:CAP]
        idx[e, :len(ii)] = ii
        valid[e, :len(ii)] = True
    w_gate, w_up, w_down = inputs["w_gate"], inputs["w_up"], inputs["w_down"]
    maps = []
    for i in cores:
        m = {}
        xs = []
        gsl = np.zeros((128, 16), f)
        for j in range(2):
            e = 2 * i + j
            rows = h2b[idx[e]]
            xs.append(np.ascontiguousarray(rows.T.reshape(KC, 128, CAP).transpose(1, 0, 2)).reshape(128, KC * CAP))
            ge = np.where(valid[e], aff[idx[e], e], 0).astype(f)
            gsl[:, j * 8:(j + 1) * 8] = ge.reshape(8, 128).T
        m["xsT"] = np.stack(xs, 0)
        m["gsl"] = gsl
        for nm, w in (("wg", w_gate), ("wu", w_up)):
            m[nm] = np.stack([np.ascontiguousarray(
                np.asarray(w[0, 2 * i + j], f).reshape(KC, 128, NFG, 256).transpose(2, 1, 0, 3)).reshape(NFG, 128, KC * 256)
                for j in range(2)], 0)
        m["wd"] = np.stack([np.ascontiguousarray(
            np.asarray(w_down[0, 2 * i + j], f).reshape(FC, 128, NDB, 256).transpose(2, 1, 0, 3)).reshape(NDB, 128, FC * 256)
            for j in range(2)], 0)
        maps.append(m)
    res = _run(build_experts(), maps)
    del maps
    y_all = np.concatenate([np.asarray(r["y"], f) for r in res], 0)
    sel = [[] for _ in range(8192)]
    for e in range(16):
        for s_, t_ in enumerate(idx[e]):
            if valid[e, s_]:
                sel[int(t_)].append((e, s_))
    K = max(1, max(len(v) for v in sel))
    yk = np.zeros((K, 8192, D), f)
    for k in range(K):
        tt = [t_ for t_ in range(8192) if len(sel[t_]) > k]
        if tt:
            ee = [sel[t_][k][0] for t_ in tt]
            ss = [sel[t_][k][1] for t_ in tt]
            yk[k, tt] = y_all[ee, ss]
    rows3 = np.ascontiguousarray(np.stack([np.broadcast_to(v[None], (128, D)) for v in
                                           (mod[5], np.asarray(inputs["ln2_g"], f)[0], np.asarray(inputs["ln2_b"], f)[0])], 0))
    maps = [{"xm": np.ascontiguousarray(x_mid[1024 * i:1024 * (i + 1)]),
             "yk": np.ascontiguousarray(yk[:, 1024 * i:1024 * (i + 1)]), "rows": rows3} for i in cores]
    res = _run(build_combine(K), maps)
    out = np.concatenate([np.asarray(r["out"], f) for r in res], 0)
    return out.reshape(1, 8192, D).astype(f)
```

```python
import numpy as np
import concourse.bass as bass
import concourse.mybir as mybir
from concourse.bass_utils import run_bass_kernel_spmd

F32 = mybir.dt.float32
BF16 = mybir.dt.bfloat16
I32 = mybir.dt.int32
AF = mybir.ActivationFunctionType
ALU = mybir.AluOpType
AX = mybir.AxisListType

NCORES = 8
D = 2048
KC = 16
NT = 10
TOK = 1280
DFF = 5632
FC = 44
CAP = 1024
ALPHA = 2.0 ** 0.25
EPS = 1e-5


class Eng:
    def __init__(self, nc, e, name):
        self.e = e
        self.name = name
        self.sem = nc.alloc_semaphore("s_" + name)
        self.n = 0
        self.seen = {}
        self.serial = False


class Ser:
    def __init__(self, eng):
        self._eng = eng
        eng.serial = True

    def __getattr__(self, name):
        eng = self._eng
        fn = getattr(eng.e, name)

        def call(*a, **k):
            if eng.n > 0:
                eng.e.wait_ge(eng.sem, eng.n)
            ins = fn(*a, **k)
            eng.n += 1
            ins.then_inc(eng.sem, 1)
            return ins
        return call


class Ctx:
    def __init__(self, nc):
        self.nc = nc
        self.pe = Eng(nc, nc.tensor, "pe")
        self.act = Eng(nc, nc.scalar, "act")
        self.dve = Eng(nc, nc.vector, "dve")
        self.pool = Eng(nc, nc.gpsimd, "pool")
        self.sp = Eng(nc, nc.sync, "sp")
        self.nsem = 0

    def done(self, eng, ins):
        if eng.serial:
            return ("e", eng, eng.n)
        eng.n += 1
        ins.then_inc(eng.sem, 1)
        return ("e", eng, eng.n)

    def newsem(self):
        self.nsem += 1
        return [self.nc.alloc_semaphore("d%d" % self.nsem), 0]

    def dma(self, eng, out, in_, ds, **kw):
        ins = eng.e.dma_start(out=out, in_=in_, **kw)
        ds[1] += 16
        ins.then_inc(ds[0], 16)
        return ("d", ds[0], ds[1])

    def need(self, eng, tok):
        if tok is None:
            return
        if isinstance(tok, list):
            for t in tok:
                self.need(eng, t)
            return
        if tok[0] == "e":
            src, n = tok[1], tok[2]
            if src is eng:
                return
            key = src.name
        else:
            key, n = id(tok[1]), tok[2]
        if eng.seen.get(key, 0) >= n:
            return
        eng.seen[key] = n
        eng.e.wait_ge(tok[1].sem if tok[0] == "e" else tok[1], n)


def build(debug=None):
    nc = bass.Bass("TRN2", target_bir_lowering=False)

    def din(name, shape, dt=F32):
        return nc.dram_tensor(name, list(shape), dt, kind="ExternalInput").ap()

    def dout(name, shape, dt=F32):
        return nc.dram_tensor(name, list(shape), dt, kind="ExternalOutput").ap()

    xh = din("xh", [TOK, D])
    ctxd = din("ctx", [256, D])
    win_d = din("win", [7, 128, KC * 512])
    pcols_d = din("pcols", [128, 16 + 8 * 31 + 24 + 2])
    bqkv_d = din("bqkv", [128, 1536])
    rope_d = din("rope", [128, NT, 128])
    masks_d = din("masks", [128, 2, 128])
    sink_d = din("sink", [128, 8])
    wout_d = din("wout", [4, 128, KC * 512])
    rows_d = din("rows", [5, 128, D])
    wr_d = din("wr", [128, KC * 16])
    modT_d = din("modT", [128, 192])
    mrows_d = din("mrows", [3, 128, D])

    dbg = {}

    def dbg_out(name, shape, dt=F32):
        dbg[name] = dout("dbg_" + name, shape, dt)
        return dbg[name]

    mixd = nc.dram_tensor("mixd", [1024, D], F32)

    def sb(name, cols, dt=F32):
        return nc.alloc_sbuf_tensor("sb_" + name, [128, cols], dt)[:]

    ident_b = sb("ident_b", 128, BF16)
    ident_f = sb("ident_f", 128, F32)
    pcols = sb("pcols", 16 + 8 * 31 + 24 + 2)
    bconv = pcols[:, 0:16]
    wdw = pcols[:, 16:16 + 248].rearrange("p (c k) -> p c k", k=31)
    bdw = pcols[:, 264:272]
    clg = pcols[:, 272:280]
    clb = pcols[:, 280:288]
    hval = pcols[:, 288:290]
    modT = sb("modT", 2 * 96)
    sc1p = sb("sc1p", 32)
    sc2p = sb("sc2p", 16)
    ones_f = sb("ones_f", 128, F32)
    ones_b = sb("ones_b", 128, BF16)
    small = sb("small", 64)
    stats = sb("stats", 4 * 6 * 2)
    bqkv = sb("bqkv", 1536)
    rope_t = sb("rope_t", NT * 128)
    masks_f = sb("masks_f", 256)
    masks = sb("masks", 4 * 128, BF16)
    sinke = sb("sinke", 8)
    wr = sb("wr", KC * 16)

    RA = sb("RA", 16384)
    RB = sb("RB", 8448)
    RC = sb("RC", 8192)
    RE = sb("RE", 7680)
    RF = sb("RF", 6144)

    hT = RA[:, 0:10240].bitcast(BF16).rearrange("p (k t) -> p k t", k=KC)
    hcT = RA[:, 10240:12288].bitcast(BF16).rearrange("p (k t) -> p k t", k=KC)
    xstage = [RA[:, 12288:14336], RA[:, 14336:16384]]
    xn_b = [RF[:, 0:1024].bitcast(BF16), RF[:, 1024:2048].bitcast(BF16)]
    wslot = [RC[:, 0:4096].bitcast(BF16).rearrange("p (k c) -> p k c", k=KC),
             RC[:, 4096:8192].bitcast(BF16).rearrange("p (k c) -> p k c", k=KC)]
    wmod_sb = RC.rearrange("p (k c) -> p k c", k=KC)
    uT = RB.rearrange("p (c t) -> p c t", c=8)
    qT = RE[:, 0:4096].bitcast(BF16).rearrange("p (h t) -> p h t", h=8)
    kT = RE[:, 4096:5376].bitcast(BF16).rearrange("p (h t) -> p h t", h=2)
    vtok = RE[:, 5376:6656].bitcast(BF16).rearrange("p (t c) -> p t c", t=NT)
    kcT = RE[:, 6656:6912].bitcast(BF16).rearrange("p (h t) -> p h t", h=2)
    vctok = RE[:, 6912:7168].bitcast(BF16).rearrange("p (t c) -> p t c", t=2)
    qtok = RF[:, 2048:2560]
    ropeA = RF[:, 2560:3072]
    ropeB = RF[:, 3072:3584]
    qrb = RF[:, 3584:3840].bitcast(BF16)
    gsig = RF[:, 3840:4224]

    ps = [nc.alloc_psum_tensor("ps%d" % i, [128, 512], F32)[:] for i in range(6)]
    psb = [nc.alloc_psum_tensor("psb%d" % i, [128, 1024], BF16)[:] for i in range(2)]

    C = Ctx(nc)
    pe, act, dve, pool, sp = C.pe, C.act, C.dve, C.pool, C.sp
    T, S = nc.tensor, nc.sync
    V, A, G = Ser(dve), Ser(act), Ser(pool)

    with nc.Block() as block:
        @block.sync
        def _(sync_engine):
            ld0 = C.newsem()
            C.dma(sp, pcols, pcols_d, ld0)
            C.dma(sp, bqkv, bqkv_d, ld0)
            C.dma(sp, rope_t, rope_d.rearrange("p t c -> p (t c)"), ld0)
            C.dma(sp, masks_f, masks_d.rearrange("p a c -> p (a c)"), ld0)
            C.dma(sp, sinke, sink_d, ld0)
            t_ld0 = C.dma(sp, wr, wr_d, ld0)

            G.memset(ident_b, 0.0)
            G.affine_select(out=ident_b, in_=ident_b, pattern=[[-1, 128]], compare_op=ALU.not_equal,
                            fill=1.0, base=0, channel_multiplier=1)
            G.memset(ones_f, 1.0)
            G.memset(ones_b, 1.0)
            G.memset(ident_f, 0.0)
            t_id = C.done(pool, G.affine_select(out=ident_f, in_=ident_f, pattern=[[-1, 128]],
                                                compare_op=ALU.not_equal, fill=1.0, base=0,
                                                channel_multiplier=1))

            t_mt = C.dma(sp, modT, modT_d, ld0)
            C.need(dve, t_mt)
            V.tensor_scalar(out=sc1p[:, 0:16], in0=modT[:, 16:32], scalar1=1.0, scalar2=None, op0=ALU.add)
            V.tensor_scalar(out=sc1p[:, 16:32], in0=modT[:, 96 + 16:96 + 32], scalar1=1.0, scalar2=None, op0=ALU.add)
            t_mod = C.done(dve, V.tensor_scalar(out=sc2p, in0=modT[:, 64:80], scalar1=1.0, scalar2=None, op0=ALU.add))
            sh1 = [modT[:, 0:16], modT[:, 96:96 + 16]]
            sh2 = modT[:, 48:64]

            if debug == "ln0":
                o = dbg_out("sc", [128, 32])
                C.need(sp, t_mod)
                C.need(sp, t_id)
                t = C.dma(sp, o, sc1p, C.newsem())
                C.need(sp, t)
                return
            wsem = [C.newsem(), C.newsem()]
            wtok = {}
            wrel = {}

            def issue_w(g, src):
                slot = g % 2
                if g - 2 in wrel:
                    C.need(pool, wrel[g - 2])
                for q in range(4):
                    tk = C.dma(pool, wslot[slot].rearrange("p k c -> p (k c)")[:, q * 2048:(q + 1) * 2048],
                               src[:, q * 2048:(q + 1) * 2048], wsem[slot])
                wtok[g] = tk

            if debug != "ln1":
                issue_w(0, win_d[0])
                issue_w(1, win_d[1])

            xsem = [C.newsem(), C.newsem()]
            x_rel = [None, None]
            xn_rel = [None, None]
            tr_rel = [None, None]
            t_h = None
            for it in range(NT + 2):
                s = it % 2
                isctx = it >= NT
                src = ctxd[(it - NT) * 128:(it - NT + 1) * 128, :] if isctx else xh[it * 128:(it + 1) * 128, :]
                C.need(sp, x_rel[s])
                t_x = C.dma(sp, xstage[s], src, xsem[s])
                C.need(dve, t_x)
                st = stats[:, 0:24].rearrange("p (c s) -> p c s", s=6)
                for c4 in range(4):
                    V.bn_stats(out=st[:, c4, :], in_=xstage[s][:, c4 * 512:(c4 + 1) * 512])
                mv = small[:, 0:2]
                V.bn_aggr(out=mv, in_=stats[:, 0:24])
                rstd = small[:, 2 + 2 * s:3 + 2 * s]
                nmr = small[:, 3 + 2 * s:4 + 2 * s]
                C.need(dve, xn_rel[s])
                t_v = C.done(dve, V.tensor_scalar(out=small[:, 8:9], in0=mv[:, 1:2], scalar1=EPS, scalar2=None, op0=ALU.add))
                C.need(act, t_v)
                t_sq = C.done(act, A.sqrt(out=small[:, 9:10], in_=small[:, 8:9]))
                C.need(dve, t_sq)
                V.reciprocal(out=rstd, in_=small[:, 9:10])
                t_st = C.done(dve, V.scalar_tensor_tensor(out=nmr, in0=mv[:, 0:1], scalar=-1.0, in1=rstd,
                                                          op0=ALU.mult, op1=ALU.mult))
                C.need(act, t_st)
                C.need(act, xn_rel[s])
                t_xn = C.done(act, A.activation(out=xn_b[s], in_=xstage[s], func=AF.Identity, bias=nmr, scale=rstd))
                x_rel[s] = t_xn
                C.need(pe, t_xn)
                C.need(pe, t_id)
                for half in range(2):
                    pst = psb[half]
                    C.need(pe, tr_rel[half])
                    for k8 in range(8):
                        kc = half * 8 + k8
                        mm = T.transpose(pst[:, k8 * 128:(k8 + 1) * 128], xn_b[s][:, kc * 128:(kc + 1) * 128], ident_b)
                    t_tr = C.done(pe, mm)
                    if half == 1:
                        xn_rel[s] = t_tr
                    dst = hcT if isctx else hT
                    t0 = (it - NT) * 128 if isctx else it * 128
                    j = 1 if isctx else 0
                    C.need(dve, t_tr)
                    C.need(act, t_tr)
                    C.need(dve, t_mod)
                    C.need(act, t_mod)
                    for k8 in range(8):
                        kc = half * 8 + k8
                        o_ = dst[:, kc, t0:t0 + 128]
                        i_ = pst[:, k8 * 128:(k8 + 1) * 128]
                        insd = V.tensor_scalar(out=o_, in0=i_, scalar1=sc1p[:, j * 16 + kc:j * 16 + kc + 1],
                                               scalar2=sh1[j][:, kc:kc + 1], op0=ALU.mult, op1=ALU.add)
                    td = C.done(dve, insd)
                    ta = td
                    tr_rel[half] = [td, ta]
                    t_h = [td, ta]

            if debug in ("h", "ln1"):
                o = dbg_out("hT", [128, 6 * TOK], F32)
                C.need(dve, t_h)
                t_c = C.done(dve, V.tensor_copy(out=RB[:, 0:6 * TOK], in_=hT.rearrange("p k t -> p (k t)")[:, 0:6 * TOK]))
                C.need(sp, t_c)
                t = C.dma(sp, o, RB[:, 0:6 * TOK], C.newsem())
                C.need(sp, t)
                return

            C.need(pe, t_h)
            C.need(act, t_ld0)
            C.need(dve, t_ld0)
            bank_rel = {}

            def getbank(b, eng):
                C.need(eng, bank_rel.get(b))

            TP = 352
            t_u = None
            for g in range(4):
                if g >= 2:
                    pass
                C.need(pe, wtok[g])
                W = wslot[g % 2]
                for cj in range(2):
                    cc = 2 * g + cj
                    for tp in range(3):
                        tk0 = 112 + tp * TP
                        bg, bv = (tp % 2) * 2, 1 + (tp % 2) * 2
                        getbank(bg, pe)
                        for kc in range(KC):
                            mm = T.matmul(ps[bg][:, 0:TP], lhsT=W[:, kc, 256 + cj * 128:256 + (cj + 1) * 128],
                                          rhs=hT[:, kc, tk0:tk0 + TP], start=(kc == 0), stop=(kc == KC - 1))
                        t_g = C.done(pe, mm)
                        getbank(bv, pe)
                        for kc in range(KC):
                            mm = T.matmul(ps[bv][:, 0:TP], lhsT=W[:, kc, cj * 128:(cj + 1) * 128],
                                          rhs=hT[:, kc, tk0:tk0 + TP], start=(kc == 0), stop=(kc == KC - 1))
                        t_v = C.done(pe, mm)
                        C.need(act, t_g)
                        C.need(act, bank_rel.get(("gsig", tp % 2)))
                        gs_ = gsig if tp % 2 == 0 else ropeA[:, 0:384]
                        t_s = C.done(act, A.activation(out=gs_[:, 0:TP], in_=ps[bg][:, 0:TP], func=AF.Sigmoid,
                                                       bias=bconv[:, 8 + cc:9 + cc]))
                        bank_rel[bg] = t_s
                        C.need(dve, t_s)
                        C.need(dve, t_v)
                        t_u = C.done(dve, V.scalar_tensor_tensor(out=uT[:, cc, tp * TP:(tp + 1) * TP], in0=ps[bv][:, 0:TP],
                                                                 scalar=bconv[:, cc:cc + 1], in1=gs_[:, 0:TP],
                                                                 op0=ALU.add, op1=ALU.mult))
                        bank_rel[bv] = t_u
                        bank_rel[("gsig", tp % 2)] = t_u
                wrel[g] = C.done(pe, T.matmul(ps[bv][:, 0:1], lhsT=W[:, 0, 0:128], rhs=hT[:, 0, 0:1], start=True, stop=True)) if False else t_v
                if g + 2 < 7:
                    issue_w(g + 2, win_d[g + 2])

            if debug == "u":
                o = dbg_out("uT", [128, 8 * 1056])
                C.need(sp, t_u)
                t = C.dma(sp, o, RB, C.newsem())
                C.need(sp, t)
                return

            def rope(dst_b, src_f, nh, tile):
                for a in range(2):
                    Xa = src_f.rearrange("p (h a f) -> p h a f", a=2, f=64)[:, :, a, :]
                    Oa = dst_b.rearrange("p (h a f) -> p h a f", a=2, f=64)[:, :, a, :]
                    Aa = ropeA[:, 0:nh * 64].rearrange("p (h f) -> p h f", f=64)
                    Ba = ropeB[:, 0:nh * 64].rearrange("p (h f) -> p h f", f=64)
                    cs = rope_t[:, tile * 128 + a * 32:tile * 128 + a * 32 + 32]
                    sn = rope_t[:, tile * 128 + 64 + a * 32:tile * 128 + 64 + a * 32 + 32]
                    for hf in range(2):
                        V.tensor_tensor(out=Aa[:, :, hf * 32:(hf + 1) * 32], in0=Xa[:, :, hf * 32:(hf + 1) * 32],
                                        in1=cs.unsqueeze(1).to_broadcast([128, nh, 32]), op=ALU.mult)
                        V.tensor_tensor(out=Ba[:, :, hf * 32:(hf + 1) * 32], in0=Xa[:, :, (1 - hf) * 32:(2 - hf) * 32],
                                        in1=sn.unsqueeze(1).to_broadcast([128, nh, 32]), op=ALU.mult)
                    V.tensor_tensor(out=Oa[:, :, 0:32], in0=Aa[:, :, 0:32], in1=Ba[:, :, 0:32], op=ALU.subtract)
                    last = V.tensor_tensor(out=Oa[:, :, 32:64], in0=Aa[:, :, 32:64], in1=Ba[:, :, 32:64], op=ALU.add)
                return last

            qrb_rel = None
            trb_rel = None
            t_last = None
            for g in (4, 5, 6):
                C.need(pe, wtok[g])
                W = wslot[g % 2]
                tiles = list(range(1, 9)) if g < 6 else list(range(NT + 2))
                mmtok = {}

                def emit_mm(ti):
                    tl = tiles[ti]
                    isctx = tl >= NT
                    lh = hcT[:, :, (tl - NT) * 128:(tl - NT + 1) * 128] if isctx else hT[:, :, tl * 128:(tl + 1) * 128]
                    b = 4 + (ti % 2)
                    getbank(b, pe)
                    for kc in range(KC):
                        mm = T.matmul(ps[b], lhsT=lh[:, kc, :], rhs=W[:, kc, :], start=(kc == 0), stop=(kc == KC - 1))
                    mmtok[ti] = C.done(pe, mm)

                emit_mm(0)
                for ti, tl in enumerate(tiles):
                    if ti + 1 < len(tiles):
                        emit_mm(ti + 1)
                    isctx = tl >= NT
                    b = 4 + (ti % 2)
                    t_mm = mmtok[ti]
                    C.need(dve, t_mm)
                    boff = (g - 4) * 512
                    C.need(dve, qrb_rel)
                    if g < 6:
                        V.tensor_tensor(out=qtok, in0=ps[b], in1=bqkv[:, boff:boff + 512], op=ALU.add)
                        t_r = C.done(dve, rope(qrb, qtok, 4, tl))
                        bank_rel[b] = t_r
                        ntr = 4
                    else:
                        V.tensor_tensor(out=qtok, in0=ps[b], in1=bqkv[:, boff:boff + 512], op=ALU.add)
                        vdst = vctok[:, tl - NT, :] if isctx else vtok[:, tl, :]
                        if isctx:
                            V.tensor_copy(out=qrb[:, 0:256], in_=qtok[:, 0:256])
                            t_r = C.done(dve, V.tensor_copy(out=vdst, in_=qtok[:, 256:512]))
                        else:
                            V.tensor_copy(out=vdst, in_=qtok[:, 256:512])
                            t_r = C.done(dve, rope(qrb[:, 0:256], qtok[:, 0:256], 2, tl))
                        bank_rel[b] = t_r
                        ntr = 2
                    C.need(pe, t_r)
                    C.need(pe, trb_rel)
                    C.need(pe, tr_rel[0])
                    pst = psb[0]
                    for hh in range(ntr):
                        mm = T.transpose(pst[:, hh * 128:(hh + 1) * 128], qrb[:, hh * 128:(hh + 1) * 128], ident_b)
                    t_tr = C.done(pe, mm)
                    qrb_rel = t_tr
                    C.need(act, t_tr)
                    if g < 6:
                        o_ = qT[:, (g - 4) * 4:(g - 4) * 4 + 4, (tl - 1) * 128:tl * 128]
                    elif isctx:
                        o_ = kcT[:, :, (tl - NT) * 128:(tl - NT + 1) * 128]
                    else:
                        o_ = kT[:, :, tl * 128:(tl + 1) * 128]
                    t_last = C.done(act, A.activation(out=o_, in_=pst[:, 0:ntr * 128].rearrange("p (h t) -> p h t", t=128),
                                                      func=AF.Copy))
                    trb_rel = t_last
                t_mm = mmtok[len(tiles) - 1]
                wrel[g] = t_tr
                if g + 2 < 7:
                    issue_w(g + 2, win_d[g + 2])

            if debug == "qkv":
                o1 = dbg_out("qT", [128, 8 * 1024], BF16)
                o2 = dbg_out("kT", [128, 2 * 1280], BF16)
                o3 = dbg_out("vtok", [128, NT * 256], BF16)
                o4 = dbg_out("kcT", [128, 512], BF16)
                C.need(sp, t_last)
                C.need(sp, t_r)
                ds_ = C.newsem()
                C.dma(sp, o1, qT.rearrange("p h t -> p (h t)"), ds_)
                C.dma(sp, o2, kT.rearrange("p h t -> p (h t)"), ds_)
                C.dma(sp, o3, vtok.rearrange("p t c -> p (t c)"), ds_)
                t = C.dma(sp, o4, kcT.rearrange("p h t -> p (h t)"), ds_)
                C.need(sp, t)
                return
            cv = RA[:, 0:8192].rearrange("p (c t) -> p c t", c=8)
            aT = RA[:, 8192:16384].bitcast(BF16).rearrange("p (k t) -> p k t", k=KC)
            C.need(dve, [t_last, t_mm, t_r])
            C.need(pool, [t_last, t_mm, t_r])
            V.tensor_scalar(out=uT[:, :, 0:16], in0=uT[:, :, 0:16], scalar1=hval[:, 0:1], scalar2=None, op0=ALU.mult)
            t_hm = C.done(dve, V.tensor_scalar(out=uT[:, :, 1040:1056], in0=uT[:, :, 1040:1056],
                                               scalar1=hval[:, 1:2], scalar2=None, op0=ALU.mult))
            C.need(pool, t_hm)
            t_cv = []
            uTb = RC[:, 2560:6784].bitcast(BF16).rearrange("p (c t) -> p c t", c=8)
            dgb = [RF[:, 0:1984].bitcast(BF16).rearrange("p (k c) -> p k c", k=31),
                   RF[:, 3584:5568].bitcast(BF16).rearrange("p (k c) -> p k c", k=31)]
            t_ub = C.done(dve, V.tensor_copy(out=uTb, in_=uT))
            dg_rel = [None, None]
            cb_rel = [None, None]

            def taps_pe(cc):
                dg = dgb[cc % 2]
                C.need(dve, dg_rel[cc % 2])
                t_dg = C.done(dve, V.tensor_tensor(out=dg, in0=ident_b.unsqueeze(1).to_broadcast([128, 31, 128]),
                                                   in1=wdw[:, cc, :].unsqueeze(2).to_broadcast([128, 31, 128]), op=ALU.mult))
                C.need(pe, [t_dg, t_ub])
                for th in range(2):
                    C.need(pe, cb_rel[th])
                    for k in range(31):
                        mm = T.matmul(ps[4 + th], lhsT=dg[:, k, :], rhs=uTb[:, cc, k + 1 + th * 512:k + 1 + th * 512 + 512],
                                      start=(k == 0), stop=(k == 30))
                    t_m = C.done(pe, mm)
                    C.need(act, t_m)
                    t_e = C.done(act, A.activation(out=cv[:, cc, th * 512:(th + 1) * 512], in_=ps[4 + th], func=AF.Identity,
                                                   bias=bdw[:, cc:cc + 1]))
                    cb_rel[th] = t_e
                    t_cv.append(t_e)
                dg_rel[cc % 2] = t_m

            taps_dve = taps_pe

            V.tensor_copy(out=masks[:, 0:256], in_=masks_f)
            V.tensor_scalar(out=masks[:, 256:384], in0=masks_f[:, 0:128], scalar1=hval[:, 0:1], scalar2=None, op0=ALU.mult)
            t_mk = C.done(dve, V.tensor_scalar(out=masks[:, 384:512], in0=masks_f[:, 128:256], scalar1=hval[:, 1:2],
                                               scalar2=None, op0=ALU.mult))
            t_sk = C.done(act, A.activation(out=sinke, in_=sinke, func=AF.Exp))
            pbuf = [RC[:, i * 256:(i + 1) * 256].bitcast(BF16) for i in range(10)]
            dtmp = RF[:, 3072:3584]
            SCALE = 128.0 ** -0.5
            C.need(pe, [t_mk, t_sk])
            s_rel = [None, None]
            p_rel = [None] * 10
            od_rel = [None, None]
            itn = 0
            scnt = 0
            t_f = None
            t_pv = None
            next_tap = 0

            def hq(ap):
                return ap.rearrange("p (h q) -> p h q", h=4)

            for t in range(1, 9):
                for kv in range(2):
                    par = itn % 2
                    tiles = [("c", 0), ("c", 1), ("w", t - 1), ("w", t), ("w", t + 1)]
                    ptoks = []
                    for i, (kind, idx) in enumerate(tiles):
                        keyT = kcT[:, kv, idx * 128:(idx + 1) * 128] if kind == "c" else kT[:, kv, idx * 128:(idx + 1) * 128]
                        sbk = scnt % 2
                        scnt += 1
                        C.need(pe, s_rel[sbk])
                        t_s = C.done(pe, T.matmul(ps[sbk], lhsT=keyT, rhs=qT[:, 4 * kv:4 * kv + 4, (t - 1) * 128:t * 128],
                                                  start=True, stop=True))
                        pb = pbuf[par * 5 + i]
                        C.need(act, t_s)
                        C.need(act, p_rel[par * 5 + i])
                        t_e = C.done(act, A.activation(out=pb, in_=ps[sbk], func=AF.Exp, scale=SCALE))
                        s_rel[sbk] = t_e
                        if kind == "w" and idx != t:
                            if idx == t - 1:
                                mk = masks[:, 256:384] if t == 1 else masks[:, 0:128]
                            else:
                                mk = masks[:, 384:512] if t == 8 else masks[:, 128:256]
                            C.need(dve, t_e)
                            t_e = C.done(dve, V.tensor_tensor(out=hq(pb), in0=hq(pb),
                                                              in1=mk.unsqueeze(1).to_broadcast([128, 4, 128]), op=ALU.mult))
                        ptoks.append(t_e)
                    C.need(pe, od_rel[0])
                    for i, (kind, idx) in enumerate(tiles):
                        vt = vctok[:, idx, kv * 128:(kv + 1) * 128] if kind == "c" else vtok[:, idx, kv * 128:(kv + 1) * 128]
                        C.need(pe, ptoks[i])
                        T.matmul(ps[2], lhsT=vt, rhs=pbuf[par * 5 + i], start=(i == 0), stop=(i == 4))
                    for i in range(5):
                        mm = T.matmul(ps[3], lhsT=ones_b, rhs=pbuf[par * 5 + i], start=(i == 0), stop=(i == 4))
                    t_pv = C.done(pe, mm)
                    for i in range(5):
                        p_rel[par * 5 + i] = t_pv
                    C.need(dve, t_pv)
                    C.need(dve, t_sk)
                    V.tensor_tensor(out=hq(dtmp), in0=hq(ps[3]),
                                    in1=sinke[:, 4 * kv:4 * kv + 4].unsqueeze(2).to_broadcast([128, 4, 128]), op=ALU.add)
                    V.reciprocal(out=dtmp, in_=dtmp)
                    t_f = C.done(dve, V.tensor_tensor(out=aT[:, 8 + 4 * kv:8 + 4 * kv + 4, (t - 1) * 128:t * 128],
                                                      in0=hq(ps[2]), in1=hq(dtmp), op=ALU.mult))
                    od_rel[0] = t_f
                    itn += 1
                    if itn % 2 == 0 and next_tap < 8:
                        taps_dve(next_tap)
                        next_tap += 1
            while next_tap < 8:
                taps_dve(next_tap)
                next_tap += 1

            if debug == "attn":
                o = dbg_out("attn", [128, 8 * 1024], BF16)
                C.need(sp, t_f)
                t = C.dma(sp, o, aT[:, 8:16, :].rearrange("p k t -> p (k t)"), C.newsem())
                C.need(sp, t)
                return

            sq = RB[:, 0:8192].rearrange("p (c t) -> p c t", c=8)
            C.need(act, t_cv)
            for cc in range(8):
                ins = A.activation(out=sq[:, cc, :], in_=cv[:, cc, :], func=AF.Square)
            t_sq = C.done(act, ins)
            C.need(pe, t_sq)
            C.need(pe, t_cv)
            C.need(pe, t_id)
            C.need(pe, [t_f] + s_rel)
            for th in range(2):
                for cc in range(8):
                    T.matmul(ps[th], lhsT=ones_f, rhs=cv[:, cc, th * 512:(th + 1) * 512], start=(cc == 0), stop=(cc == 7))
                for cc in range(8):
                    mm = T.matmul(ps[2 + th], lhsT=ones_f, rhs=sq[:, cc, th * 512:(th + 1) * 512], start=(cc == 0), stop=(cc == 7))
            t_stat = C.done(pe, mm)
            cmean = RF[:, 0:1024]
            crstd = RF[:, 1024:2048]
            ctmp = RF[:, 2048:3072]
            C.need(dve, t_stat)
            C.need(dve, t_cv)
            for th in range(2):
                sl = slice(th * 512, (th + 1) * 512)
                V.tensor_scalar(out=cmean[:, sl], in0=ps[th], scalar1=1.0 / 1024, scalar2=None, op0=ALU.mult)
                V.tensor_tensor(out=ctmp[:, sl], in0=cmean[:, sl], in1=cmean[:, sl], op=ALU.mult)
                V.scalar_tensor_tensor(out=ctmp[:, sl], in0=ps[2 + th], scalar=1.0 / 1024, in1=ctmp[:, sl],
                                       op0=ALU.mult, op1=ALU.subtract)
                ins = V.tensor_scalar(out=ctmp[:, sl], in0=ctmp[:, sl], scalar1=EPS, scalar2=None, op0=ALU.add)
            t_var = C.done(dve, ins)
            C.need(act, t_var)
            t_sd = C.done(act, A.sqrt(out=ctmp, in_=ctmp))
            C.need(dve, t_sd)
            V.reciprocal(out=crstd, in_=ctmp)
            t_ac = None
            for cc in range(8):
                V.tensor_tensor(out=cv[:, cc, :], in0=cv[:, cc, :], in1=cmean, op=ALU.subtract)
                t_z = C.done(dve, V.tensor_tensor(out=cv[:, cc, :], in0=cv[:, cc, :], in1=crstd, op=ALU.mult))
                C.need(act, t_z)
                t_ac = C.done(act, A.activation(out=aT[:, cc, :], in_=cv[:, cc, :], func=AF.Silu,
                                                bias=clb[:, cc:cc + 1], scale=clg[:, cc:cc + 1]))

            if debug == "aconv":
                o = dbg_out("aconv", [128, 8 * 1024], BF16)
                C.need(sp, t_ac)
                t = C.dma(sp, o, aT[:, 0:8, :].rearrange("p k t -> p (k t)"), C.newsem())
                C.need(sp, t)
                return

            rowsA = RB[:, 0:8192].rearrange("p (r d) -> p r d", r=4)
            rowsB = RE[:, 0:4096].rearrange("p (r d) -> p r d", r=2)
            C.need(sp, [t_stat, t_pv, t_f])
            rsem = C.newsem()
            C.dma(sp, rowsA[:, 0:3, :], rows_d[0:3].rearrange("r p d -> p r d"), rsem)
            C.dma(sp, rowsA[:, 3, :], mrows_d[0], rsem)
            t_rows = C.dma(sp, rowsB, mrows_d[1:3].rearrange("r p d -> p r d"), rsem)
            C.need(dve, t_rows)
            t_rows2 = C.done(dve, V.tensor_scalar(out=rowsB[:, 0, :], in0=rowsB[:, 0, :], scalar1=1.0, scalar2=None, op0=ALU.add))
            wosem = [C.newsem(), C.newsem()]
            wotok = {}
            worel = {}

            def issue_wo(g):
                slot = g % 2
                if g - 2 in worel:
                    C.need(pool, worel[g - 2])
                for q in range(4):
                    tk = C.dma(pool, wslot[slot].rearrange("p k c -> p (k c)")[:, q * 2048:(q + 1) * 2048],
                               wout_d[g][:, q * 2048:(q + 1) * 2048], wosem[slot])
                wotok[g] = tk

            C.need(pool, t_pv)
            C.need(pool, t_cv)
            issue_wo(0)
            issue_wo(1)
            C.need(pe, [t_f, t_ac, t_var])
            C.need(pe, s_rel)
            ev = [RF[:, 3584:4096], RF[:, 4096:4608]]
            ev_rel = [None, None]
            mx_sem = [C.newsem(), C.newsem()]
            bk_rel = [None, None]
            cnt = 0
            for g in range(4):
                C.need(pe, wotok[g])
                W = wslot[g % 2]
                for t in range(8):
                    b = cnt % 2
                    cnt += 1
                    C.need(pe, bk_rel[b])
                    for fc in range(KC):
                        mm = T.matmul(ps[b], lhsT=aT[:, fc, t * 128:(t + 1) * 128], rhs=W[:, fc, :],
                                      start=(fc == 0), stop=(fc == KC - 1))
                    t_mm = C.done(pe, mm)
                    C.need(dve, t_mm)
                    C.need(dve, ev_rel[b])
                    t_c = C.done(dve, V.tensor_tensor(out=ev[b], in0=ps[b], in1=rowsA[:, 0, g * 512:(g + 1) * 512], op=ALU.add))
                    bk_rel[b] = t_c
                    C.need(sp, t_c)
                    ev_rel[b] = C.dma(sp, mixd.ap()[t * 128:(t + 1) * 128, g * 512:(g + 1) * 512], ev[b], mx_sem[b])
                worel[g] = t_mm
                if g + 2 < 4:
                    issue_wo(g + 2)

            xmid_o = dout("x_mid", [1024, D])
            h2_o = dout("h2b", [1024, D], BF16)
            aff_o = dout("aff", [1024, 16])
            C.need(sp, ev_rel)
            C.need(sp, [t_mm, t_ac])
            xt = RA[:, 0:2048]
            mt = RA[:, 2048:4096]
            xmb = [RA[:, 4096:6144], RA[:, 6144:8192]]
            h2buf = [RA[:, 8192:10240], RA[:, 14336:16384]]
            h2b = RA[:, 10240:11264].bitcast(BF16)
            h2T = RA[:, 12288:14336].rearrange("p (k t) -> p k t", k=KC)
            lg = small[:, 16:32]
            lsem = C.newsem()
            osem0 = [C.newsem(), C.newsem()]
            osem1 = C.newsem()
            osem2 = C.newsem()
            st8 = {"rel_xt": None, "o0": [None, None], "o1": None, "o2": None, "tp": [None, None], "h2": [None, None],
                   "sub": None}

            def ln_stats(src, k):
                st_ = stats[:, 0:24]
                for c4 in range(4):
                    V.bn_stats(out=st_[:, c4 * 6:(c4 + 1) * 6], in_=src[:, c4 * 512:(c4 + 1) * 512])
                mv_ = small[:, 0:2]
                V.bn_aggr(out=mv_, in_=st_)
                tv = C.done(dve, V.tensor_scalar(out=small[:, 8:9], in0=mv_[:, 1:2], scalar1=EPS, scalar2=None, op0=ALU.add))
                C.need(act, tv)
                ts = C.done(act, A.sqrt(out=small[:, 9:10], in_=small[:, 8:9]))
                C.need(dve, ts)
                rs_ = small[:, 10 + 2 * k:11 + 2 * k]
                nm_ = small[:, 11 + 2 * k:12 + 2 * k]
                V.reciprocal(out=rs_, in_=small[:, 9:10])
                tk = C.done(dve, V.scalar_tensor_tensor(out=nm_, in0=mv_[:, 0:1], scalar=-1.0, in1=rs_,
                                                        op0=ALU.mult, op1=ALU.mult))
                return rs_, nm_, tk

            def H1(t):
                xm = xmb[t % 2]
                C.need(sp, st8["rel_xt"])
                C.dma(sp, xt, xh[(t + 1) * 128:(t + 2) * 128, :], lsem)
                t_l = C.dma(sp, mt, mixd.ap()[t * 128:(t + 1) * 128, :], lsem)
                C.need(dve, t_l)
                C.need(dve, t_rows2)
                V.tensor_tensor(out=mt, in0=mt, in1=rowsA[:, 3, :], op=ALU.mult)
                V.scalar_tensor_tensor(out=mt, in0=xt, scalar=ALPHA, in1=mt, op0=ALU.mult, op1=ALU.add)
                rs_, nm_, tk = ln_stats(mt, 0)
                C.need(act, tk)
                C.need(act, st8["o0"][t % 2])
                t_a = C.done(act, A.activation(out=xm, in_=mt, func=AF.Identity, bias=nm_, scale=rs_))
                st8["rel_xt"] = t_a
                C.need(dve, t_a)
                V.tensor_tensor(out=xm, in0=xm, in1=rowsA[:, 1, :], op=ALU.mult)
                t_xm = C.done(dve, V.tensor_tensor(out=xm, in0=xm, in1=rowsA[:, 2, :], op=ALU.add))
                C.need(sp, t_xm)
                st8["o0"][t % 2] = C.dma(sp, xmid_o[t * 128:(t + 1) * 128, :], xm, osem0[t % 2])

            def H2a(t):
                xm = xmb[t % 2]
                h2 = h2buf[t % 2]
                rs_, nm_, tk = ln_stats(xm, 1)
                C.need(act, tk)
                C.need(act, st8["tp"][t % 2])
                t_a = C.done(act, A.activation(out=h2, in_=xm, func=AF.Identity, bias=nm_, scale=rs_))
                C.need(dve, t_a)
                V.tensor_tensor(out=h2, in0=h2, in1=rowsB[:, 0, :], op=ALU.mult)
                t_h2 = C.done(dve, V.tensor_tensor(out=h2, in0=h2, in1=rowsB[:, 1, :], op=ALU.add))
                st8["h2"][t % 2] = t_h2
                C.need(act, t_h2)
                C.need(act, st8["o1"])
                t_hb = C.done(act, A.activation(out=h2b, in_=h2, func=AF.Copy))
                C.need(sp, t_hb)
                st8["o1"] = C.dma(sp, h2_o[t * 128:(t + 1) * 128, :], h2b, osem1)

            def H3(t):
                h2 = h2buf[t % 2]
                C.need(pe, st8["h2"][t % 2])
                C.need(pe, st8["sub"])
                for kc in range(KC):
                    mm = T.transpose(ps[kc // 4][:, (kc % 4) * 128:(kc % 4 + 1) * 128], h2[:, kc * 128:(kc + 1) * 128], ident_f)
                t_tp = C.done(pe, mm)
                st8["tp"][t % 2] = t_tp
                C.need(act, t_tp)
                for q in range(4):
                    ins = A.activation(out=h2T[:, 4 * q:4 * q + 4, :], in_=ps[q].rearrange("p (k t) -> p k t", k=4), func=AF.Copy)
                t_cp = C.done(act, ins)
                C.need(pe, t_cp)
                for kc in range(KC):
                    mm = T.matmul(ps[4][:, 0:16], lhsT=h2T[:, kc, :], rhs=wr[:, kc * 16:(kc + 1) * 16],
                                  start=(kc == 0), stop=(kc == KC - 1))
                t_lg = C.done(pe, mm)
                C.need(dve, t_lg)
                C.need(dve, st8["o2"])
                V.tensor_reduce(out=small[:, 32:33], in_=ps[4][:, 0:16], axis=AX.X, op=ALU.max)
                t_sub = C.done(dve, V.tensor_scalar(out=lg, in0=ps[4][:, 0:16], scalar1=small[:, 32:33], scalar2=None,
                                                    op0=ALU.subtract))
                st8["sub"] = t_sub
                C.need(act, t_sub)
                t_ex = C.done(act, A.activation(out=lg, in_=lg, func=AF.Exp))
                C.need(dve, t_ex)
                V.tensor_reduce(out=small[:, 33:34], in_=lg, axis=AX.X, op=ALU.add)
                V.reciprocal(out=small[:, 34:35], in_=small[:, 33:34])
                t_af = C.done(dve, V.tensor_scalar(out=lg, in0=lg, scalar1=small[:, 34:35], scalar2=None, op0=ALU.mult))
                C.need(sp, t_af)
                st8["o2"] = C.dma(sp, aff_o[t * 128:(t + 1) * 128, :], lg, osem2)

            H1(0)
            for t in range(8):
                if t + 1 < 8:
                    H1(t + 1)
                H2a(t)
                if t >= 1:
                    H3(t - 1)
            H3(7)
            C.need(sp, st8["o0"])
            C.need(sp, [st8["o1"], st8["o2"]])
    return nc, dbg


def build_mod():
    nc = bass.Bass("TRN2", target_bir_lowering=False)
    cT_d = nc.dram_tensor("cT", [128, KC, 2], F32, kind="ExternalInput").ap()
    wmod_d = nc.dram_tensor("wmod", [3, 128, KC * 512], F32, kind="ExternalInput").ap()
    bmod_d = nc.dram_tensor("bmod", [2, 1536], F32, kind="ExternalInput").ap()
    o = nc.dram_tensor("modsl", [2, 1536], F32, kind="ExternalOutput").ap()
    cT = nc.alloc_sbuf_tensor("cT_sb", [128, KC * 2], F32)[:]
    scT = nc.alloc_sbuf_tensor("scT_sb", [128, KC * 2], F32)[:]
    bmod_sb = nc.alloc_sbuf_tensor("bm_sb", [2, 1536], F32)[:]
    modsl = nc.alloc_sbuf_tensor("modsl_sb", [2, 1536], F32)[:]
    RC = nc.alloc_sbuf_tensor("wm_sb", [128, KC * 512], F32)[:]
    wmod_sb = RC.rearrange("p (k c) -> p k c", k=KC)
    ps0 = nc.alloc_psum_tensor("psm", [128, 512], F32)[:]
    C = Ctx(nc)
    pe, act, dve, pool, sp = C.pe, C.act, C.dve, C.pool, C.sp
    T = nc.tensor
    V, A = Ser(dve), Ser(act)
    with nc.Block() as block:
        @block.sync
        def _(sync_engine):
            ld0 = C.newsem()
            C.dma(sp, cT, cT_d.rearrange("p k j -> p (k j)"), ld0)
            t_ld0 = C.dma(sp, bmod_sb, bmod_d, ld0)
            C.need(act, t_ld0)
            t_sc = C.done(act, A.activation(out=scT, in_=cT, func=AF.Silu))
            wm_sem = C.newsem()
            t_ev = None
            for cc in range(3):
                C.need(sp, t_ev)
                t_w = C.dma(sp, RC, wmod_d[cc], wm_sem)
                C.need(pe, t_w)
                C.need(pe, t_sc)
                for kc in range(KC):
                    mm = T.matmul(ps0[0:2, :], lhsT=scT.rearrange("p (k j) -> p k j", j=2)[:, kc, :],
                                  rhs=wmod_sb[:, kc, :], start=(kc == 0), stop=(kc == KC - 1))
                t_mm = C.done(pe, mm)
                C.need(dve, t_mm)
                C.need(dve, t_ld0)
                t_ev = C.done(dve, V.tensor_tensor(out=modsl[:, cc * 512:(cc + 1) * 512], in0=ps0[0:2, :],
                                                   in1=bmod_sb[:, cc * 512:(cc + 1) * 512], op=ALU.add))
            C.need(sp, t_ev)
            t = C.dma(sp, o, modsl, C.newsem())
            C.need(sp, t)
    return nc


def host_mod_inputs(inp):
    f = np.float32
    w_mod = np.asarray(inp["w_mod"], f)[0]
    b_mod = np.asarray(inp["b_mod"], f)[0]
    cvec = np.stack([np.asarray(inp["c"], f)[0], np.asarray(inp["c_ctx"], f)], 0)
    cT = np.ascontiguousarray(cvec.reshape(2, KC, 128).transpose(2, 1, 0))
    maps = []
    for i in range(NCORES):
        m = {"cT": cT}
        m["wmod"] = np.ascontiguousarray(
            w_mod[:, i * 1536:(i + 1) * 1536].reshape(KC, 128, 3, 512).transpose(2, 1, 0, 3)).reshape(3, 128, KC * 512)
        m["bmod"] = np.ascontiguousarray(np.broadcast_to(b_mod[i * 1536:(i + 1) * 1536][None], (2, 1536)))
        maps.append(m)
    return maps


def run_mod(inp):
    res = run_bass_kernel_spmd(build_mod(), host_mod_inputs(inp), core_ids=list(range(NCORES)))
    sl = np.stack([np.asarray(r["modsl"], np.float32) for r in res.results], 0)
    mod = np.ascontiguousarray(sl[:, 0, :]).reshape(6, D)
    mod_c = np.ascontiguousarray(sl[:, 1, :]).reshape(6, D)
    return mod, mod_c


def host_inputs(inp, mod, mod_c):
    f = np.float32
    x = np.asarray(inp["x"], f)[0]
    w_in = np.asarray(inp["w_in"], f)[0]
    b_in = np.asarray(inp["b_in"], f)[0]
    xpad = np.zeros((8192 + 256, D), f)
    xpad[128:128 + 8192] = x
    cols = []
    for g in range(4):
        cols.append(np.concatenate([np.arange(256 * g, 256 * g + 256), np.arange(1024 + 256 * g, 1024 + 256 * g + 256)]))
    cols.append(np.arange(2048, 2560))
    cols.append(np.arange(2560, 3072))
    cols.append(np.arange(3072, 3584))
    win = np.stack([np.ascontiguousarray(w_in[:, c].reshape(KC, 128, 512).transpose(1, 0, 2)).reshape(128, KC * 512)
                    for c in cols], 0)
    pc = np.zeros((128, 16 + 248 + 24 + 2), f)
    pc[:, 0:16] = b_in[:2048].reshape(16, 128).T
    pc[:, 16:264] = np.asarray(inp["w_dw"], f)[0].T.reshape(8, 128, 31).transpose(1, 0, 2).reshape(128, 248)
    pc[:, 264:272] = np.asarray(inp["b_dw"], f)[0].reshape(8, 128).T
    pc[:, 272:280] = np.asarray(inp["conv_ln_g"], f)[0].reshape(8, 128).T
    pc[:, 280:288] = np.asarray(inp["conv_ln_b"], f)[0].reshape(8, 128).T
    bqkv = np.ascontiguousarray(np.broadcast_to(b_in[2048:3584][None], (128, 1536)))
    inv = (10000.0 ** (-np.arange(0, 64, 2, dtype=np.float32) / 64)).astype(f)
    mk = np.zeros((128, 2, 128), f)
    jj = np.arange(128)[:, None]
    ii = np.arange(128)[None, :]
    mk[:, 0, :] = (jj >= ii)
    mk[:, 1, :] = (jj <= ii)
    sink = np.ascontiguousarray(np.broadcast_to(np.asarray(inp["sink"], f)[0][None], (128, 8)))
    w_out = np.asarray(inp["w_out"], f)[0]
    wout = np.stack([np.ascontiguousarray(w_out[:, g * 512:(g + 1) * 512].reshape(KC, 128, 512).transpose(1, 0, 2)).reshape(128, KC * 512)
                     for g in range(4)], 0)
    rows = np.stack([np.broadcast_to(np.asarray(inp[k], f)[0][None], (128, D))
                     for k in ("b_out", "ln1_g", "ln1_b", "ln2_g", "ln2_b")], 0)
    rows = np.ascontiguousarray(rows)
    wr = np.ascontiguousarray(np.asarray(inp["w_router"], f)[0].reshape(KC, 128, 16).transpose(1, 0, 2)).reshape(128, KC * 16)
    ctx = np.asarray(inp["ctx"], f)[0]
    modT = np.ascontiguousarray(np.concatenate([mod.reshape(96, 128).T, mod_c.reshape(96, 128).T], 1))
    mrows = np.ascontiguousarray(np.stack([np.broadcast_to(mod[k][None], (128, D)) for k in (2, 4, 3)], 0))
    maps = []
    for i in range(NCORES):
        m = {}
        m["xh"] = np.ascontiguousarray(xpad[1024 * i:1024 * i + TOK])
        m["ctx"] = ctx
        m["win"] = win
        p = pc.copy()
        p[:, 288] = 0.0 if i == 0 else 1.0
        p[:, 289] = 0.0 if i == NCORES - 1 else 1.0
        m["pcols"] = p
        m["bqkv"] = bqkv
        t = np.arange(1024 * i - 128, 1024 * i - 128 + TOK)
        ar = (t // 64).astype(f)[:, None] * inv[None]
        ac = (t % 64).astype(f)[:, None] * inv[None]
        tab = np.concatenate([np.cos(ar), np.cos(ac), np.sin(ar), np.sin(ac)], 1).astype(f)
        m["rope"] = np.ascontiguousarray(tab.reshape(NT, 128, 128).transpose(1, 0, 2))
        m["masks"] = mk
        m["sink"] = sink
        m["wout"] = wout
        m["rows"] = rows
        m["wr"] = wr
        m["modT"] = modT
        m["mrows"] = mrows
        maps.append(m)
    return maps


def _mk(nc):
    C = Ctx(nc)
    return C, nc.tensor, Ser(C.dve), Ser(C.act), Ser(C.pool)


def build_route():
    nc = bass.Bass("TRN2", target_bir_lowering=False)
    aff_d = nc.dram_tensor("aff", [8192, 16], F32, kind="ExternalInput").ap()
    mask_o = nc.dram_tensor("mask", [8192, 16], F32, kind="ExternalOutput").ap()

    def sb(name, cols, dt=F32):
        return nc.alloc_sbuf_tensor("r_" + name, [128, cols], dt)[:]
    aff = sb("aff", 1024)
    cmp_ = sb("cmp", 1024)
    ones = sb("ones", 128)
    lo, hi, mid, cntp, ge, dd = [sb(n, 16) for n in ("lo", "hi", "mid", "cntp", "ge", "dd")]
    ps = nc.alloc_psum_tensor("r_ps", [128, 512], F32)[:]
    C, T, V, A, G = _mk(nc)
    pe, dve, pool, sp = C.pe, C.dve, C.pool, C.sp
    aff3 = aff.rearrange("p (c e) -> p c e", e=16)
    cmp3 = cmp_.rearrange("p (c e) -> p c e", e=16)
    with nc.Block() as block:
        @block.sync
        def _(se):
            t_l = C.dma(sp, aff, aff_d.rearrange("(p c) e -> p (c e)", p=128), C.newsem())
            t_o = C.done(pool, G.memset(ones, 1.0))
            V.memset(lo, 0.0)
            V.memset(hi, 1.0)
            C.need(dve, t_l)
            C.need(pe, t_o)
            for it in range(34):
                V.tensor_tensor(out=mid, in0=lo, in1=hi, op=ALU.add)
                V.tensor_scalar(out=mid, in0=mid, scalar1=0.5, scalar2=None, op0=ALU.mult)
                V.tensor_tensor(out=cmp3, in0=aff3, in1=mid.unsqueeze(1).to_broadcast([128, 64, 16]), op=ALU.is_ge)
                t_c = C.done(dve, V.tensor_reduce(out=cntp, in_=cmp_.rearrange("p (c e) -> p e c", e=16),
                                                  axis=AX.X, op=ALU.add))
                C.need(pe, t_c)
                t_m = C.done(pe, T.matmul(ps[:, 0:16], lhsT=ones, rhs=cntp, start=True, stop=True))
                C.need(dve, t_m)
                V.tensor_scalar(out=ge, in0=ps[:, 0:16], scalar1=float(CAP) - 0.5, scalar2=None, op0=ALU.is_ge)
                V.tensor_tensor(out=dd, in0=mid, in1=lo, op=ALU.subtract)
                V.tensor_tensor(out=dd, in0=dd, in1=ge, op=ALU.mult)
                V.tensor_tensor(out=lo, in0=lo, in1=dd, op=ALU.add)
                V.tensor_tensor(out=dd, in0=hi, in1=mid, op=ALU.subtract)
                V.tensor_tensor(out=dd, in0=dd, in1=ge, op=ALU.mult)
                V.tensor_tensor(out=hi, in0=mid, in1=dd, op=ALU.add)
            t_f = C.done(dve, V.tensor_tensor(out=cmp3, in0=aff3, in1=lo.unsqueeze(1).to_broadcast([128, 64, 16]),
                                              op=ALU.is_ge))
            C.need(sp, t_f)
            t = C.dma(sp, mask_o.rearrange("(p c) e -> p (c e)", p=128), cmp_, C.newsem())
            C.need(sp, t)
    return nc


NFG = 22
NDB = 8


def build_experts():
    nc = bass.Bass("TRN2", target_bir_lowering=False)
    xs_d = nc.dram_tensor("xsT", [2, 128, KC * CAP], BF16, kind="ExternalInput").ap()
    gs_d = nc.dram_tensor("gsl", [128, 16], F32, kind="ExternalInput").ap()
    wg_d = nc.dram_tensor("wg", [2, NFG, 128, KC * 256], F32, kind="ExternalInput").ap()
    wu_d = nc.dram_tensor("wu", [2, NFG, 128, KC * 256], F32, kind="ExternalInput").ap()
    wd_d = nc.dram_tensor("wd", [2, NDB, 128, FC * 256], F32, kind="ExternalInput").ap()
    y_o = nc.dram_tensor("y", [2, CAP, D], F32, kind="ExternalOutput").ap()

    def sb(name, cols, dt=F32):
        return nc.alloc_sbuf_tensor("x_" + name, [128, cols], dt)[:]
    XS = sb("XS", KC * CAP, BF16).rearrange("p (k t) -> p k t", k=KC)
    HM = sb("HM", FC * CAP, BF16).rearrange("p (f t) -> p f t", f=FC)
    WGU = [sb("wgu%d" % i, 2 * KC * 256, BF16) for i in range(2)]
    WD = [sb("wd%d" % i, FC * 256, BF16) for i in range(2)]
    gs = sb("gs", 16)
    sg = [sb("sg%d" % i, 512) for i in range(2)]
    yst = [sb("yst%d" % i, 256) for i in range(2)]
    ps = [nc.alloc_psum_tensor("x_ps%d" % i, [128, 512], F32)[:] for i in range(8)]
    C, T, V, A, G = _mk(nc)
    pe, act, dve, pool, sp = C.pe, C.act, C.dve, C.pool, C.sp
    with nc.Block() as block:
        @block.sync
        def _(se):
            t_gs = C.dma(sp, gs, gs_d, C.newsem())
            xsem = C.newsem()
            gsem = [C.newsem(), C.newsem()]
            dsem = [C.newsem(), C.newsem()]
            ysem = [C.newsem(), C.newsem()]
            g_rel = [None, None]
            d_rel = [None, None]
            bank_rel = [None] * 8
            sg_rel = [None, None]
            y_rel = [None, None]
            gcnt = 0
            dcnt = 0
            ycnt = 0
            bcnt = 0
            t_gu_last = None
            t_hm = None
            for e in range(2):
                C.need(sp, t_gu_last)
                t_xs = C.dma(sp, XS.rearrange("p k t -> p (k t)"), xs_d[e], xsem)
                C.need(pe, t_xs)
                for fg in range(NFG):
                    slot = gcnt % 2
                    gcnt += 1
                    C.need(pool, g_rel[slot])
                    for half in range(2):
                        C.dma(pool, WGU[slot][:, half * 2048:(half + 1) * 2048],
                              wg_d[e, fg][:, half * 2048:(half + 1) * 2048], gsem[slot])
                    for half in range(2):
                        t_w = C.dma(pool, WGU[slot][:, 4096 + half * 2048:4096 + (half + 1) * 2048],
                                    wu_d[e, fg][:, half * 2048:(half + 1) * 2048], gsem[slot])
                    C.need(pe, t_w)
                    Wg = WGU[slot][:, 0:4096].rearrange("p (k c) -> p k c", k=KC)
                    Wu = WGU[slot][:, 4096:8192].rearrange("p (k c) -> p k c", k=KC)
                    for fj in range(2):
                        fc = fg * 2 + fj
                        base = (fc % 2) * 4
                        for th in range(2):
                            bg, bu = base + th, base + 2 + th
                            C.need(pe, bank_rel[bg])
                            for kc in range(KC):
                                mm = T.matmul(ps[bg], lhsT=Wg[:, kc, fj * 128:(fj + 1) * 128],
                                              rhs=XS[:, kc, th * 512:(th + 1) * 512], start=(kc == 0), stop=(kc == KC - 1))
                            t_g = C.done(pe, mm)
                            C.need(pe, bank_rel[bu])
                            for kc in range(KC):
                                mm = T.matmul(ps[bu], lhsT=Wu[:, kc, fj * 128:(fj + 1) * 128],
                                              rhs=XS[:, kc, th * 512:(th + 1) * 512], start=(kc == 0), stop=(kc == KC - 1))
                            t_u = C.done(pe, mm)
                            C.need(act, t_g)
                            C.need(act, sg_rel[th])
                            t_s = C.done(act, A.activation(out=sg[th], in_=ps[bg], func=AF.Silu))
                            bank_rel[bg] = t_s
                            C.need(dve, [t_s, t_u])
                            C.need(dve, d_rel)
                            t_hm = C.done(dve, V.tensor_tensor(out=HM[:, fc, th * 512:(th + 1) * 512], in0=ps[bu],
                                                               in1=sg[th], op=ALU.mult))
                            bank_rel[bu] = t_hm
                            sg_rel[th] = t_hm
                    g_rel[slot] = t_u
                    t_gu_last = t_u
                C.need(pe, t_hm)
                C.need(dve, t_gs)
                for db in range(NDB):
                    slot = dcnt % 2
                    dcnt += 1
                    C.need(pool, d_rel[slot])
                    for q in range(6):
                        c0 = q * 2048
                        c1 = min(c0 + 2048, FC * 256)
                        t_w = C.dma(pool, WD[slot][:, c0:c1], wd_d[e, db][:, c0:c1], dsem[slot])
                    C.need(pe, t_w)
                    Wd = WD[slot].rearrange("p (f c) -> p f c", f=FC)
                    for tt in range(8):
                        b = bcnt % 4
                        bcnt += 1
                        C.need(pe, bank_rel[b])
                        for fc in range(FC):
                            mm = T.matmul(ps[b][:, 0:256], lhsT=HM[:, fc, tt * 128:(tt + 1) * 128], rhs=Wd[:, fc, :],
                                          start=(fc == 0), stop=(fc == FC - 1))
                        t_d = C.done(pe, mm)
                        yb = ycnt % 2
                        ycnt += 1
                        C.need(dve, t_d)
                        C.need(dve, y_rel[yb])
                        t_y = C.done(dve, V.tensor_scalar(out=yst[yb], in0=ps[b][:, 0:256],
                                                          scalar1=gs[:, e * 8 + tt:e * 8 + tt + 1], scalar2=None, op0=ALU.mult))
                        bank_rel[b] = t_y
                        C.need(sp, t_y)
                        y_rel[yb] = C.dma(sp, y_o[e, tt * 128:(tt + 1) * 128, db * 256:(db + 1) * 256], yst[yb], ysem[yb])
                    d_rel[slot] = t_d
            C.need(sp, y_rel)
    return nc


def build_combine(K):
    nc = bass.Bass("TRN2", target_bir_lowering=False)
    xm_d = nc.dram_tensor("xm", [1024, D], F32, kind="ExternalInput").ap()
    yk_d = nc.dram_tensor("yk", [K, 1024, D], F32, kind="ExternalInput").ap()
    rows_d = nc.dram_tensor("rows", [3, 128, D], F32, kind="ExternalInput").ap()
    out_o = nc.dram_tensor("out", [1024, D], F32, kind="ExternalOutput").ap()

    def sb(name, cols, dt=F32):
        return nc.alloc_sbuf_tensor("c_" + name, [128, cols], dt)[:]
    rows = sb("rows", 3 * D).rearrange("p (r d) -> p r d", r=3)
    xm = sb("xm", D)
    acc = sb("acc", D)
    yb = [sb("yb%d" % i, D) for i in range(4)]
    ot = sb("ot", D)
    stats = sb("stats", 24)
    small = sb("small", 16)
    C, T, V, A, G = _mk(nc)
    act, dve, sp, pool = C.act, C.dve, C.sp, C.pool
    with nc.Block() as block:
        @block.sync
        def _(se):
            t_rows = C.dma(sp, rows, rows_d.rearrange("r p d -> p r d"), C.newsem())
            xsem = C.newsem()
            ysem = [C.newsem() for _ in range(4)]
            osem = C.newsem()
            y_rel = [None] * 4
            x_rel = None
            o_rel = None
            ycnt = 0
            C.need(dve, t_rows)
            for t in range(8):
                C.need(sp, x_rel)
                t_x = C.dma(sp, xm, xm_d[t * 128:(t + 1) * 128, :], xsem)
                for k in range(K):
                    b = ycnt % 4
                    ycnt += 1
                    q_ = sp if b % 2 == 0 else pool
                    C.need(q_, y_rel[b])
                    t_y = C.dma(q_, yb[b], yk_d[k, t * 128:(t + 1) * 128, :], ysem[b])
                    C.need(dve, t_y)
                    if k == 0:
                        ins = V.tensor_copy(out=acc, in_=yb[b])
                    else:
                        ins = V.tensor_tensor(out=acc, in0=acc, in1=yb[b], op=ALU.add)
                    y_rel[b] = C.done(dve, ins)
                C.need(dve, t_x)
                V.tensor_tensor(out=acc, in0=acc, in1=rows[:, 0, :], op=ALU.mult)
                V.scalar_tensor_tensor(out=acc, in0=xm, scalar=ALPHA, in1=acc, op0=ALU.mult, op1=ALU.add)
                for c4 in range(4):
                    V.bn_stats(out=stats[:, c4 * 6:(c4 + 1) * 6], in_=acc[:, c4 * 512:(c4 + 1) * 512])
                V.bn_aggr(out=small[:, 0:2], in_=stats)
                tv = C.done(dve, V.tensor_scalar(out=small[:, 8:9], in0=small[:, 1:2], scalar1=EPS, scalar2=None, op0=ALU.add))
                x_rel = tv
                C.need(act, tv)
                ts = C.done(act, A.sqrt(out=small[:, 9:10], in_=small[:, 8:9]))
                C.need(dve, ts)
                V.reciprocal(out=small[:, 2:3], in_=small[:, 9:10])
                tk = C.done(dve, V.scalar_tensor_tensor(out=small[:, 3:4], in0=small[:, 0:1], scalar=-1.0, in1=small[:, 2:3],
                                                        op0=ALU.mult, op1=ALU.mult))
                C.need(act, tk)
                C.need(act, o_rel)
                ta = C.done(act, A.activation(out=ot, in_=acc, func=AF.Identity, bias=small[:, 3:4], scale=small[:, 2:3]))
                C.need(dve, ta)
                V.tensor_tensor(out=ot, in0=ot, in1=rows[:, 1, :], op=ALU.mult)
                to = C.done(dve, V.tensor_tensor(out=ot, in0=ot, in1=rows[:, 2, :], op=ALU.add))
                C.need(sp, to)
                o_rel = C.dma(sp, out_o[t * 128:(t + 1) * 128, :], ot, osem)
            C.need(sp, o_rel)
    return nc


def build_combine2(NR):
    nc = bass.Bass("TRN2", target_bir_lowering=False)
    xm_d = nc.dram_tensor("xm", [1024, D], F32, kind="ExternalInput").ap()
    yc_d = nc.dram_tensor("yc", [8, NR * 128, D], F32, kind="ExternalInput").ap()
    S_d = nc.dram_tensor("S", [8, NR * 128, 128], F32, kind="ExternalInput").ap()
    rows_d = nc.dram_tensor("rows", [3, 128, D], F32, kind="ExternalInput").ap()
    out_o = nc.dram_tensor("out", [1024, D], F32, kind="ExternalOutput").ap()

    def sb(name, cols, dt=F32):
        return nc.alloc_sbuf_tensor("c_" + name, [128, cols], dt)[:]
    rows = sb("rows", 3 * D).rearrange("p (r d) -> p r d", r=3)
    xm = [sb("xm%d" % i, D) for i in range(2)]
    yb = [[sb("yb%d_%d" % (i, k), D) for k in range(NR)] for i in range(2)]
    Sb = [sb("S%d" % i, NR * 128).rearrange("p (k c) -> p k c", k=NR) for i in range(2)]
    acc = sb("acc", D)
    ot = sb("ot", D)
    stats = sb("stats", 24)
    small = sb("small", 16)
    ps = [nc.alloc_psum_tensor("c_ps%d" % i, [128, 512], F32)[:] for i in range(8)]
    C, T, V, A, G = _mk(nc)
    pe, act, dve, sp, pool = C.pe, C.act, C.dve, C.sp, C.pool
    with nc.Block() as block:
        @block.sync
        def _(se):
            t_rows = C.dma(sp, rows, rows_d.rearrange("r p d -> p r d"), C.newsem())
            lsem = [C.newsem(), C.newsem()]
            ysem = [[C.newsem() for _ in range(NR)] for _ in range(2)]
            osem = C.newsem()
            in_rel = [None, None]
            bank_rel = [None] * 8
            o_rel = None
            C.need(dve, t_rows)
            ltok = {}

            def loads(t):
                b = t % 2
                C.need(sp, in_rel[b])
                C.need(pool, in_rel[b])
                C.dma(sp, xm[b], xm_d[t * 128:(t + 1) * 128, :], lsem[b])
                tl = C.dma(sp, Sb[b], S_d[t].rearrange("(k p) c -> p k c", p=128), lsem[b])
                ty = []
                for k in range(NR):
                    q_ = pool if k % 2 == 0 else sp
                    ty.append(C.dma(q_, yb[b][k], yc_d[t, k * 128:(k + 1) * 128, :], ysem[b][k]))
                ltok[t] = (tl, ty)

            loads(0)
            for t in range(8):
                if t + 1 < 8:
                    loads(t + 1)
                b = t % 2
                tl, ty = ltok[t]
                C.need(pe, tl)
                for dg in range(4):
                    bk = b * 4 + dg
                    C.need(pe, bank_rel[bk])
                    for k in range(NR):
                        C.need(pe, ty[k])
                        mm = T.matmul(ps[bk], lhsT=Sb[b][:, k, :], rhs=yb[b][k][:, dg * 512:(dg + 1) * 512],
                                      start=(k == 0), stop=(k == NR - 1))
                t_mm = C.done(pe, mm)
                C.need(dve, t_mm)
                C.need(dve, tl)
                for dg in range(4):
                    ins = V.tensor_tensor(out=acc[:, dg * 512:(dg + 1) * 512], in0=ps[b * 4 + dg],
                                          in1=rows[:, 0, dg * 512:(dg + 1) * 512], op=ALU.mult)
                t_ev = C.done(dve, ins)
                for dg in range(4):
                    bank_rel[b * 4 + dg] = t_ev
                t_x = C.done(dve, V.scalar_tensor_tensor(out=acc, in0=xm[b], scalar=ALPHA, in1=acc, op0=ALU.mult, op1=ALU.add))
                in_rel[b] = [t_mm, t_x]
                for c4 in range(4):
                    V.bn_stats(out=stats[:, c4 * 6:(c4 + 1) * 6], in_=acc[:, c4 * 512:(c4 + 1) * 512])
                V.bn_aggr(out=small[:, 0:2], in_=stats)
                tv = C.done(dve, V.tensor_scalar(out=small[:, 8:9], in0=small[:, 1:2], scalar1=EPS, scalar2=None, op0=ALU.add))
                C.need(act, tv)
                ts = C.done(act, A.sqrt(out=small[:, 9:10], in_=small[:, 8:9]))
                C.need(dve, ts)
                V.reciprocal(out=small[:, 2:3], in_=small[:, 9:10])
                tk = C.done(dve, V.scalar_tensor_tensor(out=small[:, 3:4], in0=small[:, 0:1], scalar=-1.0, in1=small[:, 2:3],
                                                        op0=ALU.mult, op1=ALU.mult))
                C.need(act, tk)
                C.need(act, o_rel)
                ta = C.done(act, A.activation(out=ot, in_=acc, func=AF.Identity, bias=small[:, 3:4], scale=small[:, 2:3]))
                C.need(dve, ta)
                V.tensor_tensor(out=ot, in0=ot, in1=rows[:, 1, :], op=ALU.mult)
                to = C.done(dve, V.tensor_tensor(out=ot, in0=ot, in1=rows[:, 2, :], op=ALU.add))
                C.need(sp, to)
                o_rel = C.dma(sp, out_o[t * 128:(t + 1) * 128, :], ot, osem)
            C.need(sp, o_rel)
    return nc


def _run(nc, maps):
    return run_bass_kernel_spmd(nc, maps, core_ids=list(range(NCORES))).results


def kernel(**inputs):
    f = np.float32
    cores = list(range(NCORES))
    mod, mod_c = run_mod(inputs)
    nc, _ = build()
    res = _run(nc, host_inputs(inputs, mod, mod_c))
    x_mid = np.concatenate([np.asarray(r["x_mid"]) for r in res], 0)
    h2b = np.concatenate([np.asarray(r["h2b"]) for r in res], 0)
    aff = np.ascontiguousarray(np.concatenate([np.asarray(r["aff"], f) for r in res], 0))
    res = _run(build_route(), [{"aff": aff} for _ in cores])
    mask = np.asarray(res[0]["mask"]) > 0.5
    idx = np.zeros((16, CAP), np.int64)
    valid = np.zeros((16, CAP), bool)
    for e in range(16):
        ii = np.nonzero(mask[:, e])[0][:CAP]
        idx[e, :len(ii)] = ii
        valid[e, :len(ii)] = True
    w_gate, w_up, w_down = inputs["w_gate"], inputs["w_up"], inputs["w_down"]
    maps = []
    for i in cores:
        m = {}
        xs = []
        gsl = np.zeros((128, 16), f)
        for j in range(2):
            e = 2 * i + j
            rows = h2b[idx[e]]
            xs.append(np.ascontiguousarray(rows.T.reshape(KC, 128, CAP).transpose(1, 0, 2)).reshape(128, KC * CAP))
            ge = np.where(valid[e], aff[idx[e], e], 0).astype(f)
            gsl[:, j * 8:(j + 1) * 8] = ge.reshape(8, 128).T
        m["xsT"] = np.stack(xs, 0)
        m["gsl"] = gsl
        for nm, w in (("wg", w_gate), ("wu", w_up)):
            m[nm] = np.stack([np.ascontiguousarray(
                np.asarray(w[0, 2 * i + j], f).reshape(KC, 128, NFG, 256).transpose(2, 1, 0, 3)).reshape(NFG, 128, KC * 256)
                for j in range(2)], 0)
        m["wd"] = np.stack([np.ascontiguousarray(
            np.asarray(w_down[0, 2 * i + j], f).reshape(FC, 128, NDB, 256).transpose(2, 1, 0, 3)).reshape(NDB, 128, FC * 256)
            for j in range(2)], 0)
        maps.append(m)
    res = _run(build_experts(), maps)
    del maps
    y_all = np.concatenate([np.asarray(r["y"], f) for r in res], 0)
    pe_ = np.repeat(np.arange(16), CAP)[valid.reshape(-1)]
    ps_ = np.tile(np.arange(CAP), 16)[valid.reshape(-1)]
    pt_ = idx.reshape(-1)[valid.reshape(-1)]
    order = np.argsort(pt_, kind="stable")
    pe_, ps_, pt_ = pe_[order], ps_[order], pt_[order]
    bounds = np.searchsorted(pt_, np.arange(0, 8192 + 1, 128))
    NR = max(1, int(-(-int(np.max(np.diff(bounds))) // 128)))
    rows3 = np.ascontiguousarray(np.stack([np.broadcast_to(v[None], (128, D)) for v in
                                           (mod[5], np.asarray(inputs["ln2_g"], f)[0], np.asarray(inputs["ln2_b"], f)[0])], 0))
    maps = []
    for i in cores:
        yc = np.zeros((8, NR * 128, D), f)
        S = np.zeros((8, NR * 128, 128), f)
        for t_ in range(8):
            lo_, hi_ = bounds[8 * i + t_], bounds[8 * i + t_ + 1]
            n_ = hi_ - lo_
            yc[t_, :n_] = y_all[pe_[lo_:hi_], ps_[lo_:hi_]]
            S[t_, np.arange(n_), pt_[lo_:hi_] - (1024 * i + 128 * t_)] = 1.0
        maps.append({"xm": np.ascontiguousarray(x_mid[1024 * i:1024 * (i + 1)]), "yc": yc, "S": S, "rows": rows3})
    res = _run(build_combine2(NR), maps)
    out = np.concatenate([np.asarray(r["out"], f) for r in res], 0)
    return out.reshape(1, 8192, D).astype(f)
```

```python
import numpy as np
import concourse.bass as bass
import concourse.mybir as mybir
from concourse.bass_utils import run_bass_kernel_spmd

F32 = mybir.dt.float32
BF16 = mybir.dt.bfloat16
I32 = mybir.dt.int32
AF = mybir.ActivationFunctionType
ALU = mybir.AluOpType
AX = mybir.AxisListType

NCORES = 8
D = 2048
KC = 16
NT = 10
TOK = 1280
DFF = 5632
FC = 44
CAP = 1024
ALPHA = 2.0 ** 0.25
EPS = 1e-5


class Eng:
    def __init__(self, nc, e, name):
        self.e = e
        self.name = name
        self.sem = nc.alloc_semaphore("s_" + name)
        self.n = 0
        self.seen = {}
        self.serial = False


class Ser:
    def __init__(self, eng):
        self._eng = eng
        eng.serial = True

    def __getattr__(self, name):
        eng = self._eng
        fn = getattr(eng.e, name)

        def call(*a, **k):
            if eng.n > 0:
                eng.e.wait_ge(eng.sem, eng.n)
            ins = fn(*a, **k)
            eng.n += 1
            ins.then_inc(eng.sem, 1)
            return ins
        return call


class Ctx:
    def __init__(self, nc):
        self.nc = nc
        self.pe = Eng(nc, nc.tensor, "pe")
        self.act = Eng(nc, nc.scalar, "act")
        self.dve = Eng(nc, nc.vector, "dve")
        self.pool = Eng(nc, nc.gpsimd, "pool")
        self.sp = Eng(nc, nc.sync, "sp")
        self.nsem = 0

    def done(self, eng, ins):
        if eng.serial:
            return ("e", eng, eng.n)
        eng.n += 1
        ins.then_inc(eng.sem, 1)
        return ("e", eng, eng.n)

    def newsem(self):
        self.nsem += 1
        return [self.nc.alloc_semaphore("d%d" % self.nsem), 0]

    def dma(self, eng, out, in_, ds, **kw):
        ins = eng.e.dma_start(out=out, in_=in_, **kw)
        ds[1] += 16
        ins.then_inc(ds[0], 16)
        return ("d", ds[0], ds[1])

    def need(self, eng, tok):
        if tok is None:
            return
        if isinstance(tok, list):
            for t in tok:
                self.need(eng, t)
            return
        if tok[0] == "e":
            src, n = tok[1], tok[2]
            if src is eng:
                return
            key = src.name
        else:
            key, n = id(tok[1]), tok[2]
        if eng.seen.get(key, 0) >= n:
            return
        eng.seen[key] = n
        eng.e.wait_ge(tok[1].sem if tok[0] == "e" else tok[1], n)


def build(debug=None):
    nc = bass.Bass("TRN2", target_bir_lowering=False)

    def din(name, shape, dt=F32):
        return nc.dram_tensor(name, list(shape), dt, kind="ExternalInput").ap()

    def dout(name, shape, dt=F32):
        return nc.dram_tensor(name, list(shape), dt, kind="ExternalOutput").ap()

    xh = din("xh", [TOK, D])
    ctxd = din("ctx", [256, D])
    win_d = din("win", [7, 128, KC * 512])
    pcols_d = din("pcols", [128, 16 + 8 * 31 + 24 + 2])
    bqkv_d = din("bqkv", [128, 1536])
    rope_d = din("rope", [128, NT, 128])
    masks_d = din("masks", [128, 2, 128])
    sink_d = din("sink", [128, 8])
    wout_d = din("wout", [4, 128, KC * 512])
    rows_d = din("rows", [5, 128, D])
    wr_d = din("wr", [128, KC * 16])
    modT_d = din("modT", [128, 192])
    mrows_d = din("mrows", [3, 128, D])

    dbg = {}

    def dbg_out(name, shape, dt=F32):
        dbg[name] = dout("dbg_" + name, shape, dt)
        return dbg[name]

    mixd = nc.dram_tensor("mixd", [1024, D], F32)

    def sb(name, cols, dt=F32):
        return nc.alloc_sbuf_tensor("sb_" + name, [128, cols], dt)[:]

    ident_b = sb("ident_b", 128, BF16)
    ident_f = sb("ident_f", 128, F32)
    pcols = sb("pcols", 16 + 8 * 31 + 24 + 2)
    bconv = pcols[:, 0:16]
    wdw = pcols[:, 16:16 + 248].rearrange("p (c k) -> p c k", k=31)
    bdw = pcols[:, 264:272]
    clg = pcols[:, 272:280]
    clb = pcols[:, 280:288]
    hval = pcols[:, 288:290]
    modT = sb("modT", 2 * 96)
    sc1p = sb("sc1p", 32)
    sc2p = sb("sc2p", 16)
    ones_f = sb("ones_f", 128, F32)
    ones_b = sb("ones_b", 128, BF16)
    small = sb("small", 64)
    stats = sb("stats", 4 * 6 * 2)
    bqkv = sb("bqkv", 1536)
    rope_t = sb("rope_t", NT * 128)
    masks_f = sb("masks_f", 256)
    masks = sb("masks", 4 * 128, BF16)
    sinke = sb("sinke", 8)
    wr = sb("wr", KC * 16)

    RA = sb("RA", 16384)
    RB = sb("RB", 8448)
    RC = sb("RC", 8192)
    RE = sb("RE", 7680)
    RF = sb("RF", 6144)

    hT = RA[:, 0:10240].bitcast(BF16).rearrange("p (k t) -> p k t", k=KC)
    hcT = RA[:, 10240:12288].bitcast(BF16).rearrange("p (k t) -> p k t", k=KC)
    xstage = [RA[:, 12288:14336], RA[:, 14336:16384]]
    xn_b = [RF[:, 0:1024].bitcast(BF16), RF[:, 1024:2048].bitcast(BF16)]
    wslot = [RC[:, 0:4096].bitcast(BF16).rearrange("p (k c) -> p k c", k=KC),
             RC[:, 4096:8192].bitcast(BF16).rearrange("p (k c) -> p k c", k=KC)]
    wmod_sb = RC.rearrange("p (k c) -> p k c", k=KC)
    uT = RB.rearrange("p (c t) -> p c t", c=8)
    qT = RE[:, 0:4096].bitcast(BF16).rearrange("p (h t) -> p h t", h=8)
    kT = RE[:, 4096:5376].bitcast(BF16).rearrange("p (h t) -> p h t", h=2)
    vtok = RE[:, 5376:6656].bitcast(BF16).rearrange("p (t c) -> p t c", t=NT)
    kcT = RE[:, 6656:6912].bitcast(BF16).rearrange("p (h t) -> p h t", h=2)
    vctok = RE[:, 6912:7168].bitcast(BF16).rearrange("p (t c) -> p t c", t=2)
    qtok = RF[:, 2048:2560]
    ropeA = RF[:, 2560:3072]
    ropeB = RF[:, 3072:3584]
    qrb = RF[:, 3584:3840].bitcast(BF16)
    gsig = RF[:, 3840:4224]

    ps = [nc.alloc_psum_tensor("ps%d" % i, [128, 512], F32)[:] for i in range(6)]
    psb = [nc.alloc_psum_tensor("psb%d" % i, [128, 1024], BF16)[:] for i in range(2)]

    C = Ctx(nc)
    pe, act, dve, pool, sp = C.pe, C.act, C.dve, C.pool, C.sp
    T, S = nc.tensor, nc.sync
    V, A, G = Ser(dve), Ser(act), Ser(pool)

    with nc.Block() as block:
        @block.sync
        def _(sync_engine):
            ld0 = C.newsem()
            C.dma(sp, pcols, pcols_d, ld0)
            C.dma(sp, bqkv, bqkv_d, ld0)
            C.dma(sp, rope_t, rope_d.rearrange("p t c -> p (t c)"), ld0)
            C.dma(sp, masks_f, masks_d.rearrange("p a c -> p (a c)"), ld0)
            C.dma(sp, sinke, sink_d, ld0)
            t_ld0 = C.dma(sp, wr, wr_d, ld0)

            G.memset(ident_b, 0.0)
            G.affine_select(out=ident_b, in_=ident_b, pattern=[[-1, 128]], compare_op=ALU.not_equal,
                            fill=1.0, base=0, channel_multiplier=1)
            G.memset(ones_f, 1.0)
            G.memset(ones_b, 1.0)
            G.memset(ident_f, 0.0)
            t_id = C.done(pool, G.affine_select(out=ident_f, in_=ident_f, pattern=[[-1, 128]],
                                                compare_op=ALU.not_equal, fill=1.0, base=0,
                                                channel_multiplier=1))

            t_mt = C.dma(sp, modT, modT_d, ld0)
            C.need(dve, t_mt)
            V.tensor_scalar(out=sc1p[:, 0:16], in0=modT[:, 16:32], scalar1=1.0, scalar2=None, op0=ALU.add)
            V.tensor_scalar(out=sc1p[:, 16:32], in0=modT[:, 96 + 16:96 + 32], scalar1=1.0, scalar2=None, op0=ALU.add)
            t_mod = C.done(dve, V.tensor_scalar(out=sc2p, in0=modT[:, 64:80], scalar1=1.0, scalar2=None, op0=ALU.add))
            sh1 = [modT[:, 0:16], modT[:, 96:96 + 16]]
            sh2 = modT[:, 48:64]

            if debug == "ln0":
                o = dbg_out("sc", [128, 32])
                C.need(sp, t_mod)
                C.need(sp, t_id)
                t = C.dma(sp, o, sc1p, C.newsem())
                C.need(sp, t)
                return
            wsem = [C.newsem(), C.newsem()]
            wtok = {}
            wrel = {}

            def issue_w(g, src):
                slot = g % 2
                if g - 2 in wrel:
                    C.need(pool, wrel[g - 2])
                for q in range(4):
                    tk = C.dma(pool, wslot[slot].rearrange("p k c -> p (k c)")[:, q * 2048:(q + 1) * 2048],
                               src[:, q * 2048:(q + 1) * 2048], wsem[slot])
                wtok[g] = tk

            if debug != "ln1":
                issue_w(0, win_d[0])
                issue_w(1, win_d[1])

            xsem = [C.newsem(), C.newsem()]
            x_rel = [None, None]
            xn_rel = [None, None]
            tr_rel = [None, None]
            t_h = None
            for it in range(NT + 2):
                s = it % 2
                isctx = it >= NT
                src = ctxd[(it - NT) * 128:(it - NT + 1) * 128, :] if isctx else xh[it * 128:(it + 1) * 128, :]
                C.need(sp, x_rel[s])
                t_x = C.dma(sp, xstage[s], src, xsem[s])
                C.need(dve, t_x)
                st = stats[:, 0:24].rearrange("p (c s) -> p c s", s=6)
                for c4 in range(4):
                    V.bn_stats(out=st[:, c4, :], in_=xstage[s][:, c4 * 512:(c4 + 1) * 512])
                mv = small[:, 0:2]
                V.bn_aggr(out=mv, in_=stats[:, 0:24])
                rstd = small[:, 2 + 2 * s:3 + 2 * s]
                nmr = small[:, 3 + 2 * s:4 + 2 * s]
                C.need(dve, xn_rel[s])
                t_v = C.done(dve, V.tensor_scalar(out=small[:, 8:9], in0=mv[:, 1:2], scalar1=EPS, scalar2=None, op0=ALU.add))
                C.need(act, t_v)
                t_sq = C.done(act, A.sqrt(out=small[:, 9:10], in_=small[:, 8:9]))
                C.need(dve, t_sq)
                V.reciprocal(out=rstd, in_=small[:, 9:10])
                t_st = C.done(dve, V.scalar_tensor_tensor(out=nmr, in0=mv[:, 0:1], scalar=-1.0, in1=rstd,
                                                          op0=ALU.mult, op1=ALU.mult))
                C.need(act, t_st)
                C.need(act, xn_rel[s])
                t_xn = C.done(act, A.activation(out=xn_b[s], in_=xstage[s], func=AF.Identity, bias=nmr, scale=rstd))
                x_rel[s] = t_xn
                C.need(pe, t_xn)
                C.need(pe, t_id)
                for half in range(2):
                    pst = psb[half]
                    C.need(pe, tr_rel[half])
                    for k8 in range(8):
                        kc = half * 8 + k8
                        mm = T.transpose(pst[:, k8 * 128:(k8 + 1) * 128], xn_b[s][:, kc * 128:(kc + 1) * 128], ident_b)
                    t_tr = C.done(pe, mm)
                    if half == 1:
                        xn_rel[s] = t_tr
                    dst = hcT if isctx else hT
                    t0 = (it - NT) * 128 if isctx else it * 128
                    j = 1 if isctx else 0
                    C.need(dve, t_tr)
                    C.need(act, t_tr)
                    C.need(dve, t_mod)
                    C.need(act, t_mod)
                    for k8 in range(8):
                        kc = half * 8 + k8
                        o_ = dst[:, kc, t0:t0 + 128]
                        i_ = pst[:, k8 * 128:(k8 + 1) * 128]
                        insd = V.tensor_scalar(out=o_, in0=i_, scalar1=sc1p[:, j * 16 + kc:j * 16 + kc + 1],
                                               scalar2=sh1[j][:, kc:kc + 1], op0=ALU.mult, op1=ALU.add)
                    td = C.done(dve, insd)
                    ta = td
                    tr_rel[half] = [td, ta]
                    t_h = [td, ta]

            if debug in ("h", "ln1"):
                o = dbg_out("hT", [128, 6 * TOK], F32)
                C.need(dve, t_h)
                t_c = C.done(dve, V.tensor_copy(out=RB[:, 0:6 * TOK], in_=hT.rearrange("p k t -> p (k t)")[:, 0:6 * TOK]))
                C.need(sp, t_c)
                t = C.dma(sp, o, RB[:, 0:6 * TOK], C.newsem())
                C.need(sp, t)
                return

            C.need(pe, t_h)
            C.need(act, t_ld0)
            C.need(dve, t_ld0)
            bank_rel = {}

            def getbank(b, eng):
                C.need(eng, bank_rel.get(b))

            TP = 352
            t_u = None
            for g in range(4):
                if g >= 2:
                    pass
                C.need(pe, wtok[g])
                W = wslot[g % 2]
                for cj in range(2):
                    cc = 2 * g + cj
                    for tp in range(3):
                        tk0 = 112 + tp * TP
                        bg, bv = (tp % 2) * 2, 1 + (tp % 2) * 2
                        getbank(bg, pe)
                        for kc in range(KC):
                            mm = T.matmul(ps[bg][:, 0:TP], lhsT=W[:, kc, 256 + cj * 128:256 + (cj + 1) * 128],
                                          rhs=hT[:, kc, tk0:tk0 + TP], start=(kc == 0), stop=(kc == KC - 1))
                        t_g = C.done(pe, mm)
                        getbank(bv, pe)
                        for kc in range(KC):
                            mm = T.matmul(ps[bv][:, 0:TP], lhsT=W[:, kc, cj * 128:(cj + 1) * 128],
                                          rhs=hT[:, kc, tk0:tk0 + TP], start=(kc == 0), stop=(kc == KC - 1))
                        t_v = C.done(pe, mm)
                        C.need(act, t_g)
                        C.need(act, bank_rel.get(("gsig", tp % 2)))
                        gs_ = gsig if tp % 2 == 0 else ropeA[:, 0:384]
                        t_s = C.done(act, A.activation(out=gs_[:, 0:TP], in_=ps[bg][:, 0:TP], func=AF.Sigmoid,
                                                       bias=bconv[:, 8 + cc:9 + cc]))
                        bank_rel[bg] = t_s
                        C.need(dve, t_s)
                        C.need(dve, t_v)
                        t_u = C.done(dve, V.scalar_tensor_tensor(out=uT[:, cc, tp * TP:(tp + 1) * TP], in0=ps[bv][:, 0:TP],
                                                                 scalar=bconv[:, cc:cc + 1], in1=gs_[:, 0:TP],
                                                                 op0=ALU.add, op1=ALU.mult))
                        bank_rel[bv] = t_u
                        bank_rel[("gsig", tp % 2)] = t_u
                wrel[g] = C.done(pe, T.matmul(ps[bv][:, 0:1], lhsT=W[:, 0, 0:128], rhs=hT[:, 0, 0:1], start=True, stop=True)) if False else t_v
                if g + 2 < 7:
                    issue_w(g + 2, win_d[g + 2])

            if debug == "u":
                o = dbg_out("uT", [128, 8 * 1056])
                C.need(sp, t_u)
                t = C.dma(sp, o, RB, C.newsem())
                C.need(sp, t)
                return

            def rope(dst_b, src_f, nh, tile):
                for a in range(2):
                    Xa = src_f.rearrange("p (h a f) -> p h a f", a=2, f=64)[:, :, a, :]
                    Oa = dst_b.rearrange("p (h a f) -> p h a f", a=2, f=64)[:, :, a, :]
                    Aa = ropeA[:, 0:nh * 64].rearrange("p (h f) -> p h f", f=64)
                    Ba = ropeB[:, 0:nh * 64].rearrange("p (h f) -> p h f", f=64)
                    cs = rope_t[:, tile * 128 + a * 32:tile * 128 + a * 32 + 32]
                    sn = rope_t[:, tile * 128 + 64 + a * 32:tile * 128 + 64 + a * 32 + 32]
                    for hf in range(2):
                        V.tensor_tensor(out=Aa[:, :, hf * 32:(hf + 1) * 32], in0=Xa[:, :, hf * 32:(hf + 1) * 32],
                                        in1=cs.unsqueeze(1).to_broadcast([128, nh, 32]), op=ALU.mult)
                        V.tensor_tensor(out=Ba[:, :, hf * 32:(hf + 1) * 32], in0=Xa[:, :, (1 - hf) * 32:(2 - hf) * 32],
                                        in1=sn.unsqueeze(1).to_broadcast([128, nh, 32]), op=ALU.mult)
                    V.tensor_tensor(out=Oa[:, :, 0:32], in0=Aa[:, :, 0:32], in1=Ba[:, :, 0:32], op=ALU.subtract)
                    last = V.tensor_tensor(out=Oa[:, :, 32:64], in0=Aa[:, :, 32:64], in1=Ba[:, :, 32:64], op=ALU.add)
                return last

            qrb_rel = None
            trb_rel = None
            t_last = None
            for g in (4, 5, 6):
                C.need(pe, wtok[g])
                W = wslot[g % 2]
                tiles = list(range(1, 9)) if g < 6 else list(range(NT + 2))
                mmtok = {}

                def emit_mm(ti):
                    tl = tiles[ti]
                    isctx = tl >= NT
                    lh = hcT[:, :, (tl - NT) * 128:(tl - NT + 1) * 128] if isctx else hT[:, :, tl * 128:(tl + 1) * 128]
                    b = 4 + (ti % 2)
                    getbank(b, pe)
                    for kc in range(KC):
                        mm = T.matmul(ps[b], lhsT=lh[:, kc, :], rhs=W[:, kc, :], start=(kc == 0), stop=(kc == KC - 1))
                    mmtok[ti] = C.done(pe, mm)

                emit_mm(0)
                for ti, tl in enumerate(tiles):
                    if ti + 1 < len(tiles):
                        emit_mm(ti + 1)
                    isctx = tl >= NT
                    b = 4 + (ti % 2)
                    t_mm = mmtok[ti]
                    C.need(dve, t_mm)
                    boff = (g - 4) * 512
                    C.need(dve, qrb_rel)
                    if g < 6:
                        V.tensor_tensor(out=qtok, in0=ps[b], in1=bqkv[:, boff:boff + 512], op=ALU.add)
                        t_r = C.done(dve, rope(qrb, qtok, 4, tl))
                        bank_rel[b] = t_r
                        ntr = 4
                    else:
                        V.tensor_tensor(out=qtok, in0=ps[b], in1=bqkv[:, boff:boff + 512], op=ALU.add)
                        vdst = vctok[:, tl - NT, :] if isctx else vtok[:, tl, :]
                        if isctx:
                            V.tensor_copy(out=qrb[:, 0:256], in_=qtok[:, 0:256])
                            t_r = C.done(dve, V.tensor_copy(out=vdst, in_=qtok[:, 256:512]))
                        else:
                            V.tensor_copy(out=vdst, in_=qtok[:, 256:512])
                            t_r = C.done(dve, rope(qrb[:, 0:256], qtok[:, 0:256], 2, tl))
                        bank_rel[b] = t_r
                        ntr = 2
                    C.need(pe, t_r)
                    C.need(pe, trb_rel)
                    C.need(pe, tr_rel[0])
                    pst = psb[0]
                    for hh in range(ntr):
                        mm = T.transpose(pst[:, hh * 128:(hh + 1) * 128], qrb[:, hh * 128:(hh + 1) * 128], ident_b)
                    t_tr = C.done(pe, mm)
                    qrb_rel = t_tr
                    C.need(act, t_tr)
                    if g < 6:
                        o_ = qT[:, (g - 4) * 4:(g - 4) * 4 + 4, (tl - 1) * 128:tl * 128]
                    elif isctx:
                        o_ = kcT[:, :, (tl - NT) * 128:(tl - NT + 1) * 128]
                    else:
                        o_ = kT[:, :, tl * 128:(tl + 1) * 128]
                    t_last = C.done(act, A.activation(out=o_, in_=pst[:, 0:ntr * 128].rearrange("p (h t) -> p h t", t=128),
                                                      func=AF.Copy))
                    trb_rel = t_last
                t_mm = mmtok[len(tiles) - 1]
                wrel[g] = t_tr
                if g + 2 < 7:
                    issue_w(g + 2, win_d[g + 2])

            if debug == "qkv":
                o1 = dbg_out("qT", [128, 8 * 1024], BF16)
                o2 = dbg_out("kT", [128, 2 * 1280], BF16)
                o3 = dbg_out("vtok", [128, NT * 256], BF16)
                o4 = dbg_out("kcT", [128, 512], BF16)
                C.need(sp, t_last)
                C.need(sp, t_r)
                ds_ = C.newsem()
                C.dma(sp, o1, qT.rearrange("p h t -> p (h t)"), ds_)
                C.dma(sp, o2, kT.rearrange("p h t -> p (h t)"), ds_)
                C.dma(sp, o3, vtok.rearrange("p t c -> p (t c)"), ds_)
                t = C.dma(sp, o4, kcT.rearrange("p h t -> p (h t)"), ds_)
                C.need(sp, t)
                return
            cv = RA[:, 0:8192].rearrange("p (c t) -> p c t", c=8)
            aT = RA[:, 8192:16384].bitcast(BF16).rearrange("p (k t) -> p k t", k=KC)
            C.need(dve, [t_last, t_mm, t_r])
            C.need(pool, [t_last, t_mm, t_r])
            V.tensor_scalar(out=uT[:, :, 0:16], in0=uT[:, :, 0:16], scalar1=hval[:, 0:1], scalar2=None, op0=ALU.mult)
            t_hm = C.done(dve, V.tensor_scalar(out=uT[:, :, 1040:1056], in0=uT[:, :, 1040:1056],
                                               scalar1=hval[:, 1:2], scalar2=None, op0=ALU.mult))
            C.need(pool, t_hm)
            t_cv = []
            uTb = RC[:, 2560:6784].bitcast(BF16).rearrange("p (c t) -> p c t", c=8)
            dgb = [RF[:, 0:1984].bitcast(BF16).rearrange("p (k c) -> p k c", k=31),
                   RF[:, 3584:5568].bitcast(BF16).rearrange("p (k c) -> p k c", k=31)]
            t_ub = C.done(dve, V.tensor_copy(out=uTb, in_=uT))
            dg_rel = [None, None]
            cb_rel = [None, None]

            def taps_pe(cc):
                dg = dgb[cc % 2]
                C.need(dve, dg_rel[cc % 2])
                t_dg = C.done(dve, V.tensor_tensor(out=dg, in0=ident_b.unsqueeze(1).to_broadcast([128, 31, 128]),
                                                   in1=wdw[:, cc, :].unsqueeze(2).to_broadcast([128, 31, 128]), op=ALU.mult))
                C.need(pe, [t_dg, t_ub])
                for th in range(2):
                    C.need(pe, cb_rel[th])
                    for k in range(31):
                        mm = T.matmul(ps[4 + th], lhsT=dg[:, k, :], rhs=uTb[:, cc, k + 1 + th * 512:k + 1 + th * 512 + 512],
                                      start=(k == 0), stop=(k == 30))
                    t_m = C.done(pe, mm)
                    C.need(act, t_m)
                    t_e = C.done(act, A.activation(out=cv[:, cc, th * 512:(th + 1) * 512], in_=ps[4 + th], func=AF.Identity,
                                                   bias=bdw[:, cc:cc + 1]))
                    cb_rel[th] = t_e
                    t_cv.append(t_e)
                dg_rel[cc % 2] = t_m

            taps_dve = taps_pe

            V.tensor_copy(out=masks[:, 0:256], in_=masks_f)
            V.tensor_scalar(out=masks[:, 256:384], in0=masks_f[:, 0:128], scalar1=hval[:, 0:1], scalar2=None, op0=ALU.mult)
            t_mk = C.done(dve, V.tensor_scalar(out=masks[:, 384:512], in0=masks_f[:, 128:256], scalar1=hval[:, 1:2],
                                               scalar2=None, op0=ALU.mult))
            t_sk = C.done(act, A.activation(out=sinke, in_=sinke, func=AF.Exp))
            pbuf = [RC[:, i * 256:(i + 1) * 256].bitcast(BF16) for i in range(10)]
            dtmp = RF[:, 3072:3584]
            SCALE = 128.0 ** -0.5
            C.need(pe, [t_mk, t_sk])
            s_rel = [None, None]
            p_rel = [None] * 10
            od_rel = [None, None]
            itn = 0
            scnt = 0
            t_f = None
            t_pv = None
            next_tap = 0

            def hq(ap):
                return ap.rearrange("p (h q) -> p h q", h=4)

            for t in range(1, 9):
                for kv in range(2):
                    par = itn % 2
                    tiles = [("c", 0), ("c", 1), ("w", t - 1), ("w", t), ("w", t + 1)]
                    ptoks = []
                    for i, (kind, idx) in enumerate(tiles):
                        keyT = kcT[:, kv, idx * 128:(idx + 1) * 128] if kind == "c" else kT[:, kv, idx * 128:(idx + 1) * 128]
                        sbk = scnt % 2
                        scnt += 1
                        C.need(pe, s_rel[sbk])
                        t_s = C.done(pe, T.matmul(ps[sbk], lhsT=keyT, rhs=qT[:, 4 * kv:4 * kv + 4, (t - 1) * 128:t * 128],
                                                  start=True, stop=True))
                        pb = pbuf[par * 5 + i]
                        C.need(act, t_s)
                        C.need(act, p_rel[par * 5 + i])
                        t_e = C.done(act, A.activation(out=pb, in_=ps[sbk], func=AF.Exp, scale=SCALE))
                        s_rel[sbk] = t_e
                        if kind == "w" and idx != t:
                            if idx == t - 1:
                                mk = masks[:, 256:384] if t == 1 else masks[:, 0:128]
                            else:
                                mk = masks[:, 384:512] if t == 8 else masks[:, 128:256]
                            C.need(dve, t_e)
                            t_e = C.done(dve, V.tensor_tensor(out=hq(pb), in0=hq(pb),
                                                              in1=mk.unsqueeze(1).to_broadcast([128, 4, 128]), op=ALU.mult))
                        ptoks.append(t_e)
                    C.need(pe, od_rel[0])
                    for i, (kind, idx) in enumerate(tiles):
                        vt = vctok[:, idx, kv * 128:(kv + 1) * 128] if kind == "c" else vtok[:, idx, kv * 128:(kv + 1) * 128]
                        C.need(pe, ptoks[i])
                        T.matmul(ps[2], lhsT=vt, rhs=pbuf[par * 5 + i], start=(i == 0), stop=(i == 4))
                    for i in range(5):
                        mm = T.matmul(ps[3], lhsT=ones_b, rhs=pbuf[par * 5 + i], start=(i == 0), stop=(i == 4))
                    t_pv = C.done(pe, mm)
                    for i in range(5):
                        p_rel[par * 5 + i] = t_pv
                    C.need(dve, t_pv)
                    C.need(dve, t_sk)
                    V.tensor_tensor(out=hq(dtmp), in0=hq(ps[3]),
                                    in1=sinke[:, 4 * kv:4 * kv + 4].unsqueeze(2).to_broadcast([128, 4, 128]), op=ALU.add)
                    V.reciprocal(out=dtmp, in_=dtmp)
                    t_f = C.done(dve, V.tensor_tensor(out=aT[:, 8 + 4 * kv:8 + 4 * kv + 4, (t - 1) * 128:t * 128],
                                                      in0=hq(ps[2]), in1=hq(dtmp), op=ALU.mult))
                    od_rel[0] = t_f
                    itn += 1
                    if itn % 2 == 0 and next_tap < 8:
                        taps_dve(next_tap)
                        next_tap += 1
            while next_tap < 8:
                taps_dve(next_tap)
                next_tap += 1

            if debug == "attn":
                o = dbg_out("attn", [128, 8 * 1024], BF16)
                C.need(sp, t_f)
                t = C.dma(sp, o, aT[:, 8:16, :].rearrange("p k t -> p (k t)"), C.newsem())
                C.need(sp, t)
                return

            sq = RB[:, 0:8192].rearrange("p (c t) -> p c t", c=8)
            C.need(act, t_cv)
            for cc in range(8):
                ins = A.activation(out=sq[:, cc, :], in_=cv[:, cc, :], func=AF.Square)
            t_sq = C.done(act, ins)
            C.need(pe, t_sq)
            C.need(pe, t_cv)
            C.need(pe, t_id)
            C.need(pe, [t_f] + s_rel)
            for th in range(2):
                for cc in range(8):
                    T.matmul(ps[th], lhsT=ones_f, rhs=cv[:, cc, th * 512:(th + 1) * 512], start=(cc == 0), stop=(cc == 7))
                for cc in range(8):
                    mm = T.matmul(ps[2 + th], lhsT=ones_f, rhs=sq[:, cc, th * 512:(th + 1) * 512], start=(cc == 0), stop=(cc == 7))
            t_stat = C.done(pe, mm)
            cmean = RF[:, 0:1024]
            crstd = RF[:, 1024:2048]
            ctmp = RF[:, 2048:3072]
            C.need(dve, t_stat)
            C.need(dve, t_cv)
            for th in range(2):
                sl = slice(th * 512, (th + 1) * 512)
                V.tensor_scalar(out=cmean[:, sl], in0=ps[th], scalar1=1.0 / 1024, scalar2=None, op0=ALU.mult)
                V.tensor_tensor(out=ctmp[:, sl], in0=cmean[:, sl], in1=cmean[:, sl], op=ALU.mult)
                V.scalar_tensor_tensor(out=ctmp[:, sl], in0=ps[2 + th], scalar=1.0 / 1024, in1=ctmp[:, sl],
                                       op0=ALU.mult, op1=ALU.subtract)
                ins = V.tensor_scalar(out=ctmp[:, sl], in0=ctmp[:, sl], scalar1=EPS, scalar2=None, op0=ALU.add)
            t_var = C.done(dve, ins)
            C.need(act, t_var)
            t_sd = C.done(act, A.sqrt(out=ctmp, in_=ctmp))
            C.need(dve, t_sd)
            V.reciprocal(out=crstd, in_=ctmp)
            t_ac = None
            for cc in range(8):
                V.tensor_tensor(out=cv[:, cc, :], in0=cv[:, cc, :], in1=cmean, op=ALU.subtract)
                t_z = C.done(dve, V.tensor_tensor(out=cv[:, cc, :], in0=cv[:, cc, :], in1=crstd, op=ALU.mult))
                C.need(act, t_z)
                t_ac = C.done(act, A.activation(out=aT[:, cc, :], in_=cv[:, cc, :], func=AF.Silu,
                                                bias=clb[:, cc:cc + 1], scale=clg[:, cc:cc + 1]))

            if debug == "aconv":
                o = dbg_out("aconv", [128, 8 * 1024], BF16)
                C.need(sp, t_ac)
                t = C.dma(sp, o, aT[:, 0:8, :].rearrange("p k t -> p (k t)"), C.newsem())
                C.need(sp, t)
                return

            rowsA = RB[:, 0:8192].rearrange("p (r d) -> p r d", r=4)
            rowsB = RE[:, 0:4096].rearrange("p (r d) -> p r d", r=2)
            C.need(sp, [t_stat, t_pv, t_f])
            rsem = C.newsem()
            C.dma(sp, rowsA[:, 0:3, :], rows_d[0:3].rearrange("r p d -> p r d"), rsem)
            C.dma(sp, rowsA[:, 3, :], mrows_d[0], rsem)
            t_rows = C.dma(sp, rowsB, mrows_d[1:3].rearrange("r p d -> p r d"), rsem)
            C.need(dve, t_rows)
            t_rows2 = C.done(dve, V.tensor_scalar(out=rowsB[:, 0, :], in0=rowsB[:, 0, :], scalar1=1.0, scalar2=None, op0=ALU.add))
            wosem = [C.newsem(), C.newsem()]
            wotok = {}
            worel = {}

            def issue_wo(g):
                slot = g % 2
                if g - 2 in worel:
                    C.need(pool, worel[g - 2])
                for q in range(4):
                    tk = C.dma(pool, wslot[slot].rearrange("p k c -> p (k c)")[:, q * 2048:(q + 1) * 2048],
                               wout_d[g][:, q * 2048:(q + 1) * 2048], wosem[slot])
                wotok[g] = tk

            C.need(pool, t_pv)
            C.need(pool, t_cv)
            issue_wo(0)
            issue_wo(1)
            C.need(pe, [t_f, t_ac, t_var])
            C.need(pe, s_rel)
            ev = [RF[:, 3584:4096], RF[:, 4096:4608]]
            ev_rel = [None, None]
            mx_sem = [C.newsem(), C.newsem()]
            bk_rel = [None, None]
            cnt = 0
            for g in range(4):
                C.need(pe, wotok[g])
                W = wslot[g % 2]
                for t in range(8):
                    b = cnt % 2
                    cnt += 1
                    C.need(pe, bk_rel[b])
                    for fc in range(KC):
                        mm = T.matmul(ps[b], lhsT=aT[:, fc, t * 128:(t + 1) * 128], rhs=W[:, fc, :],
                                      start=(fc == 0), stop=(fc == KC - 1))
                    t_mm = C.done(pe, mm)
                    C.need(dve, t_mm)
                    C.need(dve, ev_rel[b])
                    t_c = C.done(dve, V.tensor_tensor(out=ev[b], in0=ps[b], in1=rowsA[:, 0, g * 512:(g + 1) * 512], op=ALU.add))
                    bk_rel[b] = t_c
                    C.need(sp, t_c)
                    ev_rel[b] = C.dma(sp, mixd.ap()[t * 128:(t + 1) * 128, g * 512:(g + 1) * 512], ev[b], mx_sem[b])
                worel[g] = t_mm
                if g + 2 < 4:
                    issue_wo(g + 2)

            xmid_o = dout("x_mid", [1024, D])
            h2_o = dout("h2b", [1024, D], BF16)
            aff_o = dout("aff", [1024, 16])
            C.need(sp, ev_rel)
            C.need(sp, [t_mm, t_ac])
            xt = RA[:, 0:2048]
            mt = RA[:, 2048:4096]
            xmb = [RA[:, 4096:6144], RA[:, 6144:8192]]
            h2buf = [RA[:, 8192:10240], RA[:, 14336:16384]]
            h2b = RA[:, 10240:11264].bitcast(BF16)
            h2T = RA[:, 12288:14336].rearrange("p (k t) -> p k t", k=KC)
            lg = small[:, 16:32]
            lsem = C.newsem()
            osem0 = [C.newsem(), C.newsem()]
            osem1 = C.newsem()
            osem2 = C.newsem()
            st8 = {"rel_xt": None, "o0": [None, None], "o1": None, "o2": None, "tp": [None, None], "h2": [None, None],
                   "sub": None}

            def ln_stats(src, k):
                st_ = stats[:, 24 * k:24 * k + 24]
                for c4 in range(4):
                    V.bn_stats(out=st_[:, c4 * 6:(c4 + 1) * 6], in_=src[:, c4 * 512:(c4 + 1) * 512])
                mv_ = small[:, 40 + 8 * k:42 + 8 * k]
                tmpv = small[:, 42 + 8 * k:43 + 8 * k]
                tmps = small[:, 43 + 8 * k:44 + 8 * k]
                V.bn_aggr(out=mv_, in_=st_)
                tv = C.done(dve, V.tensor_scalar(out=tmpv, in0=mv_[:, 1:2], scalar1=EPS, scalar2=None, op0=ALU.add))
                C.need(act, tv)
                ts = C.done(act, A.sqrt(out=tmps, in_=tmpv))
                yield
                C.need(dve, ts)
                rs_ = small[:, 10 + 2 * k:11 + 2 * k]
                nm_ = small[:, 11 + 2 * k:12 + 2 * k]
                V.reciprocal(out=rs_, in_=tmps)
                tk = C.done(dve, V.scalar_tensor_tensor(out=nm_, in0=mv_[:, 0:1], scalar=-1.0, in1=rs_,
                                                        op0=ALU.mult, op1=ALU.mult))
                return rs_, nm_, tk

            def H1(t):
                xm = xmb[t % 2]
                C.need(sp, st8["rel_xt"])
                C.dma(sp, xt, xh[(t + 1) * 128:(t + 2) * 128, :], lsem)
                t_l = C.dma(sp, mt, mixd.ap()[t * 128:(t + 1) * 128, :], lsem)
                C.need(dve, t_l)
                C.need(dve, t_rows2)
                V.tensor_tensor(out=mt, in0=mt, in1=rowsA[:, 3, :], op=ALU.mult)
                V.scalar_tensor_tensor(out=mt, in0=xt, scalar=ALPHA, in1=mt, op0=ALU.mult, op1=ALU.add)
                rs_, nm_, tk = yield from ln_stats(mt, 0)
                C.need(act, tk)
                C.need(act, st8["o0"][t % 2])
                t_a = C.done(act, A.activation(out=xm, in_=mt, func=AF.Identity, bias=nm_, scale=rs_))
                st8["rel_xt"] = t_a
                yield
                C.need(dve, t_a)
                V.tensor_tensor(out=xm, in0=xm, in1=rowsA[:, 1, :], op=ALU.mult)
                t_xm = C.done(dve, V.tensor_tensor(out=xm, in0=xm, in1=rowsA[:, 2, :], op=ALU.add))
                C.need(sp, t_xm)
                st8["o0"][t % 2] = C.dma(sp, xmid_o[t * 128:(t + 1) * 128, :], xm, osem0[t % 2])

            def H2a(t):
                xm = xmb[t % 2]
                h2 = h2buf[t % 2]
                rs_, nm_, tk = yield from ln_stats(xm, 1)
                C.need(act, tk)
                C.need(act, st8["tp"][t % 2])
                t_a = C.done(act, A.activation(out=h2, in_=xm, func=AF.Identity, bias=nm_, scale=rs_))
                yield
                C.need(dve, t_a)
                V.tensor_tensor(out=h2, in0=h2, in1=rowsB[:, 0, :], op=ALU.mult)
                t_h2 = C.done(dve, V.tensor_tensor(out=h2, in0=h2, in1=rowsB[:, 1, :], op=ALU.add))
                st8["h2"][t % 2] = t_h2
                C.need(act, t_h2)
                C.need(act, st8["o1"])
                t_hb = C.done(act, A.activation(out=h2b, in_=h2, func=AF.Copy))
                C.need(sp, t_hb)
                st8["o1"] = C.dma(sp, h2_o[t * 128:(t + 1) * 128, :], h2b, osem1)

            def H3(t):
                h2 = h2buf[t % 2]
                C.need(pe, st8["h2"][t % 2])
                C.need(pe, st8["sub"])
                for kc in range(KC):
                    mm = T.transpose(ps[kc // 4][:, (kc % 4) * 128:(kc % 4 + 1) * 128], h2[:, kc * 128:(kc + 1) * 128], ident_f)
                t_tp = C.done(pe, mm)
                st8["tp"][t % 2] = t_tp
                C.need(act, t_tp)
                for q in range(4):
                    ins = A.activation(out=h2T[:, 4 * q:4 * q + 4, :], in_=ps[q].rearrange("p (k t) -> p k t", k=4), func=AF.Copy)
                t_cp = C.done(act, ins)
                C.need(pe, t_cp)
                for kc in range(KC):
                    mm = T.matmul(ps[4][:, 0:16], lhsT=h2T[:, kc, :], rhs=wr[:, kc * 16:(kc + 1) * 16],
                                  start=(kc == 0), stop=(kc == KC - 1))
                t_lg = C.done(pe, mm)
                yield
                C.need(dve, t_lg)
                C.need(dve, st8["o2"])
                V.tensor_reduce(out=small[:, 32:33], in_=ps[4][:, 0:16], axis=AX.X, op=ALU.max)
                t_sub = C.done(dve, V.tensor_scalar(out=lg, in0=ps[4][:, 0:16], scalar1=small[:, 32:33], scalar2=None,
                                                    op0=ALU.subtract))
                st8["sub"] = t_sub
                C.need(act, t_sub)
                t_ex = C.done(act, A.activation(out=lg, in_=lg, func=AF.Exp))
                yield
                C.need(dve, t_ex)
                V.tensor_reduce(out=small[:, 33:34], in_=lg, axis=AX.X, op=ALU.add)
                V.reciprocal(out=small[:, 34:35], in_=small[:, 33:34])
                t_af = C.done(dve, V.tensor_scalar(out=lg, in0=lg, scalar1=small[:, 34:35], scalar2=None, op0=ALU.mult))
                C.need(sp, t_af)
                st8["o2"] = C.dma(sp, aff_o[t * 128:(t + 1) * 128, :], lg, osem2)

            def run_round(gens):
                gens = [g_ for g_ in gens if g_ is not None]
                while gens:
                    nxt = []
                    for g_ in gens:
                        try:
                            next(g_)
                            nxt.append(g_)
                        except StopIteration:
                            pass
                    gens = nxt

            run_round([H1(0)])
            for t in range(8):
                run_round([H1(t + 1) if t + 1 < 8 else None, H2a(t), H3(t - 1) if t >= 1 else None])
            run_round([H3(7)])
            C.need(sp, st8["o0"])
            C.need(sp, [st8["o1"], st8["o2"]])
    return nc, dbg


def build_mod():
    nc = bass.Bass("TRN2", target_bir_lowering=False)
    cT_d = nc.dram_tensor("cT", [128, KC, 2], F32, kind="ExternalInput").ap()
    wmod_d = nc.dram_tensor("wmod", [3, 128, KC * 512], F32, kind="ExternalInput").ap()
    bmod_d = nc.dram_tensor("bmod", [2, 1536], F32, kind="ExternalInput").ap()
    o = nc.dram_tensor("modsl", [2, 1536], F32, kind="ExternalOutput").ap()
    cT = nc.alloc_sbuf_tensor("cT_sb", [128, KC * 2], F32)[:]
    scT = nc.alloc_sbuf_tensor("scT_sb", [128, KC * 2], F32)[:]
    bmod_sb = nc.alloc_sbuf_tensor("bm_sb", [2, 1536], F32)[:]
    modsl = nc.alloc_sbuf_tensor("modsl_sb", [2, 1536], F32)[:]
    RC = nc.alloc_sbuf_tensor("wm_sb", [128, KC * 512], F32)[:]
    wmod_sb = RC.rearrange("p (k c) -> p k c", k=KC)
    ps0 = nc.alloc_psum_tensor("psm", [128, 512], F32)[:]
    C = Ctx(nc)
    pe, act, dve, pool, sp = C.pe, C.act, C.dve, C.pool, C.sp
    T = nc.tensor
    V, A = Ser(dve), Ser(act)
    with nc.Block() as block:
        @block.sync
        def _(sync_engine):
            ld0 = C.newsem()
            C.dma(sp, cT, cT_d.rearrange("p k j -> p (k j)"), ld0)
            t_ld0 = C.dma(sp, bmod_sb, bmod_d, ld0)
            C.need(act, t_ld0)
            t_sc = C.done(act, A.activation(out=scT, in_=cT, func=AF.Silu))
            wm_sem = C.newsem()
            t_ev = None
            for cc in range(3):
                C.need(sp, t_ev)
                t_w = C.dma(sp, RC, wmod_d[cc], wm_sem)
                C.need(pe, t_w)
                C.need(pe, t_sc)
                for kc in range(KC):
                    mm = T.matmul(ps0[0:2, :], lhsT=scT.rearrange("p (k j) -> p k j", j=2)[:, kc, :],
                                  rhs=wmod_sb[:, kc, :], start=(kc == 0), stop=(kc == KC - 1))
                t_mm = C.done(pe, mm)
                C.need(dve, t_mm)
                C.need(dve, t_ld0)
                t_ev = C.done(dve, V.tensor_tensor(out=modsl[:, cc * 512:(cc + 1) * 512], in0=ps0[0:2, :],
                                                   in1=bmod_sb[:, cc * 512:(cc + 1) * 512], op=ALU.add))
            C.need(sp, t_ev)
            t = C.dma(sp, o, modsl, C.newsem())
            C.need(sp, t)
    return nc


def host_mod_inputs(inp):
    f = np.float32
    w_mod = np.asarray(inp["w_mod"], f)[0]
    b_mod = np.asarray(inp["b_mod"], f)[0]
    cvec = np.stack([np.asarray(inp["c"], f)[0], np.asarray(inp["c_ctx"], f)], 0)
    cT = np.ascontiguousarray(cvec.reshape(2, KC, 128).transpose(2, 1, 0))
    maps = []
    for i in range(NCORES):
        m = {"cT": cT}
        m["wmod"] = np.ascontiguousarray(
            w_mod[:, i * 1536:(i + 1) * 1536].reshape(KC, 128, 3, 512).transpose(2, 1, 0, 3)).reshape(3, 128, KC * 512)
        m["bmod"] = np.ascontiguousarray(np.broadcast_to(b_mod[i * 1536:(i + 1) * 1536][None], (2, 1536)))
        maps.append(m)
    return maps


def run_mod(inp):
    res = run_bass_kernel_spmd(build_mod(), host_mod_inputs(inp), core_ids=list(range(NCORES)))
    sl = np.stack([np.asarray(r["modsl"], np.float32) for r in res.results], 0)
    mod = np.ascontiguousarray(sl[:, 0, :]).reshape(6, D)
    mod_c = np.ascontiguousarray(sl[:, 1, :]).reshape(6, D)
    return mod, mod_c


def host_inputs(inp, mod, mod_c):
    f = np.float32
    x = np.asarray(inp["x"], f)[0]
    w_in = np.asarray(inp["w_in"], f)[0]
    b_in = np.asarray(inp["b_in"], f)[0]
    xpad = np.zeros((8192 + 256, D), f)
    xpad[128:128 + 8192] = x
    cols = []
    for g in range(4):
        cols.append(np.concatenate([np.arange(256 * g, 256 * g + 256), np.arange(1024 + 256 * g, 1024 + 256 * g + 256)]))
    cols.append(np.arange(2048, 2560))
    cols.append(np.arange(2560, 3072))
    cols.append(np.arange(3072, 3584))
    win = np.stack([np.ascontiguousarray(w_in[:, c].reshape(KC, 128, 512).transpose(1, 0, 2)).reshape(128, KC * 512)
                    for c in cols], 0)
    pc = np.zeros((128, 16 + 248 + 24 + 2), f)
    pc[:, 0:16] = b_in[:2048].reshape(16, 128).T
    pc[:, 16:264] = np.asarray(inp["w_dw"], f)[0].T.reshape(8, 128, 31).transpose(1, 0, 2).reshape(128, 248)
    pc[:, 264:272] = np.asarray(inp["b_dw"], f)[0].reshape(8, 128).T
    pc[:, 272:280] = np.asarray(inp["conv_ln_g"], f)[0].reshape(8, 128).T
    pc[:, 280:288] = np.asarray(inp["conv_ln_b"], f)[0].reshape(8, 128).T
    bqkv = np.ascontiguousarray(np.broadcast_to(b_in[2048:3584][None], (128, 1536)))
    inv = (10000.0 ** (-np.arange(0, 64, 2, dtype=np.float32) / 64)).astype(f)
    mk = np.zeros((128, 2, 128), f)
    jj = np.arange(128)[:, None]
    ii = np.arange(128)[None, :]
    mk[:, 0, :] = (jj >= ii)
    mk[:, 1, :] = (jj <= ii)
    sink = np.ascontiguousarray(np.broadcast_to(np.asarray(inp["sink"], f)[0][None], (128, 8)))
    w_out = np.asarray(inp["w_out"], f)[0]
    wout = np.stack([np.ascontiguousarray(w_out[:, g * 512:(g + 1) * 512].reshape(KC, 128, 512).transpose(1, 0, 2)).reshape(128, KC * 512)
                     for g in range(4)], 0)
    rows = np.stack([np.broadcast_to(np.asarray(inp[k], f)[0][None], (128, D))
                     for k in ("b_out", "ln1_g", "ln1_b", "ln2_g", "ln2_b")], 0)
    rows = np.ascontiguousarray(rows)
    wr = np.ascontiguousarray(np.asarray(inp["w_router"], f)[0].reshape(KC, 128, 16).transpose(1, 0, 2)).reshape(128, KC * 16)
    ctx = np.asarray(inp["ctx"], f)[0]
    modT = np.ascontiguousarray(np.concatenate([mod.reshape(96, 128).T, mod_c.reshape(96, 128).T], 1))
    mrows = np.ascontiguousarray(np.stack([np.broadcast_to(mod[k][None], (128, D)) for k in (2, 4, 3)], 0))
    maps = []
    for i in range(NCORES):
        m = {}
        m["xh"] = np.ascontiguousarray(xpad[1024 * i:1024 * i + TOK])
        m["ctx"] = ctx
        m["win"] = win
        p = pc.copy()
        p[:, 288] = 0.0 if i == 0 else 1.0
        p[:, 289] = 0.0 if i == NCORES - 1 else 1.0
        m["pcols"] = p
        m["bqkv"] = bqkv
        t = np.arange(1024 * i - 128, 1024 * i - 128 + TOK)
        ar = (t // 64).astype(f)[:, None] * inv[None]
        ac = (t % 64).astype(f)[:, None] * inv[None]
        tab = np.concatenate([np.cos(ar), np.cos(ac), np.sin(ar), np.sin(ac)], 1).astype(f)
        m["rope"] = np.ascontiguousarray(tab.reshape(NT, 128, 128).transpose(1, 0, 2))
        m["masks"] = mk
        m["sink"] = sink
        m["wout"] = wout
        m["rows"] = rows
        m["wr"] = wr
        m["modT"] = modT
        m["mrows"] = mrows
        maps.append(m)
    return maps


def _mk(nc):
    C = Ctx(nc)
    return C, nc.tensor, Ser(C.dve), Ser(C.act), Ser(C.pool)


def build_route():
    nc = bass.Bass("TRN2", target_bir_lowering=False)
    aff_d = nc.dram_tensor("aff", [8192, 16], F32, kind="ExternalInput").ap()
    mask_o = nc.dram_tensor("mask", [8192, 16], F32, kind="ExternalOutput").ap()

    def sb(name, cols, dt=F32):
        return nc.alloc_sbuf_tensor("r_" + name, [128, cols], dt)[:]
    aff = sb("aff", 1024)
    cmp_ = sb("cmp", 1024)
    ones = sb("ones", 128)
    lo, hi, mid, cntp, ge, dd = [sb(n, 16) for n in ("lo", "hi", "mid", "cntp", "ge", "dd")]
    ps = nc.alloc_psum_tensor("r_ps", [128, 512], F32)[:]
    C, T, V, A, G = _mk(nc)
    pe, dve, pool, sp = C.pe, C.dve, C.pool, C.sp
    aff3 = aff.rearrange("p (c e) -> p c e", e=16)
    cmp3 = cmp_.rearrange("p (c e) -> p c e", e=16)
    with nc.Block() as block:
        @block.sync
        def _(se):
            t_l = C.dma(sp, aff, aff_d.rearrange("(p c) e -> p (c e)", p=128), C.newsem())
            t_o = C.done(pool, G.memset(ones, 1.0))
            V.memset(lo, 0.0)
            V.memset(hi, 1.0)
            C.need(dve, t_l)
            C.need(pe, t_o)
            for it in range(34):
                V.tensor_tensor(out=mid, in0=lo, in1=hi, op=ALU.add)
                V.tensor_scalar(out=mid, in0=mid, scalar1=0.5, scalar2=None, op0=ALU.mult)
                V.tensor_tensor(out=cmp3, in0=aff3, in1=mid.unsqueeze(1).to_broadcast([128, 64, 16]), op=ALU.is_ge)
                t_c = C.done(dve, V.tensor_reduce(out=cntp, in_=cmp_.rearrange("p (c e) -> p e c", e=16),
                                                  axis=AX.X, op=ALU.add))
                C.need(pe, t_c)
                t_m = C.done(pe, T.matmul(ps[:, 0:16], lhsT=ones, rhs=cntp, start=True, stop=True))
                C.need(dve, t_m)
                V.tensor_scalar(out=ge, in0=ps[:, 0:16], scalar1=float(CAP) - 0.5, scalar2=None, op0=ALU.is_ge)
                V.tensor_tensor(out=dd, in0=mid, in1=lo, op=ALU.subtract)
                V.tensor_tensor(out=dd, in0=dd, in1=ge, op=ALU.mult)
                V.tensor_tensor(out=lo, in0=lo, in1=dd, op=ALU.add)
                V.tensor_tensor(out=dd, in0=hi, in1=mid, op=ALU.subtract)
                V.tensor_tensor(out=dd, in0=dd, in1=ge, op=ALU.mult)
                V.tensor_tensor(out=hi, in0=mid, in1=dd, op=ALU.add)
            t_f = C.done(dve, V.tensor_tensor(out=cmp3, in0=aff3, in1=lo.unsqueeze(1).to_broadcast([128, 64, 16]),
                                              op=ALU.is_ge))
            C.need(sp, t_f)
            t = C.dma(sp, mask_o.rearrange("(p c) e -> p (c e)", p=128), cmp_, C.newsem())
            C.need(sp, t)
    return nc


NFG = 22
NDB = 8


def build_experts():
    nc = bass.Bass("TRN2", target_bir_lowering=False)
    xs_d = nc.dram_tensor("xsT", [2, 128, KC * CAP], BF16, kind="ExternalInput").ap()
    gs_d = nc.dram_tensor("gsl", [128, 16], F32, kind="ExternalInput").ap()
    wg_d = nc.dram_tensor("wg", [2, NFG, 128, KC * 256], F32, kind="ExternalInput").ap()
    wu_d = nc.dram_tensor("wu", [2, NFG, 128, KC * 256], F32, kind="ExternalInput").ap()
    wd_d = nc.dram_tensor("wd", [2, NDB, 128, FC * 256], F32, kind="ExternalInput").ap()
    y_o = nc.dram_tensor("y", [2, CAP, D], F32, kind="ExternalOutput").ap()

    def sb(name, cols, dt=F32):
        return nc.alloc_sbuf_tensor("x_" + name, [128, cols], dt)[:]
    XS = sb("XS", KC * CAP, BF16).rearrange("p (k t) -> p k t", k=KC)
    HM = sb("HM", FC * CAP, BF16).rearrange("p (f t) -> p f t", f=FC)
    WGU = [sb("wgu%d" % i, 2 * KC * 256, BF16) for i in range(2)]
    WD = [sb("wd%d" % i, FC * 256, BF16) for i in range(2)]
    gs = sb("gs", 16)
    sg = [sb("sg%d" % i, 512) for i in range(2)]
    yst = [sb("yst%d" % i, 256) for i in range(2)]
    ps = [nc.alloc_psum_tensor("x_ps%d" % i, [128, 512], F32)[:] for i in range(8)]
    C, T, V, A, G = _mk(nc)
    pe, act, dve, pool, sp = C.pe, C.act, C.dve, C.pool, C.sp
    with nc.Block() as block:
        @block.sync
        def _(se):
            t_gs = C.dma(sp, gs, gs_d, C.newsem())
            xsem = C.newsem()
            gsem = [C.newsem(), C.newsem()]
            dsem = [C.newsem(), C.newsem()]
            ysem = [C.newsem(), C.newsem()]
            g_rel = [None, None]
            d_rel = [None, None]
            bank_rel = [None] * 8
            sg_rel = [None, None]
            y_rel = [None, None]
            gcnt = 0
            dcnt = 0
            ycnt = 0
            bcnt = 0
            t_gu_last = None
            t_hm = None
            for e in range(2):
                C.need(sp, t_gu_last)
                t_xs = C.dma(sp, XS.rearrange("p k t -> p (k t)"), xs_d[e], xsem)
                C.need(pe, t_xs)
                for fg in range(NFG):
                    slot = gcnt % 2
                    gcnt += 1
                    C.need(pool, g_rel[slot])
                    for half in range(2):
                        C.dma(pool, WGU[slot][:, half * 2048:(half + 1) * 2048],
                              wg_d[e, fg][:, half * 2048:(half + 1) * 2048], gsem[slot])
                    for half in range(2):
                        t_w = C.dma(pool, WGU[slot][:, 4096 + half * 2048:4096 + (half + 1) * 2048],
                                    wu_d[e, fg][:, half * 2048:(half + 1) * 2048], gsem[slot])
                    C.need(pe, t_w)
                    Wg = WGU[slot][:, 0:4096].rearrange("p (k c) -> p k c", k=KC)
                    Wu = WGU[slot][:, 4096:8192].rearrange("p (k c) -> p k c", k=KC)
                    for fj in range(2):
                        fc = fg * 2 + fj
                        base = (fc % 2) * 4
                        for th in range(2):
                            bg, bu = base + th, base + 2 + th
                            C.need(pe, bank_rel[bg])
                            for kc in range(KC):
                                mm = T.matmul(ps[bg], lhsT=Wg[:, kc, fj * 128:(fj + 1) * 128],
                                              rhs=XS[:, kc, th * 512:(th + 1) * 512], start=(kc == 0), stop=(kc == KC - 1))
                            t_g = C.done(pe, mm)
                            C.need(pe, bank_rel[bu])
                            for kc in range(KC):
                                mm = T.matmul(ps[bu], lhsT=Wu[:, kc, fj * 128:(fj + 1) * 128],
                                              rhs=XS[:, kc, th * 512:(th + 1) * 512], start=(kc == 0), stop=(kc == KC - 1))
                            t_u = C.done(pe, mm)
                            C.need(act, t_g)
                            C.need(act, sg_rel[th])
                            t_s = C.done(act, A.activation(out=sg[th], in_=ps[bg], func=AF.Silu))
                            bank_rel[bg] = t_s
                            C.need(dve, [t_s, t_u])
                            C.need(dve, d_rel)
                            t_hm = C.done(dve, V.tensor_tensor(out=HM[:, fc, th * 512:(th + 1) * 512], in0=ps[bu],
                                                               in1=sg[th], op=ALU.mult))
                            bank_rel[bu] = t_hm
                            sg_rel[th] = t_hm
                    g_rel[slot] = t_u
                    t_gu_last = t_u
                C.need(pe, t_hm)
                C.need(dve, t_gs)
                for db in range(NDB):
                    slot = dcnt % 2
                    dcnt += 1
                    C.need(pool, d_rel[slot])
                    for q in range(6):
                        c0 = q * 2048
                        c1 = min(c0 + 2048, FC * 256)
                        t_w = C.dma(pool, WD[slot][:, c0:c1], wd_d[e, db][:, c0:c1], dsem[slot])
                    C.need(pe, t_w)
                    Wd = WD[slot].rearrange("p (f c) -> p f c", f=FC)
                    for tt in range(8):
                        b = bcnt % 4
                        bcnt += 1
                        C.need(pe, bank_rel[b])
                        for fc in range(FC):
                            mm = T.matmul(ps[b][:, 0:256], lhsT=HM[:, fc, tt * 128:(tt + 1) * 128], rhs=Wd[:, fc, :],
                                          start=(fc == 0), stop=(fc == FC - 1))
                        t_d = C.done(pe, mm)
                        yb = ycnt % 2
                        ycnt += 1
                        C.need(dve, t_d)
                        C.need(dve, y_rel[yb])
                        t_y = C.done(dve, V.tensor_scalar(out=yst[yb], in0=ps[b][:, 0:256],
                                                          scalar1=gs[:, e * 8 + tt:e * 8 + tt + 1], scalar2=None, op0=ALU.mult))
                        bank_rel[b] = t_y
                        C.need(sp, t_y)
                        y_rel[yb] = C.dma(sp, y_o[e, tt * 128:(tt + 1) * 128, db * 256:(db + 1) * 256], yst[yb], ysem[yb])
                    d_rel[slot] = t_d
            C.need(sp, y_rel)
    return nc


def build_combine(K):
    nc = bass.Bass("TRN2", target_bir_lowering=False)
    xm_d = nc.dram_tensor("xm", [1024, D], F32, kind="ExternalInput").ap()
    yk_d = nc.dram_tensor("yk", [K, 1024, D], F32, kind="ExternalInput").ap()
    rows_d = nc.dram_tensor("rows", [3, 128, D], F32, kind="ExternalInput").ap()
    out_o = nc.dram_tensor("out", [1024, D], F32, kind="ExternalOutput").ap()

    def sb(name, cols, dt=F32):
        return nc.alloc_sbuf_tensor("c_" + name, [128, cols], dt)[:]
    rows = sb("rows", 3 * D).rearrange("p (r d) -> p r d", r=3)
    xm = sb("xm", D)
    acc = sb("acc", D)
    yb = [sb("yb%d" % i, D) for i in range(4)]
    ot = sb("ot", D)
    stats = sb("stats", 24)
    small = sb("small", 16)
    C, T, V, A, G = _mk(nc)
    act, dve, sp, pool = C.act, C.dve, C.sp, C.pool
    with nc.Block() as block:
        @block.sync
        def _(se):
            t_rows = C.dma(sp, rows, rows_d.rearrange("r p d -> p r d"), C.newsem())
            xsem = C.newsem()
            ysem = [C.newsem() for _ in range(4)]
            osem = C.newsem()
            y_rel = [None] * 4
            x_rel = None
            o_rel = None
            ycnt = 0
            C.need(dve, t_rows)
            for t in range(8):
                C.need(sp, x_rel)
                t_x = C.dma(sp, xm, xm_d[t * 128:(t + 1) * 128, :], xsem)
                for k in range(K):
                    b = ycnt % 4
                    ycnt += 1
                    q_ = sp if b % 2 == 0 else pool
                    C.need(q_, y_rel[b])
                    t_y = C.dma(q_, yb[b], yk_d[k, t * 128:(t + 1) * 128, :], ysem[b])
                    C.need(dve, t_y)
                    if k == 0:
                        ins = V.tensor_copy(out=acc, in_=yb[b])
                    else:
                        ins = V.tensor_tensor(out=acc, in0=acc, in1=yb[b], op=ALU.add)
                    y_rel[b] = C.done(dve, ins)
                C.need(dve, t_x)
                V.tensor_tensor(out=acc, in0=acc, in1=rows[:, 0, :], op=ALU.mult)
                V.scalar_tensor_tensor(out=acc, in0=xm, scalar=ALPHA, in1=acc, op0=ALU.mult, op1=ALU.add)
                for c4 in range(4):
                    V.bn_stats(out=stats[:, c4 * 6:(c4 + 1) * 6], in_=acc[:, c4 * 512:(c4 + 1) * 512])
                V.bn_aggr(out=small[:, 0:2], in_=stats)
                tv = C.done(dve, V.tensor_scalar(out=small[:, 8:9], in0=small[:, 1:2], scalar1=EPS, scalar2=None, op0=ALU.add))
                x_rel = tv
                C.need(act, tv)
                ts = C.done(act, A.sqrt(out=small[:, 9:10], in_=small[:, 8:9]))
                C.need(dve, ts)
                V.reciprocal(out=small[:, 2:3], in_=small[:, 9:10])
                tk = C.done(dve, V.scalar_tensor_tensor(out=small[:, 3:4], in0=small[:, 0:1], scalar=-1.0, in1=small[:, 2:3],
                                                        op0=ALU.mult, op1=ALU.mult))
                C.need(act, tk)
                C.need(act, o_rel)
                ta = C.done(act, A.activation(out=ot, in_=acc, func=AF.Identity, bias=small[:, 3:4], scale=small[:, 2:3]))
                C.need(dve, ta)
                V.tensor_tensor(out=ot, in0=ot, in1=rows[:, 1, :], op=ALU.mult)
                to = C.done(dve, V.tensor_tensor(out=ot, in0=ot, in1=rows[:, 2, :], op=ALU.add))
                C.need(sp, to)
                o_rel = C.dma(sp, out_o[t * 128:(t + 1) * 128, :], ot, osem)
            C.need(sp, o_rel)
    return nc


def build_combine2(NR):
    nc = bass.Bass("TRN2", target_bir_lowering=False)
    xm_d = nc.dram_tensor("xm", [1024, D], F32, kind="ExternalInput").ap()
    yc_d = nc.dram_tensor("yc", [8, NR * 128, D], F32, kind="ExternalInput").ap()
    S_d = nc.dram_tensor("S", [8, NR * 128, 128], F32, kind="ExternalInput").ap()
    rows_d = nc.dram_tensor("rows", [3, 128, D], F32, kind="ExternalInput").ap()
    out_o = nc.dram_tensor("out", [1024, D], F32, kind="ExternalOutput").ap()

    def sb(name, cols, dt=F32):
        return nc.alloc_sbuf_tensor("c_" + name, [128, cols], dt)[:]
    rows = sb("rows", 3 * D).rearrange("p (r d) -> p r d", r=3)
    xm = [sb("xm%d" % i, D) for i in range(2)]
    yb = [[sb("yb%d_%d" % (i, k), D) for k in range(NR)] for i in range(2)]
    Sb = [sb("S%d" % i, NR * 128).rearrange("p (k c) -> p k c", k=NR) for i in range(2)]
    acc = sb("acc", D)
    ot = sb("ot", D)
    stats = sb("stats", 24)
    small = sb("small", 16)
    ps = [nc.alloc_psum_tensor("c_ps%d" % i, [128, 512], F32)[:] for i in range(8)]
    C, T, V, A, G = _mk(nc)
    pe, act, dve, sp, pool = C.pe, C.act, C.dve, C.sp, C.pool
    with nc.Block() as block:
        @block.sync
        def _(se):
            t_rows = C.dma(sp, rows, rows_d.rearrange("r p d -> p r d"), C.newsem())
            lsem = [C.newsem(), C.newsem()]
            ysem = [[C.newsem() for _ in range(NR)] for _ in range(2)]
            osem = C.newsem()
            in_rel = [None, None]
            bank_rel = [None] * 8
            o_rel = None
            C.need(dve, t_rows)
            ltok = {}

            def loads(t):
                b = t % 2
                C.need(sp, in_rel[b])
                C.need(pool, in_rel[b])
                C.dma(sp, xm[b], xm_d[t * 128:(t + 1) * 128, :], lsem[b])
                tl = C.dma(sp, Sb[b], S_d[t].rearrange("(k p) c -> p k c", p=128), lsem[b])
                ty = []
                for k in range(NR):
                    q_ = pool if k % 2 == 0 else sp
                    ty.append(C.dma(q_, yb[b][k], yc_d[t, k * 128:(k + 1) * 128, :], ysem[b][k]))
                ltok[t] = (tl, ty)

            loads(0)
            for t in range(8):
                if t + 1 < 8:
                    loads(t + 1)
                b = t % 2
                tl, ty = ltok[t]
                C.need(pe, tl)
                for dg in range(4):
                    bk = b * 4 + dg
                    C.need(pe, bank_rel[bk])
                    for k in range(NR):
                        C.need(pe, ty[k])
                        mm = T.matmul(ps[bk], lhsT=Sb[b][:, k, :], rhs=yb[b][k][:, dg * 512:(dg + 1) * 512],
                                      start=(k == 0), stop=(k == NR - 1))
                t_mm = C.done(pe, mm)
                C.need(dve, t_mm)
                C.need(dve, tl)
                for dg in range(4):
                    ins = V.tensor_tensor(out=acc[:, dg * 512:(dg + 1) * 512], in0=ps[b * 4 + dg],
                                          in1=rows[:, 0, dg * 512:(dg + 1) * 512], op=ALU.mult)
                t_ev = C.done(dve, ins)
                for dg in range(4):
                    bank_rel[b * 4 + dg] = t_ev
                t_x = C.done(dve, V.scalar_tensor_tensor(out=acc, in0=xm[b], scalar=ALPHA, in1=acc, op0=ALU.mult, op1=ALU.add))
                in_rel[b] = [t_mm, t_x]
                for c4 in range(4):
                    V.bn_stats(out=stats[:, c4 * 6:(c4 + 1) * 6], in_=acc[:, c4 * 512:(c4 + 1) * 512])
                V.bn_aggr(out=small[:, 0:2], in_=stats)
                tv = C.done(dve, V.tensor_scalar(out=small[:, 8:9], in0=small[:, 1:2], scalar1=EPS, scalar2=None, op0=ALU.add))
                C.need(act, tv)
                ts = C.done(act, A.sqrt(out=small[:, 9:10], in_=small[:, 8:9]))
                C.need(dve, ts)
                V.reciprocal(out=small[:, 2:3], in_=small[:, 9:10])
                tk = C.done(dve, V.scalar_tensor_tensor(out=small[:, 3:4], in0=small[:, 0:1], scalar=-1.0, in1=small[:, 2:3],
                                                        op0=ALU.mult, op1=ALU.mult))
                C.need(act, tk)
                C.need(act, o_rel)
                ta = C.done(act, A.activation(out=ot, in_=acc, func=AF.Identity, bias=small[:, 3:4], scale=small[:, 2:3]))
                C.need(dve, ta)
                V.tensor_tensor(out=ot, in0=ot, in1=rows[:, 1, :], op=ALU.mult)
                to = C.done(dve, V.tensor_tensor(out=ot, in0=ot, in1=rows[:, 2, :], op=ALU.add))
                C.need(sp, to)
                o_rel = C.dma(sp, out_o[t * 128:(t + 1) * 128, :], ot, osem)
            C.need(sp, o_rel)
    return nc


def _run(nc, maps):
    return run_bass_kernel_spmd(nc, maps, core_ids=list(range(NCORES))).results


def kernel(**inputs):
    f = np.float32
    cores = list(range(NCORES))
    mod, mod_c = run_mod(inputs)
    nc, _ = build()
    res = _run(nc, host_inputs(inputs, mod, mod_c))
    x_mid = np.concatenate([np.asarray(r["x_mid"]) for r in res], 0)
    h2b = np.concatenate([np.asarray(r["h2b"]) for r in res], 0)
    aff = np.ascontiguousarray(np.concatenate([np.asarray(r["aff"], f) for r in res], 0))
    res = _run(build_route(), [{"aff": aff} for _ in cores])
    mask = np.asarray(res[0]["mask"]) > 0.5
    idx = np.zeros((16, CAP), np.int64)
    valid = np.zeros((16, CAP), bool)
    for e in range(16):
        ii = np.nonzero(mask[:, e])[0][:CAP]
        idx[e, :len(ii)] = ii
        valid[e, :len(ii)] = True
    w_gate, w_up, w_down = inputs["w_gate"], inputs["w_up"], inputs["w_down"]
    maps = []
    for i in cores:
        m = {}
        xs = []
        gsl = np.zeros((128, 16), f)
        for j in range(2):
            e = 2 * i + j
            rows = h2b[idx[e]]
            xs.append(np.ascontiguousarray(rows.T.reshape(KC, 128, CAP).transpose(1, 0, 2)).reshape(128, KC * CAP))
            ge = np.where(valid[e], aff[idx[e], e], 0).astype(f)
            gsl[:, j * 8:(j + 1) * 8] = ge.reshape(8, 128).T
        m["xsT"] = np.stack(xs, 0)
        m["gsl"] = gsl
        for nm, w in (("wg", w_gate), ("wu", w_up)):
            m[nm] = np.stack([np.ascontiguousarray(
                np.asarray(w[0, 2 * i + j], f).reshape(KC, 128, NFG, 256).transpose(2, 1, 0, 3)).reshape(NFG, 128, KC * 256)
                for j in range(2)], 0)
        m["wd"] = np.stack([np.ascontiguousarray(
            np.asarray(w_down[0, 2 * i + j], f).reshape(FC, 128, NDB, 256).transpose(2, 1, 0, 3)).reshape(NDB, 128, FC * 256)
            for j in range(2)], 0)
        maps.append(m)
    res = _run(build_experts(), maps)
    del maps
    y_all = np.concatenate([np.asarray(r["y"], f) for r in res], 0)
    pe_ = np.repeat(np.arange(16), CAP)[valid.reshape(-1)]
    ps_ = np.tile(np.arange(CAP), 16)[valid.reshape(-1)]
    pt_ = idx.reshape(-1)[valid.reshape(-1)]
    order = np.argsort(pt_, kind="stable")
    pe_, ps_, pt_ = pe_[order], ps_[order], pt_[order]
    bounds = np.searchsorted(pt_, np.arange(0, 8192 + 1, 128))
    NR = max(1, int(-(-int(np.max(np.diff(bounds))) // 128)))
    rows3 = np.ascontiguousarray(np.stack([np.broadcast_to(v[None], (128, D)) for v in
                                           (mod[5], np.asarray(inputs["ln2_g"], f)[0], np.asarray(inputs["ln2_b"], f)[0])], 0))
    maps = []
    for i in cores:
        yc = np.zeros((8, NR * 128, D), f)
        S = np.zeros((8, NR * 128, 128), f)
        for t_ in range(8):
            lo_, hi_ = bounds[8 * i + t_], bounds[8 * i + t_ + 1]
            n_ = hi_ - lo_
            yc[t_, :n_] = y_all[pe_[lo_:hi_], ps_[lo_:hi_]]
            S[t_, np.arange(n_), pt_[lo_:hi_] - (1024 * i + 128 * t_)] = 1.0
        maps.append({"xm": np.ascontiguousarray(x_mid[1024 * i:1024 * (i + 1)]), "yc": yc, "S": S, "rows": rows3})
    res = _run(build_combine2(NR), maps)
    out = np.concatenate([np.asarray(r["out"], f) for r in res], 0)
    return out.reshape(1, 8192, D).astype(f)
```

```python
import numpy as np
import concourse.bass as bass
import concourse.mybir as mybir
from concourse.bass_utils import run_bass_kernel_spmd

F32 = mybir.dt.float32
BF16 = mybir.dt.bfloat16
I32 = mybir.dt.int32
AF = mybir.ActivationFunctionType
ALU = mybir.AluOpType
AX = mybir.AxisListType

NCORES = 8
D = 2048
KC = 16
NT = 10
TOK = 1280
DFF = 5632
FC = 44
CAP = 1024
ALPHA = 2.0 ** 0.25
EPS = 1e-5


class Eng:
    def __init__(self, nc, e, name):
        self.e = e
        self.name = name
        self.sem = nc.alloc_semaphore("s_" + name)
        self.n = 0
        self.seen = {}
        self.serial = False


class Ser:
    def __init__(self, eng):
        self._eng = eng
        eng.serial = True

    def __getattr__(self, name):
        eng = self._eng
        fn = getattr(eng.e, name)

        def call(*a, **k):
            if eng.n > 0:
                eng.e.wait_ge(eng.sem, eng.n)
            ins = fn(*a, **k)
            eng.n += 1
            ins.then_inc(eng.sem, 1)
            return ins
        return call


class Ctx:
    def __init__(self, nc):
        self.nc = nc
        self.pe = Eng(nc, nc.tensor, "pe")
        self.act = Eng(nc, nc.scalar, "act")
        self.dve = Eng(nc, nc.vector, "dve")
        self.pool = Eng(nc, nc.gpsimd, "pool")
        self.sp = Eng(nc, nc.sync, "sp")
        self.nsem = 0

    def done(self, eng, ins):
        if eng.serial:
            return ("e", eng, eng.n)
        eng.n += 1
        ins.then_inc(eng.sem, 1)
        return ("e", eng, eng.n)

    def newsem(self):
        self.nsem += 1
        return [self.nc.alloc_semaphore("d%d" % self.nsem), 0]

    def dma(self, eng, out, in_, ds, **kw):
        ins = eng.e.dma_start(out=out, in_=in_, **kw)
        ds[1] += 16
        ins.then_inc(ds[0], 16)
        return ("d", ds[0], ds[1])

    def need(self, eng, tok):
        if tok is None:
            return
        if isinstance(tok, list):
            for t in tok:
                self.need(eng, t)
            return
        if tok[0] == "e":
            src, n = tok[1], tok[2]
            if src is eng:
                return
            key = src.name
        else:
            key, n = id(tok[1]), tok[2]
        if eng.seen.get(key, 0) >= n:
            return
        eng.seen[key] = n
        eng.e.wait_ge(tok[1].sem if tok[0] == "e" else tok[1], n)


def build(debug=None):
    nc = bass.Bass("TRN2", target_bir_lowering=False)

    def din(name, shape, dt=F32):
        return nc.dram_tensor(name, list(shape), dt, kind="ExternalInput").ap()

    def dout(name, shape, dt=F32):
        return nc.dram_tensor(name, list(shape), dt, kind="ExternalOutput").ap()

    xh = din("xh", [TOK, D])
    ctxd = din("ctx", [256, D])
    win_d = din("win", [7, 128, KC * 512])
    pcols_d = din("pcols", [128, 16 + 8 * 31 + 24 + 2])
    bqkv_d = din("bqkv", [128, 1536])
    rope_d = din("rope", [128, NT, 128])
    masks_d = din("masks", [128, 2, 128])
    sink_d = din("sink", [128, 8])
    wout_d = din("wout", [4, 128, KC * 512])
    rows_d = din("rows", [5, 128, D])
    wr_d = din("wr", [128, KC * 16])
    modT_d = din("modT", [128, 192])
    mrows_d = din("mrows", [3, 128, D])

    dbg = {}

    def dbg_out(name, shape, dt=F32):
        dbg[name] = dout("dbg_" + name, shape, dt)
        return dbg[name]

    mixd = nc.dram_tensor("mixd", [1024, D], F32)

    def sb(name, cols, dt=F32):
        return nc.alloc_sbuf_tensor("sb_" + name, [128, cols], dt)[:]

    ident_b = sb("ident_b", 128, BF16)
    ident_f = sb("ident_f", 128, F32)
    pcols = sb("pcols", 16 + 8 * 31 + 24 + 2)
    bconv = pcols[:, 0:16]
    wdw = pcols[:, 16:16 + 248].rearrange("p (c k) -> p c k", k=31)
    bdw = pcols[:, 264:272]
    clg = pcols[:, 272:280]
    clb = pcols[:, 280:288]
    hval = pcols[:, 288:290]
    modT = sb("modT", 2 * 96)
    sc1p = sb("sc1p", 32)
    sc2p = sb("sc2p", 16)
    ones_f = sb("ones_f", 128, F32)
    ones_b = sb("ones_b", 128, BF16)
    small = sb("small", 64)
    stats = sb("stats", 4 * 6 * 2)
    bqkv = sb("bqkv", 1536)
    rope_t = sb("rope_t", NT * 128)
    masks_f = sb("masks_f", 256)
    masks = sb("masks", 4 * 128, BF16)
    sinke = sb("sinke", 8)
    wr = sb("wr", KC * 16)

    RA = sb("RA", 16384)
    RB = sb("RB", 8448)
    RC = sb("RC", 8192)
    RE = sb("RE", 7680)
    RF = sb("RF", 6144)

    hT = RA[:, 0:10240].bitcast(BF16).rearrange("p (k t) -> p k t", k=KC)
    hcT = RA[:, 10240:12288].bitcast(BF16).rearrange("p (k t) -> p k t", k=KC)
    xstage = [RA[:, 12288:14336], RA[:, 14336:16384]]
    xn_b = [RF[:, 0:1024].bitcast(BF16), RF[:, 1024:2048].bitcast(BF16)]
    wslot = [RC[:, 0:4096].bitcast(BF16).rearrange("p (k c) -> p k c", k=KC),
             RC[:, 4096:8192].bitcast(BF16).rearrange("p (k c) -> p k c", k=KC)]
    wmod_sb = RC.rearrange("p (k c) -> p k c", k=KC)
    uT = RB.rearrange("p (c t) -> p c t", c=8)
    qT = RE[:, 0:4096].bitcast(BF16).rearrange("p (h t) -> p h t", h=8)
    kT = RE[:, 4096:5376].bitcast(BF16).rearrange("p (h t) -> p h t", h=2)
    vtok = RE[:, 5376:6656].bitcast(BF16).rearrange("p (t c) -> p t c", t=NT)
    kcT = RE[:, 6656:6912].bitcast(BF16).rearrange("p (h t) -> p h t", h=2)
    vctok = RE[:, 6912:7168].bitcast(BF16).rearrange("p (t c) -> p t c", t=2)
    qtok = RF[:, 2048:2560]
    ropeA = RF[:, 2560:3072]
    ropeB = RF[:, 3072:3584]
    qrb = RF[:, 3584:3840].bitcast(BF16)
    gsig = RF[:, 3840:4224]

    ps = [nc.alloc_psum_tensor("ps%d" % i, [128, 512], F32)[:] for i in range(6)]
    psb = [nc.alloc_psum_tensor("psb%d" % i, [128, 1024], BF16)[:] for i in range(2)]

    C = Ctx(nc)
    pe, act, dve, pool, sp = C.pe, C.act, C.dve, C.pool, C.sp
    T, S = nc.tensor, nc.sync
    V, A, G = Ser(dve), Ser(act), Ser(pool)

    with nc.Block() as block:
        @block.sync
        def _(sync_engine):
            ld0 = C.newsem()
            C.dma(sp, pcols, pcols_d, ld0)
            C.dma(sp, bqkv, bqkv_d, ld0)
            C.dma(sp, rope_t, rope_d.rearrange("p t c -> p (t c)"), ld0)
            C.dma(sp, masks_f, masks_d.rearrange("p a c -> p (a c)"), ld0)
            C.dma(sp, sinke, sink_d, ld0)
            t_ld0 = C.dma(sp, wr, wr_d, ld0)

            G.memset(ident_b, 0.0)
            G.affine_select(out=ident_b, in_=ident_b, pattern=[[-1, 128]], compare_op=ALU.not_equal,
                            fill=1.0, base=0, channel_multiplier=1)
            G.memset(ones_f, 1.0)
            G.memset(ones_b, 1.0)
            G.memset(ident_f, 0.0)
            t_id = C.done(pool, G.affine_select(out=ident_f, in_=ident_f, pattern=[[-1, 128]],
                                                compare_op=ALU.not_equal, fill=1.0, base=0,
                                                channel_multiplier=1))

            t_mt = C.dma(sp, modT, modT_d, ld0)
            C.need(dve, t_mt)
            V.tensor_scalar(out=sc1p[:, 0:16], in0=modT[:, 16:32], scalar1=1.0, scalar2=None, op0=ALU.add)
            V.tensor_scalar(out=sc1p[:, 16:32], in0=modT[:, 96 + 16:96 + 32], scalar1=1.0, scalar2=None, op0=ALU.add)
            t_mod = C.done(dve, V.tensor_scalar(out=sc2p, in0=modT[:, 64:80], scalar1=1.0, scalar2=None, op0=ALU.add))
            sh1 = [modT[:, 0:16], modT[:, 96:96 + 16]]
            sh2 = modT[:, 48:64]

            if debug == "ln0":
                o = dbg_out("sc", [128, 32])
                C.need(sp, t_mod)
                C.need(sp, t_id)
                t = C.dma(sp, o, sc1p, C.newsem())
                C.need(sp, t)
                return
            wsem = [C.newsem(), C.newsem()]
            wtok = {}
            wrel = {}

            def issue_w(g, src):
                slot = g % 2
                if g - 2 in wrel:
                    C.need(pool, wrel[g - 2])
                for q in range(4):
                    tk = C.dma(pool, wslot[slot].rearrange("p k c -> p (k c)")[:, q * 2048:(q + 1) * 2048],
                               src[:, q * 2048:(q + 1) * 2048], wsem[slot])
                wtok[g] = tk

            if debug != "ln1":
                issue_w(0, win_d[0])
                issue_w(1, win_d[1])

            xsem = [C.newsem(), C.newsem()]
            x_rel = [None, None]
            xn_rel = [None, None]
            tr_rel = [None, None]
            t_h = None
            hstate = {"t_h": None, "tr": {}}

            def rr(gens):
                gens = [g_ for g_ in gens if g_ is not None]
                while gens:
                    nxt = []
                    for g_ in gens:
                        try:
                            next(g_)
                            nxt.append(g_)
                        except StopIteration:
                            pass
                    gens = nxt

            def LNa(it):
                s = it % 2
                isctx = it >= NT
                src = ctxd[(it - NT) * 128:(it - NT + 1) * 128, :] if isctx else xh[it * 128:(it + 1) * 128, :]
                C.need(sp, x_rel[s])
                t_x = C.dma(sp, xstage[s], src, xsem[s])
                C.need(dve, t_x)
                st_ = stats[:, 24 * s:24 * s + 24]
                for c4 in range(4):
                    V.bn_stats(out=st_[:, c4 * 6:(c4 + 1) * 6], in_=xstage[s][:, c4 * 512:(c4 + 1) * 512])
                mv = small[:, 36 + 4 * s:38 + 4 * s]
                tmpv = small[:, 38 + 4 * s:39 + 4 * s]
                tmps = small[:, 39 + 4 * s:40 + 4 * s]
                V.bn_aggr(out=mv, in_=st_)
                rstd = small[:, 2 + 2 * s:3 + 2 * s]
                nmr = small[:, 3 + 2 * s:4 + 2 * s]
                C.need(dve, xn_rel[s])
                t_v = C.done(dve, V.tensor_scalar(out=tmpv, in0=mv[:, 1:2], scalar1=EPS, scalar2=None, op0=ALU.add))
                C.need(act, t_v)
                t_sq = C.done(act, A.sqrt(out=tmps, in_=tmpv))
                yield
                C.need(dve, t_sq)
                V.reciprocal(out=rstd, in_=tmps)
                t_st = C.done(dve, V.scalar_tensor_tensor(out=nmr, in0=mv[:, 0:1], scalar=-1.0, in1=rstd,
                                                          op0=ALU.mult, op1=ALU.mult))
                C.need(act, t_st)
                C.need(act, xn_rel[s])
                t_xn = C.done(act, A.activation(out=xn_b[s], in_=xstage[s], func=AF.Identity, bias=nmr, scale=rstd))
                x_rel[s] = t_xn
                yield
                C.need(pe, t_xn)
                C.need(pe, t_id)
                toks = []
                for half in range(2):
                    pst = psb[half]
                    C.need(pe, tr_rel[half])
                    for k8 in range(8):
                        kc = half * 8 + k8
                        mm = T.transpose(pst[:, k8 * 128:(k8 + 1) * 128], xn_b[s][:, kc * 128:(kc + 1) * 128], ident_b)
                    toks.append(C.done(pe, mm))
                xn_rel[s] = toks[1]
                hstate["tr"][it] = toks

            def LNb(it):
                isctx = it >= NT
                dst = hcT if isctx else hT
                t0 = (it - NT) * 128 if isctx else it * 128
                j = 1 if isctx else 0
                for half in range(2):
                    pst = psb[half]
                    t_tr = hstate["tr"][it][half]
                    C.need(dve, t_tr)
                    C.need(dve, t_mod)
                    for k8 in range(8):
                        kc = half * 8 + k8
                        insd = V.tensor_scalar(out=dst[:, kc, t0:t0 + 128], in0=pst[:, k8 * 128:(k8 + 1) * 128],
                                               scalar1=sc1p[:, j * 16 + kc:j * 16 + kc + 1],
                                               scalar2=sh1[j][:, kc:kc + 1], op0=ALU.mult, op1=ALU.add)
                    td = C.done(dve, insd)
                    tr_rel[half] = [td, td]
                    hstate["t_h"] = [td, td]
                    yield

            rr([LNa(0)])
            for it in range(NT + 2):
                rr([LNa(it + 1) if it + 1 < NT + 2 else None, LNb(it)])
            t_h = hstate["t_h"]

            if debug in ("h", "ln1"):
                o = dbg_out("hT", [128, 6 * TOK], F32)
                C.need(dve, t_h)
                t_c = C.done(dve, V.tensor_copy(out=RB[:, 0:6 * TOK], in_=hT.rearrange("p k t -> p (k t)")[:, 0:6 * TOK]))
                C.need(sp, t_c)
                t = C.dma(sp, o, RB[:, 0:6 * TOK], C.newsem())
                C.need(sp, t)
                return

            C.need(pe, t_h)
            C.need(act, t_ld0)
            C.need(dve, t_ld0)
            bank_rel = {}

            def getbank(b, eng):
                C.need(eng, bank_rel.get(b))

            TP = 352
            t_u = None
            for g in range(4):
                if g >= 2:
                    pass
                C.need(pe, wtok[g])
                W = wslot[g % 2]
                for cj in range(2):
                    cc = 2 * g + cj
                    for tp in range(3):
                        tk0 = 112 + tp * TP
                        bg, bv = (tp % 2) * 2, 1 + (tp % 2) * 2
                        getbank(bg, pe)
                        for kc in range(KC):
                            mm = T.matmul(ps[bg][:, 0:TP], lhsT=W[:, kc, 256 + cj * 128:256 + (cj + 1) * 128],
                                          rhs=hT[:, kc, tk0:tk0 + TP], start=(kc == 0), stop=(kc == KC - 1))
                        t_g = C.done(pe, mm)
                        getbank(bv, pe)
                        for kc in range(KC):
                            mm = T.matmul(ps[bv][:, 0:TP], lhsT=W[:, kc, cj * 128:(cj + 1) * 128],
                                          rhs=hT[:, kc, tk0:tk0 + TP], start=(kc == 0), stop=(kc == KC - 1))
                        t_v = C.done(pe, mm)
                        C.need(act, t_g)
                        C.need(act, bank_rel.get(("gsig", tp % 2)))
                        gs_ = gsig if tp % 2 == 0 else ropeA[:, 0:384]
                        t_s = C.done(act, A.activation(out=gs_[:, 0:TP], in_=ps[bg][:, 0:TP], func=AF.Sigmoid,
                                                       bias=bconv[:, 8 + cc:9 + cc]))
                        bank_rel[bg] = t_s
                        C.need(dve, t_s)
                        C.need(dve, t_v)
                        t_u = C.done(dve, V.scalar_tensor_tensor(out=uT[:, cc, tp * TP:(tp + 1) * TP], in0=ps[bv][:, 0:TP],
                                                                 scalar=bconv[:, cc:cc + 1], in1=gs_[:, 0:TP],
                                                                 op0=ALU.add, op1=ALU.mult))
                        bank_rel[bv] = t_u
                        bank_rel[("gsig", tp % 2)] = t_u
                wrel[g] = C.done(pe, T.matmul(ps[bv][:, 0:1], lhsT=W[:, 0, 0:128], rhs=hT[:, 0, 0:1], start=True, stop=True)) if False else t_v
                if g + 2 < 7:
                    issue_w(g + 2, win_d[g + 2])

            if debug == "u":
                o = dbg_out("uT", [128, 8 * 1056])
                C.need(sp, t_u)
                t = C.dma(sp, o, RB, C.newsem())
                C.need(sp, t)
                return

            def rope(dst_b, src_f, nh, tile):
                for a in range(2):
                    Xa = src_f.rearrange("p (h a f) -> p h a f", a=2, f=64)[:, :, a, :]
                    Oa = dst_b.rearrange("p (h a f) -> p h a f", a=2, f=64)[:, :, a, :]
                    Aa = ropeA[:, 0:nh * 64].rearrange("p (h f) -> p h f", f=64)
                    Ba = ropeB[:, 0:nh * 64].rearrange("p (h f) -> p h f", f=64)
                    cs = rope_t[:, tile * 128 + a * 32:tile * 128 + a * 32 + 32]
                    sn = rope_t[:, tile * 128 + 64 + a * 32:tile * 128 + 64 + a * 32 + 32]
                    for hf in range(2):
                        V.tensor_tensor(out=Aa[:, :, hf * 32:(hf + 1) * 32], in0=Xa[:, :, hf * 32:(hf + 1) * 32],
                                        in1=cs.unsqueeze(1).to_broadcast([128, nh, 32]), op=ALU.mult)
                        V.tensor_tensor(out=Ba[:, :, hf * 32:(hf + 1) * 32], in0=Xa[:, :, (1 - hf) * 32:(2 - hf) * 32],
                                        in1=sn.unsqueeze(1).to_broadcast([128, nh, 32]), op=ALU.mult)
                    V.tensor_tensor(out=Oa[:, :, 0:32], in0=Aa[:, :, 0:32], in1=Ba[:, :, 0:32], op=ALU.subtract)
                    last = V.tensor_tensor(out=Oa[:, :, 32:64], in0=Aa[:, :, 32:64], in1=Ba[:, :, 32:64], op=ALU.add)
                return last

            qrb_rel = None
            trb_rel = None
            t_last = None
            for g in (4, 5, 6):
                C.need(pe, wtok[g])
                W = wslot[g % 2]
                tiles = list(range(1, 9)) if g < 6 else list(range(NT + 2))
                mmtok = {}

                def emit_mm(ti):
                    tl = tiles[ti]
                    isctx = tl >= NT
                    lh = hcT[:, :, (tl - NT) * 128:(tl - NT + 1) * 128] if isctx else hT[:, :, tl * 128:(tl + 1) * 128]
                    b = 4 + (ti % 2)
                    getbank(b, pe)
                    for kc in range(KC):
                        mm = T.matmul(ps[b], lhsT=lh[:, kc, :], rhs=W[:, kc, :], start=(kc == 0), stop=(kc == KC - 1))
                    mmtok[ti] = C.done(pe, mm)

                emit_mm(0)
                for ti, tl in enumerate(tiles):
                    if ti + 1 < len(tiles):
                        emit_mm(ti + 1)
                    isctx = tl >= NT
                    b = 4 + (ti % 2)
                    t_mm = mmtok[ti]
                    C.need(dve, t_mm)
                    boff = (g - 4) * 512
                    C.need(dve, qrb_rel)
                    if g < 6:
                        V.tensor_tensor(out=qtok, in0=ps[b], in1=bqkv[:, boff:boff + 512], op=ALU.add)
                        t_r = C.done(dve, rope(qrb, qtok, 4, tl))
                        bank_rel[b] = t_r
                        ntr = 4
                    else:
                        V.tensor_tensor(out=qtok, in0=ps[b], in1=bqkv[:, boff:boff + 512], op=ALU.add)
                        vdst = vctok[:, tl - NT, :] if isctx else vtok[:, tl, :]
                        if isctx:
                            V.tensor_copy(out=qrb[:, 0:256], in_=qtok[:, 0:256])
                            t_r = C.done(dve, V.tensor_copy(out=vdst, in_=qtok[:, 256:512]))
                        else:
                            V.tensor_copy(out=vdst, in_=qtok[:, 256:512])
                            t_r = C.done(dve, rope(qrb[:, 0:256], qtok[:, 0:256], 2, tl))
                        bank_rel[b] = t_r
                        ntr = 2
                    C.need(pe, t_r)
                    C.need(pe, trb_rel)
                    C.need(pe, tr_rel[0])
                    pst = psb[0]
                    for hh in range(ntr):
                        mm = T.transpose(pst[:, hh * 128:(hh + 1) * 128], qrb[:, hh * 128:(hh + 1) * 128], ident_b)
                    t_tr = C.done(pe, mm)
                    qrb_rel = t_tr
                    C.need(act, t_tr)
                    if g < 6:
                        o_ = qT[:, (g - 4) * 4:(g - 4) * 4 + 4, (tl - 1) * 128:tl * 128]
                    elif isctx:
                        o_ = kcT[:, :, (tl - NT) * 128:(tl - NT + 1) * 128]
                    else:
                        o_ = kT[:, :, tl * 128:(tl + 1) * 128]
                    t_last = C.done(act, A.activation(out=o_, in_=pst[:, 0:ntr * 128].rearrange("p (h t) -> p h t", t=128),
                                                      func=AF.Copy))
                    trb_rel = t_last
                t_mm = mmtok[len(tiles) - 1]
                wrel[g] = t_tr
                if g + 2 < 7:
                    issue_w(g + 2, win_d[g + 2])

            if debug == "qkv":
                o1 = dbg_out("qT", [128, 8 * 1024], BF16)
                o2 = dbg_out("kT", [128, 2 * 1280], BF16)
                o3 = dbg_out("vtok", [128, NT * 256], BF16)
                o4 = dbg_out("kcT", [128, 512], BF16)
                C.need(sp, t_last)
                C.need(sp, t_r)
                ds_ = C.newsem()
                C.dma(sp, o1, qT.rearrange("p h t -> p (h t)"), ds_)
                C.dma(sp, o2, kT.rearrange("p h t -> p (h t)"), ds_)
                C.dma(sp, o3, vtok.rearrange("p t c -> p (t c)"), ds_)
                t = C.dma(sp, o4, kcT.rearrange("p h t -> p (h t)"), ds_)
                C.need(sp, t)
                return
            cv = RA[:, 0:8192].rearrange("p (c t) -> p c t", c=8)
            aT = RA[:, 8192:16384].bitcast(BF16).rearrange("p (k t) -> p k t", k=KC)
            C.need(dve, [t_last, t_mm, t_r])
            C.need(pool, [t_last, t_mm, t_r])
            V.tensor_scalar(out=uT[:, :, 0:16], in0=uT[:, :, 0:16], scalar1=hval[:, 0:1], scalar2=None, op0=ALU.mult)
            t_hm = C.done(dve, V.tensor_scalar(out=uT[:, :, 1040:1056], in0=uT[:, :, 1040:1056],
                                               scalar1=hval[:, 1:2], scalar2=None, op0=ALU.mult))
            C.need(pool, t_hm)
            t_cv = []
            uTb = RC[:, 2560:6784].bitcast(BF16).rearrange("p (c t) -> p c t", c=8)
            dgb = [RF[:, 0:1984].bitcast(BF16).rearrange("p (k c) -> p k c", k=31),
                   RF[:, 3584:5568].bitcast(BF16).rearrange("p (k c) -> p k c", k=31)]
            t_ub = C.done(dve, V.tensor_copy(out=uTb, in_=uT))
            dg_rel = [None, None]
            cb_rel = [None, None]

            def taps_pe(cc):
                dg = dgb[cc % 2]
                C.need(dve, dg_rel[cc % 2])
                t_dg = C.done(dve, V.tensor_tensor(out=dg, in0=ident_b.unsqueeze(1).to_broadcast([128, 31, 128]),
                                                   in1=wdw[:, cc, :].unsqueeze(2).to_broadcast([128, 31, 128]), op=ALU.mult))
                C.need(pe, [t_dg, t_ub])
                for th in range(2):
                    C.need(pe, cb_rel[th])
                    for k in range(31):
                        mm = T.matmul(ps[4 + th], lhsT=dg[:, k, :], rhs=uTb[:, cc, k + 1 + th * 512:k + 1 + th * 512 + 512],
                                      start=(k == 0), stop=(k == 30))
                    t_m = C.done(pe, mm)
                    C.need(act, t_m)
                    t_e = C.done(act, A.activation(out=cv[:, cc, th * 512:(th + 1) * 512], in_=ps[4 + th], func=AF.Identity,
                                                   bias=bdw[:, cc:cc + 1]))
                    cb_rel[th] = t_e
                    t_cv.append(t_e)
                dg_rel[cc % 2] = t_m

            taps_dve = taps_pe

            V.tensor_copy(out=masks[:, 0:256], in_=masks_f)
            V.tensor_scalar(out=masks[:, 256:384], in0=masks_f[:, 0:128], scalar1=hval[:, 0:1], scalar2=None, op0=ALU.mult)
            t_mk = C.done(dve, V.tensor_scalar(out=masks[:, 384:512], in0=masks_f[:, 128:256], scalar1=hval[:, 1:2],
                                               scalar2=None, op0=ALU.mult))
            t_sk = C.done(act, A.activation(out=sinke, in_=sinke, func=AF.Exp))
            pbuf = [RC[:, i * 256:(i + 1) * 256].bitcast(BF16) for i in range(10)]
            dtmp = RF[:, 3072:3584]
            SCALE = 128.0 ** -0.5
            C.need(pe, [t_mk, t_sk])
            s_rel = [None, None]
            p_rel = [None] * 10
            od_rel = [None, None]
            itn = 0
            scnt = 0
            t_f = None
            t_pv = None
            next_tap = 0

            def hq(ap):
                return ap.rearrange("p (h q) -> p h q", h=4)

            for t in range(1, 9):
                for kv in range(2):
                    par = itn % 2
                    tiles = [("c", 0), ("c", 1), ("w", t - 1), ("w", t), ("w", t + 1)]
                    ptoks = []
                    for i, (kind, idx) in enumerate(tiles):
                        keyT = kcT[:, kv, idx * 128:(idx + 1) * 128] if kind == "c" else kT[:, kv, idx * 128:(idx + 1) * 128]
                        sbk = scnt % 2
                        scnt += 1
                        C.need(pe, s_rel[sbk])
                        t_s = C.done(pe, T.matmul(ps[sbk], lhsT=keyT, rhs=qT[:, 4 * kv:4 * kv + 4, (t - 1) * 128:t * 128],
                                                  start=True, stop=True))
                        pb = pbuf[par * 5 + i]
                        C.need(act, t_s)
                        C.need(act, p_rel[par * 5 + i])
                        t_e = C.done(act, A.activation(out=pb, in_=ps[sbk], func=AF.Exp, scale=SCALE))
                        s_rel[sbk] = t_e
                        if kind == "w" and idx != t:
                            if idx == t - 1:
                                mk = masks[:, 256:384] if t == 1 else masks[:, 0:128]
                            else:
                                mk = masks[:, 384:512] if t == 8 else masks[:, 128:256]
                            C.need(dve, t_e)
                            t_e = C.done(dve, V.tensor_tensor(out=hq(pb), in0=hq(pb),
                                                              in1=mk.unsqueeze(1).to_broadcast([128, 4, 128]), op=ALU.mult))
                        ptoks.append(t_e)
                    C.need(pe, od_rel[0])
                    for i, (kind, idx) in enumerate(tiles):
                        vt = vctok[:, idx, kv * 128:(kv + 1) * 128] if kind == "c" else vtok[:, idx, kv * 128:(kv + 1) * 128]
                        C.need(pe, ptoks[i])
                        T.matmul(ps[2], lhsT=vt, rhs=pbuf[par * 5 + i], start=(i == 0), stop=(i == 4))
                    for i in range(5):
                        mm = T.matmul(ps[3], lhsT=ones_b, rhs=pbuf[par * 5 + i], start=(i == 0), stop=(i == 4))
                    t_pv = C.done(pe, mm)
                    for i in range(5):
                        p_rel[par * 5 + i] = t_pv
                    C.need(dve, t_pv)
                    C.need(dve, t_sk)
                    V.tensor_tensor(out=hq(dtmp), in0=hq(ps[3]),
                                    in1=sinke[:, 4 * kv:4 * kv + 4].unsqueeze(2).to_broadcast([128, 4, 128]), op=ALU.add)
                    V.reciprocal(out=dtmp, in_=dtmp)
                    t_f = C.done(dve, V.tensor_tensor(out=aT[:, 8 + 4 * kv:8 + 4 * kv + 4, (t - 1) * 128:t * 128],
                                                      in0=hq(ps[2]), in1=hq(dtmp), op=ALU.mult))
                    od_rel[0] = t_f
                    itn += 1
                    if itn % 2 == 0 and next_tap < 8:
                        taps_dve(next_tap)
                        next_tap += 1
            while next_tap < 8:
                taps_dve(next_tap)
                next_tap += 1

            if debug == "attn":
                o = dbg_out("attn", [128, 8 * 1024], BF16)
                C.need(sp, t_f)
                t = C.dma(sp, o, aT[:, 8:16, :].rearrange("p k t -> p (k t)"), C.newsem())
                C.need(sp, t)
                return

            sq = RB[:, 0:8192].rearrange("p (c t) -> p c t", c=8)
            C.need(act, t_cv)
            for cc in range(8):
                ins = A.activation(out=sq[:, cc, :], in_=cv[:, cc, :], func=AF.Square)
            t_sq = C.done(act, ins)
            C.need(pe, t_sq)
            C.need(pe, t_cv)
            C.need(pe, t_id)
            C.need(pe, [t_f] + s_rel)
            for th in range(2):
                for cc in range(8):
                    T.matmul(ps[th], lhsT=ones_f, rhs=cv[:, cc, th * 512:(th + 1) * 512], start=(cc == 0), stop=(cc == 7))
                for cc in range(8):
                    mm = T.matmul(ps[2 + th], lhsT=ones_f, rhs=sq[:, cc, th * 512:(th + 1) * 512], start=(cc == 0), stop=(cc == 7))
            t_stat = C.done(pe, mm)
            cmean = RF[:, 0:1024]
            crstd = RF[:, 1024:2048]
            ctmp = RF[:, 2048:3072]
            C.need(dve, t_stat)
            C.need(dve, t_cv)
            for th in range(2):
                sl = slice(th * 512, (th + 1) * 512)
                V.tensor_scalar(out=cmean[:, sl], in0=ps[th], scalar1=1.0 / 1024, scalar2=None, op0=ALU.mult)
                V.tensor_tensor(out=ctmp[:, sl], in0=cmean[:, sl], in1=cmean[:, sl], op=ALU.mult)
                V.scalar_tensor_tensor(out=ctmp[:, sl], in0=ps[2 + th], scalar=1.0 / 1024, in1=ctmp[:, sl],
                                       op0=ALU.mult, op1=ALU.subtract)
                ins = V.tensor_scalar(out=ctmp[:, sl], in0=ctmp[:, sl], scalar1=EPS, scalar2=None, op0=ALU.add)
            t_var = C.done(dve, ins)
            C.need(act, t_var)
            t_sd = C.done(act, A.sqrt(out=ctmp, in_=ctmp))
            C.need(dve, t_sd)
            V.reciprocal(out=crstd, in_=ctmp)
            t_ac = None
            for cc in range(8):
                V.tensor_tensor(out=cv[:, cc, :], in0=cv[:, cc, :], in1=cmean, op=ALU.subtract)
                t_z = C.done(dve, V.tensor_tensor(out=cv[:, cc, :], in0=cv[:, cc, :], in1=crstd, op=ALU.mult))
                C.need(act, t_z)
                t_ac = C.done(act, A.activation(out=aT[:, cc, :], in_=cv[:, cc, :], func=AF.Silu,
                                                bias=clb[:, cc:cc + 1], scale=clg[:, cc:cc + 1]))

            if debug == "aconv":
                o = dbg_out("aconv", [128, 8 * 1024], BF16)
                C.need(sp, t_ac)
                t = C.dma(sp, o, aT[:, 0:8, :].rearrange("p k t -> p (k t)"), C.newsem())
                C.need(sp, t)
                return

            rowsA = RB[:, 0:8192].rearrange("p (r d) -> p r d", r=4)
            rowsB = RE[:, 0:4096].rearrange("p (r d) -> p r d", r=2)
            C.need(sp, [t_stat, t_pv, t_f])
            rsem = C.newsem()
            C.dma(sp, rowsA[:, 0:3, :], rows_d[0:3].rearrange("r p d -> p r d"), rsem)
            C.dma(sp, rowsA[:, 3, :], mrows_d[0], rsem)
            t_rows = C.dma(sp, rowsB, mrows_d[1:3].rearrange("r p d -> p r d"), rsem)
            C.need(dve, t_rows)
            t_rows2 = C.done(dve, V.tensor_scalar(out=rowsB[:, 0, :], in0=rowsB[:, 0, :], scalar1=1.0, scalar2=None, op0=ALU.add))
            wosem = [C.newsem(), C.newsem()]
            wotok = {}
            worel = {}

            def issue_wo(g):
                slot = g % 2
                if g - 2 in worel:
                    C.need(pool, worel[g - 2])
                for q in range(4):
                    tk = C.dma(pool, wslot[slot].rearrange("p k c -> p (k c)")[:, q * 2048:(q + 1) * 2048],
                               wout_d[g][:, q * 2048:(q + 1) * 2048], wosem[slot])
                wotok[g] = tk

            C.need(pool, t_pv)
            C.need(pool, t_cv)
            issue_wo(0)
            issue_wo(1)
            C.need(pe, [t_f, t_ac, t_var])
            C.need(pe, s_rel)
            ev = [RF[:, 3584:4096], RF[:, 4096:4608]]
            ev_rel = [None, None]
            mx_sem = [C.newsem(), C.newsem()]
            bk_rel = [None, None]
            cnt = 0
            for g in range(4):
                C.need(pe, wotok[g])
                W = wslot[g % 2]
                for t in range(8):
                    b = cnt % 2
                    cnt += 1
                    C.need(pe, bk_rel[b])
                    for fc in range(KC):
                        mm = T.matmul(ps[b], lhsT=aT[:, fc, t * 128:(t + 1) * 128], rhs=W[:, fc, :],
                                      start=(fc == 0), stop=(fc == KC - 1))
                    t_mm = C.done(pe, mm)
                    C.need(dve, t_mm)
                    C.need(dve, ev_rel[b])
                    t_c = C.done(dve, V.tensor_tensor(out=ev[b], in0=ps[b], in1=rowsA[:, 0, g * 512:(g + 1) * 512], op=ALU.add))
                    bk_rel[b] = t_c
                    C.need(sp, t_c)
                    ev_rel[b] = C.dma(sp, mixd.ap()[t * 128:(t + 1) * 128, g * 512:(g + 1) * 512], ev[b], mx_sem[b])
                worel[g] = t_mm
                if g + 2 < 4:
                    issue_wo(g + 2)

            xmid_o = dout("x_mid", [1024, D])
            h2_o = dout("h2b", [1024, D], BF16)
            aff_o = dout("aff", [1024, 16])
            C.need(sp, ev_rel)
            C.need(sp, [t_mm, t_ac])
            xt = RA[:, 0:2048]
            mt = RA[:, 2048:4096]
            xmb = [RA[:, 4096:6144], RA[:, 6144:8192]]
            h2buf = [RA[:, 8192:10240], RA[:, 14336:16384]]
            h2b = RA[:, 10240:11264].bitcast(BF16)
            h2T = RA[:, 12288:14336].rearrange("p (k t) -> p k t", k=KC)
            lg = small[:, 16:32]
            lsem = C.newsem()
            osem0 = [C.newsem(), C.newsem()]
            osem1 = C.newsem()
            osem2 = C.newsem()
            st8 = {"rel_xt": None, "o0": [None, None], "o1": None, "o2": None, "tp": [None, None], "h2": [None, None],
                   "sub": None}

            def ln_stats(src, k):
                st_ = stats[:, 24 * k:24 * k + 24]
                for c4 in range(4):
                    V.bn_stats(out=st_[:, c4 * 6:(c4 + 1) * 6], in_=src[:, c4 * 512:(c4 + 1) * 512])
                mv_ = small[:, 40 + 8 * k:42 + 8 * k]
                tmpv = small[:, 42 + 8 * k:43 + 8 * k]
                tmps = small[:, 43 + 8 * k:44 + 8 * k]
                V.bn_aggr(out=mv_, in_=st_)
                tv = C.done(dve, V.tensor_scalar(out=tmpv, in0=mv_[:, 1:2], scalar1=EPS, scalar2=None, op0=ALU.add))
                C.need(act, tv)
                ts = C.done(act, A.sqrt(out=tmps, in_=tmpv))
                yield
                C.need(dve, ts)
                rs_ = small[:, 10 + 2 * k:11 + 2 * k]
                nm_ = small[:, 11 + 2 * k:12 + 2 * k]
                V.reciprocal(out=rs_, in_=tmps)
                tk = C.done(dve, V.scalar_tensor_tensor(out=nm_, in0=mv_[:, 0:1], scalar=-1.0, in1=rs_,
                                                        op0=ALU.mult, op1=ALU.mult))
                return rs_, nm_, tk

            def H1(t):
                xm = xmb[t % 2]
                C.need(sp, st8["rel_xt"])
                C.dma(sp, xt, xh[(t + 1) * 128:(t + 2) * 128, :], lsem)
                t_l = C.dma(sp, mt, mixd.ap()[t * 128:(t + 1) * 128, :], lsem)
                C.need(dve, t_l)
                C.need(dve, t_rows2)
                V.tensor_tensor(out=mt, in0=mt, in1=rowsA[:, 3, :], op=ALU.mult)
                V.scalar_tensor_tensor(out=mt, in0=xt, scalar=ALPHA, in1=mt, op0=ALU.mult, op1=ALU.add)
                rs_, nm_, tk = yield from ln_stats(mt, 0)
                C.need(act, tk)
                C.need(act, st8["o0"][t % 2])
                t_a = C.done(act, A.activation(out=xm, in_=mt, func=AF.Identity, bias=nm_, scale=rs_))
                st8["rel_xt"] = t_a
                yield
                C.need(dve, t_a)
                V.tensor_tensor(out=xm, in0=xm, in1=rowsA[:, 1, :], op=ALU.mult)
                t_xm = C.done(dve, V.tensor_tensor(out=xm, in0=xm, in1=rowsA[:, 2, :], op=ALU.add))
                C.need(sp, t_xm)
                st8["o0"][t % 2] = C.dma(sp, xmid_o[t * 128:(t + 1) * 128, :], xm, osem0[t % 2])

            def H2a(t):
                xm = xmb[t % 2]
                h2 = h2buf[t % 2]
                rs_, nm_, tk = yield from ln_stats(xm, 1)
                C.need(act, tk)
                C.need(act, st8["tp"][t % 2])
                t_a = C.done(act, A.activation(out=h2, in_=xm, func=AF.Identity, bias=nm_, scale=rs_))
                yield
                C.need(dve, t_a)
                V.tensor_tensor(out=h2, in0=h2, in1=rowsB[:, 0, :], op=ALU.mult)
                t_h2 = C.done(dve, V.tensor_tensor(out=h2, in0=h2, in1=rowsB[:, 1, :], op=ALU.add))
                st8["h2"][t % 2] = t_h2
                C.need(act, t_h2)
                C.need(act, st8["o1"])
                t_hb = C.done(act, A.activation(out=h2b, in_=h2, func=AF.Copy))
                C.need(sp, t_hb)
                st8["o1"] = C.dma(sp, h2_o[t * 128:(t + 1) * 128, :], h2b, osem1)

            def H3(t):
                h2 = h2buf[t % 2]
                C.need(pe, st8["h2"][t % 2])
                C.need(pe, st8["sub"])
                for kc in range(KC):
                    mm = T.transpose(ps[kc // 4][:, (kc % 4) * 128:(kc % 4 + 1) * 128], h2[:, kc * 128:(kc + 1) * 128], ident_f)
                t_tp = C.done(pe, mm)
                st8["tp"][t % 2] = t_tp
                C.need(act, t_tp)
                for q in range(4):
                    ins = A.activation(out=h2T[:, 4 * q:4 * q + 4, :], in_=ps[q].rearrange("p (k t) -> p k t", k=4), func=AF.Copy)
                t_cp = C.done(act, ins)
                C.need(pe, t_cp)
                for kc in range(KC):
                    mm = T.matmul(ps[4][:, 0:16], lhsT=h2T[:, kc, :], rhs=wr[:, kc * 16:(kc + 1) * 16],
                                  start=(kc == 0), stop=(kc == KC - 1))
                t_lg = C.done(pe, mm)
                yield
                C.need(dve, t_lg)
                C.need(dve, st8["o2"])
                V.tensor_reduce(out=small[:, 32:33], in_=ps[4][:, 0:16], axis=AX.X, op=ALU.max)
                t_sub = C.done(dve, V.tensor_scalar(out=lg, in0=ps[4][:, 0:16], scalar1=small[:, 32:33], scalar2=None,
                                                    op0=ALU.subtract))
                st8["sub"] = t_sub
                C.need(act, t_sub)
                t_ex = C.done(act, A.activation(out=lg, in_=lg, func=AF.Exp))
                yield
                C.need(dve, t_ex)
                V.tensor_reduce(out=small[:, 33:34], in_=lg, axis=AX.X, op=ALU.add)
                V.reciprocal(out=small[:, 34:35], in_=small[:, 33:34])
                t_af = C.done(dve, V.tensor_scalar(out=lg, in0=lg, scalar1=small[:, 34:35], scalar2=None, op0=ALU.mult))
                C.need(sp, t_af)
                st8["o2"] = C.dma(sp, aff_o[t * 128:(t + 1) * 128, :], lg, osem2)

            def run_round(gens):
                gens = [g_ for g_ in gens if g_ is not None]
                while gens:
                    nxt = []
                    for g_ in gens:
                        try:
                            next(g_)
                            nxt.append(g_)
                        except StopIteration:
                            pass
                    gens = nxt

            run_round([H1(0)])
            for t in range(8):
                run_round([H1(t + 1) if t + 1 < 8 else None, H2a(t), H3(t - 1) if t >= 1 else None])
            run_round([H3(7)])
            C.need(sp, st8["o0"])
            C.need(sp, [st8["o1"], st8["o2"]])
    return nc, dbg


def build_mod():
    nc = bass.Bass("TRN2", target_bir_lowering=False)
    cT_d = nc.dram_tensor("cT", [128, KC, 2], F32, kind="ExternalInput").ap()
    wmod_d = nc.dram_tensor("wmod", [3, 128, KC * 512], F32, kind="ExternalInput").ap()
    bmod_d = nc.dram_tensor("bmod", [2, 1536], F32, kind="ExternalInput").ap()
    o = nc.dram_tensor("modsl", [2, 1536], F32, kind="ExternalOutput").ap()
    cT = nc.alloc_sbuf_tensor("cT_sb", [128, KC * 2], F32)[:]
    scT = nc.alloc_sbuf_tensor("scT_sb", [128, KC * 2], F32)[:]
    bmod_sb = nc.alloc_sbuf_tensor("bm_sb", [2, 1536], F32)[:]
    modsl = nc.alloc_sbuf_tensor("modsl_sb", [2, 1536], F32)[:]
    RC = nc.alloc_sbuf_tensor("wm_sb", [128, KC * 512], F32)[:]
    wmod_sb = RC.rearrange("p (k c) -> p k c", k=KC)
    ps0 = nc.alloc_psum_tensor("psm", [128, 512], F32)[:]
    C = Ctx(nc)
    pe, act, dve, pool, sp = C.pe, C.act, C.dve, C.pool, C.sp
    T = nc.tensor
    V, A = Ser(dve), Ser(act)
    with nc.Block() as block:
        @block.sync
        def _(sync_engine):
            ld0 = C.newsem()
            C.dma(sp, cT, cT_d.rearrange("p k j -> p (k j)"), ld0)
            t_ld0 = C.dma(sp, bmod_sb, bmod_d, ld0)
            C.need(act, t_ld0)
            t_sc = C.done(act, A.activation(out=scT, in_=cT, func=AF.Silu))
            wm_sem = C.newsem()
            t_ev = None
            for cc in range(3):
                C.need(sp, t_ev)
                t_w = C.dma(sp, RC, wmod_d[cc], wm_sem)
                C.need(pe, t_w)
                C.need(pe, t_sc)
                for kc in range(KC):
                    mm = T.matmul(ps0[0:2, :], lhsT=scT.rearrange("p (k j) -> p k j", j=2)[:, kc, :],
                                  rhs=wmod_sb[:, kc, :], start=(kc == 0), stop=(kc == KC - 1))
                t_mm = C.done(pe, mm)
                C.need(dve, t_mm)
                C.need(dve, t_ld0)
                t_ev = C.done(dve, V.tensor_tensor(out=modsl[:, cc * 512:(cc + 1) * 512], in0=ps0[0:2, :],
                                                   in1=bmod_sb[:, cc * 512:(cc + 1) * 512], op=ALU.add))
            C.need(sp, t_ev)
            t = C.dma(sp, o, modsl, C.newsem())
            C.need(sp, t)
    return nc


def host_mod_inputs(inp):
    f = np.float32
    w_mod = np.asarray(inp["w_mod"], f)[0]
    b_mod = np.asarray(inp["b_mod"], f)[0]
    cvec = np.stack([np.asarray(inp["c"], f)[0], np.asarray(inp["c_ctx"], f)], 0)
    cT = np.ascontiguousarray(cvec.reshape(2, KC, 128).transpose(2, 1, 0))
    maps = []
    for i in range(NCORES):
        m = {"cT": cT}
        m["wmod"] = np.ascontiguousarray(
            w_mod[:, i * 1536:(i + 1) * 1536].reshape(KC, 128, 3, 512).transpose(2, 1, 0, 3)).reshape(3, 128, KC * 512)
        m["bmod"] = np.ascontiguousarray(np.broadcast_to(b_mod[i * 1536:(i + 1) * 1536][None], (2, 1536)))
        maps.append(m)
    return maps


def run_mod(inp):
    res = run_bass_kernel_spmd(build_mod(), host_mod_inputs(inp), core_ids=list(range(NCORES)))
    sl = np.stack([np.asarray(r["modsl"], np.float32) for r in res.results], 0)
    mod = np.ascontiguousarray(sl[:, 0, :]).reshape(6, D)
    mod_c = np.ascontiguousarray(sl[:, 1, :]).reshape(6, D)
    return mod, mod_c


def host_inputs(inp, mod, mod_c):
    f = np.float32
    x = np.asarray(inp["x"], f)[0]
    w_in = np.asarray(inp["w_in"], f)[0]
    b_in = np.asarray(inp["b_in"], f)[0]
    xpad = np.zeros((8192 + 256, D), f)
    xpad[128:128 + 8192] = x
    cols = []
    for g in range(4):
        cols.append(np.concatenate([np.arange(256 * g, 256 * g + 256), np.arange(1024 + 256 * g, 1024 + 256 * g + 256)]))
    cols.append(np.arange(2048, 2560))
    cols.append(np.arange(2560, 3072))
    cols.append(np.arange(3072, 3584))
    win = np.stack([np.ascontiguousarray(w_in[:, c].reshape(KC, 128, 512).transpose(1, 0, 2)).reshape(128, KC * 512)
                    for c in cols], 0)
    pc = np.zeros((128, 16 + 248 + 24 + 2), f)
    pc[:, 0:16] = b_in[:2048].reshape(16, 128).T
    pc[:, 16:264] = np.asarray(inp["w_dw"], f)[0].T.reshape(8, 128, 31).transpose(1, 0, 2).reshape(128, 248)
    pc[:, 264:272] = np.asarray(inp["b_dw"], f)[0].reshape(8, 128).T
    pc[:, 272:280] = np.asarray(inp["conv_ln_g"], f)[0].reshape(8, 128).T
    pc[:, 280:288] = np.asarray(inp["conv_ln_b"], f)[0].reshape(8, 128).T
    bqkv = np.ascontiguousarray(np.broadcast_to(b_in[2048:3584][None], (128, 1536)))
    inv = (10000.0 ** (-np.arange(0, 64, 2, dtype=np.float32) / 64)).astype(f)
    mk = np.zeros((128, 2, 128), f)
    jj = np.arange(128)[:, None]
    ii = np.arange(128)[None, :]
    mk[:, 0, :] = (jj >= ii)
    mk[:, 1, :] = (jj <= ii)
    sink = np.ascontiguousarray(np.broadcast_to(np.asarray(inp["sink"], f)[0][None], (128, 8)))
    w_out = np.asarray(inp["w_out"], f)[0]
    wout = np.stack([np.ascontiguousarray(w_out[:, g * 512:(g + 1) * 512].reshape(KC, 128, 512).transpose(1, 0, 2)).reshape(128, KC * 512)
                     for g in range(4)], 0)
    rows = np.stack([np.broadcast_to(np.asarray(inp[k], f)[0][None], (128, D))
                     for k in ("b_out", "ln1_g", "ln1_b", "ln2_g", "ln2_b")], 0)
    rows = np.ascontiguousarray(rows)
    wr = np.ascontiguousarray(np.asarray(inp["w_router"], f)[0].reshape(KC, 128, 16).transpose(1, 0, 2)).reshape(128, KC * 16)
    ctx = np.asarray(inp["ctx"], f)[0]
    modT = np.ascontiguousarray(np.concatenate([mod.reshape(96, 128).T, mod_c.reshape(96, 128).T], 1))
    mrows = np.ascontiguousarray(np.stack([np.broadcast_to(mod[k][None], (128, D)) for k in (2, 4, 3)], 0))
    maps = []
    for i in range(NCORES):
        m = {}
        m["xh"] = np.ascontiguousarray(xpad[1024 * i:1024 * i + TOK])
        m["ctx"] = ctx
        m["win"] = win
        p = pc.copy()
        p[:, 288] = 0.0 if i == 0 else 1.0
        p[:, 289] = 0.0 if i == NCORES - 1 else 1.0
        m["pcols"] = p
        m["bqkv"] = bqkv
        t = np.arange(1024 * i - 128, 1024 * i - 128 + TOK)
        ar = (t // 64).astype(f)[:, None] * inv[None]
        ac = (t % 64).astype(f)[:, None] * inv[None]
        tab = np.concatenate([np.cos(ar), np.cos(ac), np.sin(ar), np.sin(ac)], 1).astype(f)
        m["rope"] = np.ascontiguousarray(tab.reshape(NT, 128, 128).transpose(1, 0, 2))
        m["masks"] = mk
        m["sink"] = sink
        m["wout"] = wout
        m["rows"] = rows
        m["wr"] = wr
        m["modT"] = modT
        m["mrows"] = mrows
        maps.append(m)
    return maps


def _mk(nc):
    C = Ctx(nc)
    return C, nc.tensor, Ser(C.dve), Ser(C.act), Ser(C.pool)


def build_route():
    nc = bass.Bass("TRN2", target_bir_lowering=False)
    aff_d = nc.dram_tensor("aff", [8192, 16], F32, kind="ExternalInput").ap()
    mask_o = nc.dram_tensor("mask", [8192, 16], F32, kind="ExternalOutput").ap()

    def sb(name, cols, dt=F32):
        return nc.alloc_sbuf_tensor("r_" + name, [128, cols], dt)[:]
    aff = sb("aff", 1024)
    cmp_ = sb("cmp", 1024)
    ones = sb("ones", 128)
    lo, hi, mid, cntp, ge, dd = [sb(n, 16) for n in ("lo", "hi", "mid", "cntp", "ge", "dd")]
    ps = nc.alloc_psum_tensor("r_ps", [128, 512], F32)[:]
    C, T, V, A, G = _mk(nc)
    pe, dve, pool, sp = C.pe, C.dve, C.pool, C.sp
    aff3 = aff.rearrange("p (c e) -> p c e", e=16)
    cmp3 = cmp_.rearrange("p (c e) -> p c e", e=16)
    with nc.Block() as block:
        @block.sync
        def _(se):
            t_l = C.dma(sp, aff, aff_d.rearrange("(p c) e -> p (c e)", p=128), C.newsem())
            t_o = C.done(pool, G.memset(ones, 1.0))
            V.memset(lo, 0.0)
            V.memset(hi, 1.0)
            C.need(dve, t_l)
            C.need(pe, t_o)
            for it in range(34):
                V.tensor_tensor(out=mid, in0=lo, in1=hi, op=ALU.add)
                V.tensor_scalar(out=mid, in0=mid, scalar1=0.5, scalar2=None, op0=ALU.mult)
                V.tensor_tensor(out=cmp3, in0=aff3, in1=mid.unsqueeze(1).to_broadcast([128, 64, 16]), op=ALU.is_ge)
                t_c = C.done(dve, V.tensor_reduce(out=cntp, in_=cmp_.rearrange("p (c e) -> p e c", e=16),
                                                  axis=AX.X, op=ALU.add))
                C.need(pe, t_c)
                t_m = C.done(pe, T.matmul(ps[:, 0:16], lhsT=ones, rhs=cntp, start=True, stop=True))
                C.need(dve, t_m)
                V.tensor_scalar(out=ge, in0=ps[:, 0:16], scalar1=float(CAP) - 0.5, scalar2=None, op0=ALU.is_ge)
                V.tensor_tensor(out=dd, in0=mid, in1=lo, op=ALU.subtract)
                V.tensor_tensor(out=dd, in0=dd, in1=ge, op=ALU.mult)
                V.tensor_tensor(out=lo, in0=lo, in1=dd, op=ALU.add)
                V.tensor_tensor(out=dd, in0=hi, in1=mid, op=ALU.subtract)
                V.tensor_tensor(out=dd, in0=dd, in1=ge, op=ALU.mult)
                V.tensor_tensor(out=hi, in0=mid, in1=dd, op=ALU.add)
            t_f = C.done(dve, V.tensor_tensor(out=cmp3, in0=aff3, in1=lo.unsqueeze(1).to_broadcast([128, 64, 16]),
                                              op=ALU.is_ge))
            C.need(sp, t_f)
            t = C.dma(sp, mask_o.rearrange("(p c) e -> p (c e)", p=128), cmp_, C.newsem())
            C.need(sp, t)
    return nc


NFG = 22
NDB = 8


def build_experts():
    nc = bass.Bass("TRN2", target_bir_lowering=False)
    xs_d = nc.dram_tensor("xsT", [2, 128, KC * CAP], BF16, kind="ExternalInput").ap()
    gs_d = nc.dram_tensor("gsl", [128, 16], F32, kind="ExternalInput").ap()
    wg_d = nc.dram_tensor("wg", [2, NFG, 128, KC * 256], F32, kind="ExternalInput").ap()
    wu_d = nc.dram_tensor("wu", [2, NFG, 128, KC * 256], F32, kind="ExternalInput").ap()
    wd_d = nc.dram_tensor("wd", [2, NDB, 128, FC * 256], F32, kind="ExternalInput").ap()
    y_o = nc.dram_tensor("y", [2, CAP, D], F32, kind="ExternalOutput").ap()

    def sb(name, cols, dt=F32):
        return nc.alloc_sbuf_tensor("x_" + name, [128, cols], dt)[:]
    XS = sb("XS", KC * CAP, BF16).rearrange("p (k t) -> p k t", k=KC)
    HM = sb("HM", FC * CAP, BF16).rearrange("p (f t) -> p f t", f=FC)
    WGU = [sb("wgu%d" % i, 2 * KC * 256, BF16) for i in range(2)]
    WD = [sb("wd%d" % i, FC * 256, BF16) for i in range(2)]
    gs = sb("gs", 16)
    sg = [sb("sg%d" % i, 512) for i in range(2)]
    yst = [sb("yst%d" % i, 256) for i in range(2)]
    ps = [nc.alloc_psum_tensor("x_ps%d" % i, [128, 512], F32)[:] for i in range(8)]
    C, T, V, A, G = _mk(nc)
    pe, act, dve, pool, sp = C.pe, C.act, C.dve, C.pool, C.sp
    with nc.Block() as block:
        @block.sync
        def _(se):
            t_gs = C.dma(sp, gs, gs_d, C.newsem())
            xsem = C.newsem()
            gsem = [C.newsem(), C.newsem()]
            dsem = [C.newsem(), C.newsem()]
            ysem = [C.newsem(), C.newsem()]
            g_rel = [None, None]
            d_rel = [None, None]
            bank_rel = [None] * 8
            sg_rel = [None, None]
            y_rel = [None, None]
            gcnt = 0
            dcnt = 0
            ycnt = 0
            bcnt = 0
            t_gu_last = None
            t_hm = None
            for e in range(2):
                C.need(sp, t_gu_last)
                t_xs = C.dma(sp, XS.rearrange("p k t -> p (k t)"), xs_d[e], xsem)
                C.need(pe, t_xs)
                for fg in range(NFG):
                    slot = gcnt % 2
                    gcnt += 1
                    C.need(pool, g_rel[slot])
                    for half in range(2):
                        C.dma(pool, WGU[slot][:, half * 2048:(half + 1) * 2048],
                              wg_d[e, fg][:, half * 2048:(half + 1) * 2048], gsem[slot])
                    for half in range(2):
                        t_w = C.dma(pool, WGU[slot][:, 4096 + half * 2048:4096 + (half + 1) * 2048],
                                    wu_d[e, fg][:, half * 2048:(half + 1) * 2048], gsem[slot])
                    C.need(pe, t_w)
                    Wg = WGU[slot][:, 0:4096].rearrange("p (k c) -> p k c", k=KC)
                    Wu = WGU[slot][:, 4096:8192].rearrange("p (k c) -> p k c", k=KC)
                    for fj in range(2):
                        fc = fg * 2 + fj
                        base = (fc % 2) * 4
                        for th in range(2):
                            bg, bu = base + th, base + 2 + th
                            C.need(pe, bank_rel[bg])
                            for kc in range(KC):
                                mm = T.matmul(ps[bg], lhsT=Wg[:, kc, fj * 128:(fj + 1) * 128],
                                              rhs=XS[:, kc, th * 512:(th + 1) * 512], start=(kc == 0), stop=(kc == KC - 1))
                            t_g = C.done(pe, mm)
                            C.need(pe, bank_rel[bu])
                            for kc in range(KC):
                                mm = T.matmul(ps[bu], lhsT=Wu[:, kc, fj * 128:(fj + 1) * 128],
                                              rhs=XS[:, kc, th * 512:(th + 1) * 512], start=(kc == 0), stop=(kc == KC - 1))
                            t_u = C.done(pe, mm)
                            C.need(act, t_g)
                            C.need(act, sg_rel[th])
                            t_s = C.done(act, A.activation(out=sg[th], in_=ps[bg], func=AF.Silu))
                            bank_rel[bg] = t_s
                            C.need(dve, [t_s, t_u])
                            C.need(dve, d_rel)
                            t_hm = C.done(dve, V.tensor_tensor(out=HM[:, fc, th * 512:(th + 1) * 512], in0=ps[bu],
                                                               in1=sg[th], op=ALU.mult))
                            bank_rel[bu] = t_hm
                            sg_rel[th] = t_hm
                    g_rel[slot] = t_u
                    t_gu_last = t_u
                C.need(pe, t_hm)
                C.need(dve, t_gs)
                for db in range(NDB):
                    slot = dcnt % 2
                    dcnt += 1
                    C.need(pool, d_rel[slot])
                    for q in range(6):
                        c0 = q * 2048
                        c1 = min(c0 + 2048, FC * 256)
                        t_w = C.dma(pool, WD[slot][:, c0:c1], wd_d[e, db][:, c0:c1], dsem[slot])
                    C.need(pe, t_w)
                    Wd = WD[slot].rearrange("p (f c) -> p f c", f=FC)
                    for tt in range(8):
                        b = bcnt % 4
                        bcnt += 1
                        C.need(pe, bank_rel[b])
                        for fc in range(FC):
                            mm = T.matmul(ps[b][:, 0:256], lhsT=HM[:, fc, tt * 128:(tt + 1) * 128], rhs=Wd[:, fc, :],
                                          start=(fc == 0), stop=(fc == FC - 1))
                        t_d = C.done(pe, mm)
                        yb = ycnt % 2
                        ycnt += 1
                        C.need(dve, t_d)
                        C.need(dve, y_rel[yb])
                        t_y = C.done(dve, V.tensor_scalar(out=yst[yb], in0=ps[b][:, 0:256],
                                                          scalar1=gs[:, e * 8 + tt:e * 8 + tt + 1], scalar2=None, op0=ALU.mult))
                        bank_rel[b] = t_y
                        C.need(sp, t_y)
                        y_rel[yb] = C.dma(sp, y_o[e, tt * 128:(tt + 1) * 128, db * 256:(db + 1) * 256], yst[yb], ysem[yb])
                    d_rel[slot] = t_d
            C.need(sp, y_rel)
    return nc


def build_combine(K):
    nc = bass.Bass("TRN2", target_bir_lowering=False)
    xm_d = nc.dram_tensor("xm", [1024, D], F32, kind="ExternalInput").ap()
    yk_d = nc.dram_tensor("yk", [K, 1024, D], F32, kind="ExternalInput").ap()
    rows_d = nc.dram_tensor("rows", [3, 128, D], F32, kind="ExternalInput").ap()
    out_o = nc.dram_tensor("out", [1024, D], F32, kind="ExternalOutput").ap()

    def sb(name, cols, dt=F32):
        return nc.alloc_sbuf_tensor("c_" + name, [128, cols], dt)[:]
    rows = sb("rows", 3 * D).rearrange("p (r d) -> p r d", r=3)
    xm = sb("xm", D)
    acc = sb("acc", D)
    yb = [sb("yb%d" % i, D) for i in range(4)]
    ot = sb("ot", D)
    stats = sb("stats", 24)
    small = sb("small", 16)
    C, T, V, A, G = _mk(nc)
    act, dve, sp, pool = C.act, C.dve, C.sp, C.pool
    with nc.Block() as block:
        @block.sync
        def _(se):
            t_rows = C.dma(sp, rows, rows_d.rearrange("r p d -> p r d"), C.newsem())
            xsem = C.newsem()
            ysem = [C.newsem() for _ in range(4)]
            osem = C.newsem()
            y_rel = [None] * 4
            x_rel = None
            o_rel = None
            ycnt = 0
            C.need(dve, t_rows)
            for t in range(8):
                C.need(sp, x_rel)
                t_x = C.dma(sp, xm, xm_d[t * 128:(t + 1) * 128, :], xsem)
                for k in range(K):
                    b = ycnt % 4
                    ycnt += 1
                    q_ = sp if b % 2 == 0 else pool
                    C.need(q_, y_rel[b])
                    t_y = C.dma(q_, yb[b], yk_d[k, t * 128:(t + 1) * 128, :], ysem[b])
                    C.need(dve, t_y)
                    if k == 0:
                        ins = V.tensor_copy(out=acc, in_=yb[b])
                    else:
                        ins = V.tensor_tensor(out=acc, in0=acc, in1=yb[b], op=ALU.add)
                    y_rel[b] = C.done(dve, ins)
                C.need(dve, t_x)
                V.tensor_tensor(out=acc, in0=acc, in1=rows[:, 0, :], op=ALU.mult)
                V.scalar_tensor_tensor(out=acc, in0=xm, scalar=ALPHA, in1=acc, op0=ALU.mult, op1=ALU.add)
                for c4 in range(4):
                    V.bn_stats(out=stats[:, c4 * 6:(c4 + 1) * 6], in_=acc[:, c4 * 512:(c4 + 1) * 512])
                V.bn_aggr(out=small[:, 0:2], in_=stats)
                tv = C.done(dve, V.tensor_scalar(out=small[:, 8:9], in0=small[:, 1:2], scalar1=EPS, scalar2=None, op0=ALU.add))
                x_rel = tv
                C.need(act, tv)
                ts = C.done(act, A.sqrt(out=small[:, 9:10], in_=small[:, 8:9]))
                C.need(dve, ts)
                V.reciprocal(out=small[:, 2:3], in_=small[:, 9:10])
                tk = C.done(dve, V.scalar_tensor_tensor(out=small[:, 3:4], in0=small[:, 0:1], scalar=-1.0, in1=small[:, 2:3],
                                                        op0=ALU.mult, op1=ALU.mult))
                C.need(act, tk)
                C.need(act, o_rel)
                ta = C.done(act, A.activation(out=ot, in_=acc, func=AF.Identity, bias=small[:, 3:4], scale=small[:, 2:3]))
                C.need(dve, ta)
                V.tensor_tensor(out=ot, in0=ot, in1=rows[:, 1, :], op=ALU.mult)
                to = C.done(dve, V.tensor_tensor(out=ot, in0=ot, in1=rows[:, 2, :], op=ALU.add))
                C.need(sp, to)
                o_rel = C.dma(sp, out_o[t * 128:(t + 1) * 128, :], ot, osem)
            C.need(sp, o_rel)
    return nc


def build_combine2(NR):
    nc = bass.Bass("TRN2", target_bir_lowering=False)
    xm_d = nc.dram_tensor("xm", [1024, D], F32, kind="ExternalInput").ap()
    yc_d = nc.dram_tensor("yc", [8, NR * 128, D], F32, kind="ExternalInput").ap()
    S_d = nc.dram_tensor("S", [8, NR * 128, 128], F32, kind="ExternalInput").ap()
    rows_d = nc.dram_tensor("rows", [3, 128, D], F32, kind="ExternalInput").ap()
    out_o = nc.dram_tensor("out", [1024, D], F32, kind="ExternalOutput").ap()

    def sb(name, cols, dt=F32):
        return nc.alloc_sbuf_tensor("c_" + name, [128, cols], dt)[:]
    rows = sb("rows", 3 * D).rearrange("p (r d) -> p r d", r=3)
    xm = [sb("xm%d" % i, D) for i in range(2)]
    yb = [[sb("yb%d_%d" % (i, k), D) for k in range(NR)] for i in range(2)]
    Sb = [sb("S%d" % i, NR * 128).rearrange("p (k c) -> p k c", k=NR) for i in range(2)]
    acc = sb("acc", D)
    ot = sb("ot", D)
    stats = sb("stats", 24)
    small = sb("small", 16)
    ps = [nc.alloc_psum_tensor("c_ps%d" % i, [128, 512], F32)[:] for i in range(8)]
    C, T, V, A, G = _mk(nc)
    pe, act, dve, sp, pool = C.pe, C.act, C.dve, C.sp, C.pool
    with nc.Block() as block:
        @block.sync
        def _(se):
            t_rows = C.dma(sp, rows, rows_d.rearrange("r p d -> p r d"), C.newsem())
            lsem = [C.newsem(), C.newsem()]
            ysem = [[C.newsem() for _ in range(NR)] for _ in range(2)]
            osem = C.newsem()
            in_rel = [None, None]
            bank_rel = [None] * 8
            o_rel = None
            C.need(dve, t_rows)
            ltok = {}

            def loads(t):
                b = t % 2
                C.need(sp, in_rel[b])
                C.need(pool, in_rel[b])
                C.dma(sp, xm[b], xm_d[t * 128:(t + 1) * 128, :], lsem[b])
                tl = C.dma(sp, Sb[b], S_d[t].rearrange("(k p) c -> p k c", p=128), lsem[b])
                ty = []
                for k in range(NR):
                    q_ = pool if k % 2 == 0 else sp
                    ty.append(C.dma(q_, yb[b][k], yc_d[t, k * 128:(k + 1) * 128, :], ysem[b][k]))
                ltok[t] = (tl, ty)

            loads(0)
            for t in range(8):
                if t + 1 < 8:
                    loads(t + 1)
                b = t % 2
                tl, ty = ltok[t]
                C.need(pe, tl)
                for dg in range(4):
                    bk = b * 4 + dg
                    C.need(pe, bank_rel[bk])
                    for k in range(NR):
                        C.need(pe, ty[k])
                        mm = T.matmul(ps[bk], lhsT=Sb[b][:, k, :], rhs=yb[b][k][:, dg * 512:(dg + 1) * 512],
                                      start=(k == 0), stop=(k == NR - 1))
                t_mm = C.done(pe, mm)
                C.need(dve, t_mm)
                C.need(dve, tl)
                for dg in range(4):
                    ins = V.tensor_tensor(out=acc[:, dg * 512:(dg + 1) * 512], in0=ps[b * 4 + dg],
                                          in1=rows[:, 0, dg * 512:(dg + 1) * 512], op=ALU.mult)
                t_ev = C.done(dve, ins)
                for dg in range(4):
                    bank_rel[b * 4 + dg] = t_ev
                t_x = C.done(dve, V.scalar_tensor_tensor(out=acc, in0=xm[b], scalar=ALPHA, in1=acc, op0=ALU.mult, op1=ALU.add))
                in_rel[b] = [t_mm, t_x]
                for c4 in range(4):
                    V.bn_stats(out=stats[:, c4 * 6:(c4 + 1) * 6], in_=acc[:, c4 * 512:(c4 + 1) * 512])
                V.bn_aggr(out=small[:, 0:2], in_=stats)
                tv = C.done(dve, V.tensor_scalar(out=small[:, 8:9], in0=small[:, 1:2], scalar1=EPS, scalar2=None, op0=ALU.add))
                C.need(act, tv)
                ts = C.done(act, A.sqrt(out=small[:, 9:10], in_=small[:, 8:9]))
                C.need(dve, ts)
                V.reciprocal(out=small[:, 2:3], in_=small[:, 9:10])
                tk = C.done(dve, V.scalar_tensor_tensor(out=small[:, 3:4], in0=small[:, 0:1], scalar=-1.0, in1=small[:, 2:3],
                                                        op0=ALU.mult, op1=ALU.mult))
                C.need(act, tk)
                C.need(act, o_rel)
                ta = C.done(act, A.activation(out=ot, in_=acc, func=AF.Identity, bias=small[:, 3:4], scale=small[:, 2:3]))
                C.need(dve, ta)
                V.tensor_tensor(out=ot, in0=ot, in1=rows[:, 1, :], op=ALU.mult)
                to = C.done(dve, V.tensor_tensor(out=ot, in0=ot, in1=rows[:, 2, :], op=ALU.add))
                C.need(sp, to)
                o_rel = C.dma(sp, out_o[t * 128:(t + 1) * 128, :], ot, osem)
            C.need(sp, o_rel)
    return nc


def _run(nc, maps):
    return run_bass_kernel_spmd(nc, maps, core_ids=list(range(NCORES))).results


def kernel(**inputs):
    f = np.float32
    cores = list(range(NCORES))
    mod, mod_c = run_mod(inputs)
    nc, _ = build()
    res = _run(nc, host_inputs(inputs, mod, mod_c))
    x_mid = np.concatenate([np.asarray(r["x_mid"]) for r in res], 0)
    h2b = np.concatenate([np.asarray(r["h2b"]) for r in res], 0)
    aff = np.ascontiguousarray(np.concatenate([np.asarray(r["aff"], f) for r in res], 0))
    res = _run(build_route(), [{"aff": aff} for _ in cores])
    mask = np.asarray(res[0]["mask"]) > 0.5
    idx = np.zeros((16, CAP), np.int64)
    valid = np.zeros((16, CAP), bool)
    for e in range(16):
        ii = np.nonzero(mask[:, e])[0][:CAP]
        idx[e, :len(ii)] = ii
        valid[e, :len(ii)] = True
    w_gate, w_up, w_down = inputs["w_gate"], inputs["w_up"], inputs["w_down"]
    maps = []
    for i in cores:
        m = {}
        xs = []
        gsl = np.zeros((128, 16), f)
        for j in range(2):
            e = 2 * i + j
            rows = h2b[idx[e]]
            xs.append(np.ascontiguousarray(rows.T.reshape(KC, 128, CAP).transpose(1, 0, 2)).reshape(128, KC * CAP))
            ge = np.where(valid[e], aff[idx[e], e], 0).astype(f)
            gsl[:, j * 8:(j + 1) * 8] = ge.reshape(8, 128).T
        m["xsT"] = np.stack(xs, 0)
        m["gsl"] = gsl
        for nm, w in (("wg", w_gate), ("wu", w_up)):
            m[nm] = np.stack([np.ascontiguousarray(
                np.asarray(w[0, 2 * i + j], f).reshape(KC, 128, NFG, 256).transpose(2, 1, 0, 3)).reshape(NFG, 128, KC * 256)
                for j in range(2)], 0)
        m["wd"] = np.stack([np.ascontiguousarray(
            np.asarray(w_down[0, 2 * i + j], f).reshape(FC, 128, NDB, 256).transpose(2, 1, 0, 3)).reshape(NDB, 128, FC * 256)
            for j in range(2)], 0)
        maps.append(m)
    res = _run(build_experts(), maps)
    del maps
    y_all = np.concatenate([np.asarray(r["y"], f) for r in res], 0)
    pe_ = np.repeat(np.arange(16), CAP)[valid.reshape(-1)]
    ps_ = np.tile(np.arange(CAP), 16)[valid.reshape(-1)]
    pt_ = idx.reshape(-1)[valid.reshape(-1)]
    order = np.argsort(pt_, kind="stable")
    pe_, ps_, pt_ = pe_[order], ps_[order], pt_[order]
    bounds = np.searchsorted(pt_, np.arange(0, 8192 + 1, 128))
    NR = max(1, int(-(-int(np.max(np.diff(bounds))) // 128)))
    rows3 = np.ascontiguousarray(np.stack([np.broadcast_to(v[None], (128, D)) for v in
                                           (mod[5], np.asarray(inputs["ln2_g"], f)[0], np.asarray(inputs["ln2_b"], f)[0])], 0))
    maps = []
    for i in cores:
        yc = np.zeros((8, NR * 128, D), f)
        S = np.zeros((8, NR * 128, 128), f)
        for t_ in range(8):
            lo_, hi_ = bounds[8 * i + t_], bounds[8 * i + t_ + 1]
            n_ = hi_ - lo_
            yc[t_, :n_] = y_all[pe_[lo_:hi_], ps_[lo_:hi_]]
            S[t_, np.arange(n_), pt_[lo_:hi_] - (1024 * i + 128 * t_)] = 1.0
        maps.append({"xm": np.ascontiguousarray(x_mid[1024 * i:1024 * (i + 1)]), "yc": yc, "S": S, "rows": rows3})
    res = _run(build_combine2(NR), maps)
    out = np.concatenate([np.asarray(r["out"], f) for r in res], 0)
    return out.reshape(1, 8192, D).astype(f)
```
